# Optimizing a Trainium2 kernel written in Bass

```python
import jax, jax.numpy as jnp
from jax import lax
import numpy as np

D_MODEL = 1024
BATCH = 8
SEQ = 4096
DEPTH = 1

GRID_W = 64
CTX_LEN = 256
D_MIX = D_MODEL
RET_DIM = 64
RET_WIDTH = D_MIX // 2
RET_HEADS = RET_WIDTH // RET_DIM
RWKV_DIM = 64
RWKV_WIDTH = D_MIX - RET_WIDTH
RWKV_HEADS = RWKV_WIDTH // RWKV_DIM
DECAY_LORA = 64
ICLR_LORA = 64
GATE_LORA = 128
CHUNK = 128
CONV_W = 3
N_EXPERTS = 16
EC_CAPACITY = 2
D_EXPERT = D_MODEL
ROPE_BASE = 10000.0
NORM_EPS = 1e-6
RWKV_GN_EPS = 64e-5
RET_COLS = 5 * RET_WIDTH
RWKV_SPLITS = (RWKV_WIDTH, RWKV_WIDTH, RWKV_WIDTH, DECAY_LORA, DECAY_LORA, ICLR_LORA, GATE_LORA, GATE_LORA)
RWKV_COLS = sum(RWKV_SPLITS)
D_IN = RET_COLS + RWKV_COLS

kernel_name = 'hybrid_retention_rwkv7_ecmoe_dit_layer'


def split_cols(u, sizes):
    outs, start = [], 0
    for s in sizes:
        outs.append(u[..., start:start + s])
        start += s
    return outs


def flip(t):
    return jnp.flip(t, axis=1)


def rms_norm(x, g):
    xf = x.astype(jnp.float32)
    y = xf * lax.rsqrt(jnp.mean(xf * xf, axis=-1, keepdims=True) + NORM_EPS)
    return (y * g.astype(jnp.float32)).astype(x.dtype)


def head_rms(y):
    y = y * lax.rsqrt(jnp.mean(y * y, axis=-1, keepdims=True) + NORM_EPS)
    return y.reshape(y.shape[0], y.shape[1], -1)


def depthwise_conv(x, w):
    return lax.conv_general_dilated(x, w[:, None, :].astype(x.dtype), window_strides=(1,), padding='SAME',
                                    dimension_numbers=('NWC', 'WIO', 'NWC'), feature_group_count=x.shape[-1])


def axial_rope(x):
    n_tok, d = x.shape[1], x.shape[-1]
    rows = n_tok // GRID_W
    row = jnp.repeat(jnp.arange(rows, dtype=jnp.float32), GRID_W)
    col = jnp.tile(jnp.arange(GRID_W, dtype=jnp.float32), rows)
    n_freq = d // 4
    freq = ROPE_BASE ** (-jnp.arange(n_freq, dtype=jnp.float32) / n_freq)
    ang = jnp.concatenate([row[:, None] * freq, col[:, None] * freq], axis=-1)[None, :, None, :]
    cos, sin = jnp.cos(ang), jnp.sin(ang)
    x1, x2 = x[..., : d // 2], x[..., d // 2:]
    return jnp.concatenate([x1 * cos - x2 * sin, x2 * cos + x1 * sin], axis=-1)


def retention_scan(q, k, v, log_gamma, s0):
    B, T, H, d = q.shape
    n = T // CHUNK
    to_chunks = lambda t: t.reshape(B, n, CHUNK, H, d).transpose(1, 0, 3, 2, 4)
    idx = jnp.arange(CHUNK, dtype=jnp.float32)
    rel = idx[:, None] - idx[None, :]
    decay_in = jnp.where(rel >= 0, jnp.exp(jnp.maximum(rel, 0.0) * log_gamma[:, None, None]), 0.0)
    q_dec = jnp.exp((idx + 1.0) * log_gamma[:, None])[..., None]
    k_dec = jnp.exp((CHUNK - 1.0 - idx) * log_gamma[:, None])[..., None]
    chunk_dec = jnp.exp(CHUNK * log_gamma)[:, None, None]

    def step(S, inp):
        qc, kc, vc = inp
        scores = jnp.einsum('bhid,bhjd->bhij', qc, kc) * decay_in
        out = jnp.einsum('bhij,bhjd->bhid', scores, vc) + jnp.einsum('bhid,bhde->bhie', qc * q_dec, S)
        S = S * chunk_dec + jnp.einsum('bhjd,bhje->bhde', kc * k_dec, vc)
        return S, out

    S_fin, ys = lax.scan(step, s0, (to_chunks(q), to_chunks(k), to_chunks(v)))
    return ys.transpose(1, 0, 3, 2, 4).reshape(B, T, H, d), S_fin


def retention_group(u, uc, need_ctx):
    def split_heads(t):
        q, k, v, gf, gb = split_cols(t.astype(jnp.float32), (RET_WIDTH,) * 5)
        hd = lambda a: a.reshape(a.shape[0], a.shape[1], RET_HEADS, RET_DIM)
        return hd(q), hd(k) * RET_DIM ** -0.5, hd(v), gf, gb

    q, k, v, gf, gb = split_heads(u)
    qc, kc, vc, gfc, gbc = split_heads(uc)
    q, k = axial_rope(q), axial_rope(k)
    log_gamma = jnp.log1p(-jnp.exp2(-5.0 - jnp.arange(RET_HEADS, dtype=jnp.float32)))
    s0 = jnp.zeros((u.shape[0], RET_HEADS, RET_DIM, RET_DIM), jnp.float32)
    yc_f, sc_f = retention_scan(qc, kc, vc, log_gamma, s0)
    yc_b, sc_b = retention_scan(flip(qc), flip(kc), flip(vc), log_gamma, s0)
    y_f, _ = retention_scan(q, k, v, log_gamma, sc_f)
    y_b, _ = retention_scan(flip(q), flip(k), flip(v), log_gamma, sc_b)
    lat = jax.nn.silu(gf) * head_rms(y_f) + jax.nn.silu(gb) * head_rms(flip(y_b))
    if not need_ctx:
        return lat, None
    ctx_out = jax.nn.silu(gfc) * head_rms(yc_f) + jax.nn.silu(gbc) * head_rms(flip(yc_b))
    return lat, ctx_out


def rwkv_prepare(u, w0, w2, a0, a2, g2, k_k, k_a, r_k):
    u = u.astype(jnp.float32)
    B, T, _ = u.shape
    r, k, v, wl_f, wl_b, al, gl_f, gl_b = split_cols(u, RWKV_SPLITS)
    heads = lambda t: t.reshape(B, T, RWKV_HEADS, RWKV_DIM)

    def decay(wl, w0_d, w2_d):
        w_log = -jax.nn.softplus(-(w0_d + jnp.tanh(wl) @ w2_d)) - 0.5
        return heads(jnp.exp(-jnp.exp(w_log)))

    iclr = jax.nn.sigmoid(a0 + al @ a2)
    kk = heads(k * k_k)
    kk = kk / jnp.maximum(jnp.linalg.norm(kk, axis=-1, keepdims=True), 1e-12)
    k_mod = heads(k * (1.0 + (iclr - 1.0) * k_a))
    r, v = heads(r), heads(v)
    bonus = (jnp.sum(r * k_mod * r_k.reshape(RWKV_HEADS, RWKV_DIM), axis=-1, keepdims=True) * v).reshape(B, T, -1)
    g_f = jax.nn.sigmoid(gl_f) @ g2[0]
    g_b = jax.nn.sigmoid(gl_b) @ g2[1]
    return (r, decay(wl_f, w0[0], w2[0]), decay(wl_b, w0[1], w2[1]), k_mod, v, -kk, kk * heads(iclr), g_f, g_b, bonus)


def rwkv7_scan(r, w, k, v, a, b, s0):
    def step(S, inp):
        r_t, w_t, k_t, v_t, a_t, b_t = inp
        sa = jnp.einsum('bhvk,bhk->bhv', S, a_t)
        S = S * w_t[:, :, None, :] + sa[..., None] * b_t[:, :, None, :] + v_t[..., None] * k_t[:, :, None, :]
        return S, jnp.einsum('bhvk,bhk->bhv', S, r_t)

    xs = tuple(jnp.moveaxis(t, 1, 0) for t in (r, w, k, v, a, b))
    S_fin, ys = lax.scan(step, s0, xs)
    return jnp.moveaxis(ys, 0, 1), S_fin


def rwkv_group_norm(y, w, b):
    mu = jnp.mean(y, axis=-1, keepdims=True)
    var = jnp.mean((y - mu) ** 2, axis=-1, keepdims=True)
    yn = ((y - mu) * lax.rsqrt(var + RWKV_GN_EPS)).reshape(y.shape[0], y.shape[1], -1)
    return yn * w + b


def rwkv_group(rw, rwc, w0, w2, a0, a2, g2, k_k, k_a, r_k, lnx_w, lnx_b, need_ctx):
    r, w_f, w_b, k, v, a, b, g_f, g_b, bonus = rwkv_prepare(rw, w0, w2, a0, a2, g2, k_k, k_a, r_k)
    rc, wc_f, wc_b, kc, vc, ac, bc, gc_f, gc_b, bonus_c = rwkv_prepare(rwc, w0, w2, a0, a2, g2, k_k, k_a, r_k)
    s0 = jnp.zeros((rw.shape[0], RWKV_HEADS, RWKV_DIM, RWKV_DIM), jnp.float32)
    yc_f, sc_f = rwkv7_scan(rc, wc_f, kc, vc, ac, bc, s0)
    yc_b, sc_b = rwkv7_scan(*[flip(t) for t in (rc, wc_b, kc, vc, ac, bc)], s0)
    y_f, _ = rwkv7_scan(r, w_f, k, v, a, b, sc_f)
    y_b, _ = rwkv7_scan(*[flip(t) for t in (r, w_b, k, v, a, b)], sc_b)
    lat = g_f * (rwkv_group_norm(y_f, lnx_w, lnx_b) + bonus) + g_b * (rwkv_group_norm(flip(y_b), lnx_w, lnx_b) + bonus)
    if not need_ctx:
        return lat, None
    ctx_out = (gc_f * (rwkv_group_norm(yc_f, lnx_w, lnx_b) + bonus_c)
               + gc_b * (rwkv_group_norm(flip(yc_b), lnx_w, lnx_b) + bonus_c))
    return lat, ctx_out


def token_mix(h, hc, w_in, conv_w, w0, w2, a0, a2, g2, k_k, k_a, r_k, lnx_w, lnx_b, w_out, need_ctx):
    u, uc = h @ w_in, hc @ w_in
    ret_lat, ret_ctx = retention_group(u[..., :RET_COLS], uc[..., :RET_COLS], need_ctx)
    rw = depthwise_conv(u[..., RET_COLS:], conv_w)
    rwc = depthwise_conv(uc[..., RET_COLS:], conv_w)
    rwkv_lat, rwkv_ctx = rwkv_group(rw, rwc, w0, w2, a0, a2, g2, k_k, k_a, r_k, lnx_w, lnx_b, need_ctx)
    lat = jnp.concatenate([ret_lat, rwkv_lat], axis=-1).astype(h.dtype) @ w_out
    if not need_ctx:
        return lat, None
    ctx_out = jnp.concatenate([ret_ctx, rwkv_ctx], axis=-1).astype(hc.dtype) @ w_out
    return lat, ctx_out


def ec_moe(h, w_router, w_gate, w_up, w_down):
    B, T, D = h.shape
    cap = max(1, EC_CAPACITY * T // N_EXPERTS)
    affinity = jax.nn.softmax((h @ w_router).astype(jnp.float32), axis=-1)
    gate_vals, tok_idx = lax.top_k(jnp.swapaxes(affinity, 1, 2), cap)
    xs = jax.vmap(lambda hb, ib: hb[ib])(h, tok_idx)
    hid = jax.nn.silu(jnp.einsum('becd,edf->becf', xs, w_gate)) * jnp.einsum('becd,edf->becf', xs, w_up)
    ye = jnp.einsum('becf,efd->becd', hid, w_down) * gate_vals[..., None].astype(h.dtype)
    return jax.vmap(lambda yb, ib: jnp.zeros((T, D), yb.dtype).at[ib.reshape(-1)].add(yb.reshape(-1, D)))(ye, tok_idx)


def setup_inputs(seed: int = 0) -> dict:
    key = jax.random.key(seed)
    ks = jax.random.split(key, 24)
    nrm = lambda k, shape, s: s * jax.random.normal(k, shape, jnp.float32)
    L, D, W, E, F = DEPTH, D_MODEL, RWKV_WIDTH, N_EXPERTS, D_EXPERT
    return {
        'x': nrm(ks[0], (BATCH, SEQ, D), 1.0),
        'c': nrm(ks[1], (BATCH, D), 1.0),
        'ctx': nrm(ks[2], (BATCH, CTX_LEN, D), 1.0),
        'c_ctx': nrm(ks[3], (D,), 1.0),
        'w_mod': nrm(ks[4], (L, D, 6 * D), 0.5 * D ** -0.5),
        'b_mod': nrm(ks[5], (L, 6 * D), 0.02),
        'norm_gains': 1.0 + nrm(ks[6], (L, 4, D), 0.1),
        'w_in': nrm(ks[7], (L, D, D_IN), D ** -0.5),
        'rwkv_conv': jnp.array([0.3, 1.0, 0.3], jnp.float32)[None, :, None] + nrm(ks[8], (L, CONV_W, RWKV_COLS), 0.05),
        'rwkv_w0': jnp.linspace(-6.0, -1.0, W, dtype=jnp.float32) + nrm(ks[9], (L, 2, W), 0.1),
        'rwkv_w2': nrm(ks[10], (L, 2, DECAY_LORA, W), 0.5 * DECAY_LORA ** -0.5),
        'rwkv_a0': nrm(ks[11], (L, W), 0.1),
        'rwkv_a2': nrm(ks[12], (L, ICLR_LORA, W), 0.5 * ICLR_LORA ** -0.5),
        'rwkv_g2': nrm(ks[13], (L, 2, GATE_LORA, W), GATE_LORA ** -0.5),
        'rwkv_k_k': 0.85 + nrm(ks[14], (L, W), 0.05),
        'rwkv_k_a': 1.0 + nrm(ks[15], (L, W), 0.05),
        'rwkv_r_k': nrm(ks[16], (L, W), 0.1),
        'rwkv_lnx_w': 1.0 + nrm(ks[17], (L, W), 0.1),
        'rwkv_lnx_b': nrm(ks[18], (L, W), 0.02),
        'w_out': nrm(ks[19], (L, D_MIX, D), D_MIX ** -0.5),
        'w_router': nrm(ks[20], (L, D, E), D ** -0.5),
        'w_gate': nrm(ks[21], (L, E, D, F), D ** -0.5),
        'w_up': nrm(ks[22], (L, E, D, F), D ** -0.5),
        'w_down': nrm(ks[23], (L, E, F, D), F ** -0.5),
    }


def reference(x, c, ctx, c_ctx, w_mod, b_mod, norm_gains, w_in, rwkv_conv, rwkv_w0, rwkv_w2, rwkv_a0, rwkv_a2,
              rwkv_g2, rwkv_k_k, rwkv_k_a, rwkv_r_k, rwkv_lnx_w, rwkv_lnx_b, w_out, w_router, w_gate, w_up, w_down):
    for i in range(DEPTH):
        need_ctx = i < DEPTH - 1
        sh1, sc1, gt1, sh2, sc2, gt2 = jnp.split((jax.nn.silu(c) @ w_mod[i] + b_mod[i])[:, None, :], 6, axis=-1)
        csh1, csc1, cgt1, csh2, csc2, cgt2 = jnp.split(jax.nn.silu(c_ctx) @ w_mod[i] + b_mod[i], 6, axis=-1)
        g_pre_mix, g_post_mix, g_pre_ffn, g_post_ffn = norm_gains[i]
        h = rms_norm(x, g_pre_mix) * (1.0 + sc1) + sh1
        hc = rms_norm(ctx, g_pre_mix) * (1.0 + csc1) + csh1
        mix, mix_c = token_mix(h, hc, w_in[i], rwkv_conv[i], rwkv_w0[i], rwkv_w2[i], rwkv_a0[i], rwkv_a2[i],
                               rwkv_g2[i], rwkv_k_k[i], rwkv_k_a[i], rwkv_r_k[i], rwkv_lnx_w[i], rwkv_lnx_b[i],
                               w_out[i], need_ctx)
        x = x + gt1 * rms_norm(mix, g_post_mix)
        h2 = rms_norm(x, g_pre_ffn) * (1.0 + sc2) + sh2
        x = x + gt2 * rms_norm(ec_moe(h2, w_router[i], w_gate[i], w_up[i], w_down[i]), g_post_ffn)
        if need_ctx:
            ctx = ctx + cgt1 * rms_norm(mix_c, g_post_mix)
            hc2 = rms_norm(ctx, g_pre_ffn) * (1.0 + csc2) + csh2
            ctx = ctx + cgt2 * rms_norm(ec_moe(hc2, w_router[i], w_gate[i], w_up[i], w_down[i]), g_post_ffn)
    return x
```

```python
import numpy as np
from contextlib import ExitStack
import concourse.bass as bass
import concourse.mybir as mybir
from concourse.bass_utils import run_bass_kernel_spmd

F32 = mybir.dt.float32
BF16 = mybir.dt.bfloat16
U32 = mybir.dt.uint32
I32 = mybir.dt.int32
AF = mybir.ActivationFunctionType
ALU = mybir.AluOpType
AX = mybir.AxisListType

D = 1024
SEQ = 4096
CTX = 256
NCORES = 8
CTX0 = 1
LAT0 = 259
NT = 4356
EPS = 1e-6
import os
KLIM = int(os.environ.get('KLIM', '999'))
KW = int(os.environ.get('KW', '4'))
KRW = int(os.environ.get('KRW', '1'))
KSKEW_RET = int(os.environ.get('KSKEW_RET', '0'))
KSKEW_RW = int(os.environ.get('KSKEW_RW', '0'))
KSTOP = float(os.environ.get('KSTOP', '99'))
KC = int(os.environ.get('KC', '9'))


_UID = [0]


def U(name):
    _UID[0] += 1
    return "%s_u%d" % (name, _UID[0])


class Res:
    __slots__ = ("name", "w", "rs", "dsem")

    def __init__(self, name=""):
        self.name = name
        self.w = None
        self.rs = {}
        self.dsem = None


class Sched:
    ENG = ["tensor", "vector", "scalar", "gpsimd", "sync"]

    def __init__(self, nc):
        self.nc = nc
        self.sem = {n: nc.alloc_semaphore("sem_" + n) for n in self.ENG}
        self.cnt = {n: 0 for n in self.ENG}
        self.waited = {n: {} for n in self.ENG}
        self.ops = {n: [] for n in self.ENG}
        self.pending = {n: {} for n in self.ENG}
        self.dfree = []
        self.dcnt = {}
        self.dres = []
        self.allres = []
        self.nwaits = 0
        self.nops = 0
        self.mute = False

    def stage(self, n):
        self.mute = n > KSTOP

    def res(self, name=""):
        r = Res(name)
        self.allres.append(r)
        return r

    def _need(self, eng, reads, writes):
        evs = []
        for r in reads:
            if r.w is not None:
                evs.append(r.w)
        for w in writes:
            if w.w is not None:
                evs.append(w.w)
            evs.extend(w.rs.values())
        need = {}
        own = self.sem[eng].num
        wd = self.waited[eng]
        for (s, v) in evs:
            if eng == "tensor" and s.num == own:
                continue
            if wd.get(s.num, 0) >= v:
                continue
            if s.num not in need or need[s.num][1] < v:
                need[s.num] = (s, v)
        for k, (s, v) in need.items():
            wd[k] = v
        self.nwaits += len(need)
        return list(need.values())

    def _mark(self, ev, reads, writes):
        for r in reads:
            if r in writes:
                continue
            k = ev[0].num
            if k not in r.rs or r.rs[k][1] < ev[1]:
                r.rs[k] = ev
        for w in writes:
            w.w = ev
            w.rs = {}

    def op(self, eng, fn, reads=(), writes=()):
        if self.mute:
            return
        need = self._need(eng, reads, writes)
        self.cnt[eng] += 1
        sem = self.sem[eng]
        ev = (sem, self.cnt[eng])
        self._mark(ev, reads, writes)
        self.nops += 1

        def emit(e, need=need, fn=fn, sem=sem):
            for (s, v) in need:
                e.wait_ge(s, v)
            fn(e).then_inc(sem, 1)

        self.ops[eng].append(emit)

    def dma(self, eng, fn, sres, reads=(), writes=()):
        if self.mute:
            return
        need = self._need(eng, reads, writes)
        if sres.dsem is None:
            if self.dfree:
                sres.dsem = self.dfree.pop()
            else:
                sres.dsem = self.nc.alloc_semaphore("dsem%d" % len(self.dcnt))
                self.dcnt[sres.dsem.num] = 0
            self.dres.append(sres)
        ds = sres.dsem
        self.dcnt[ds.num] += 16
        ev = (ds, self.dcnt[ds.num])
        self._mark(ev, reads, writes)
        self.pending[eng][ds.num] = ev
        self.nops += 1

        def emit(e, need=need, fn=fn, ds=ds):
            for (s, v) in need:
                e.wait_ge(s, v)
            fn(e).then_inc(ds, 16)

        self.ops[eng].append(emit)

    def flush(self):
        nc = self.nc
        self.mute = False
        for n in self.ENG:
            pend = list(self.pending[n].values())
            if pend:
                def emit(e, pend=pend):
                    for (s, v) in pend:
                        e.wait_ge(s, v)
                self.ops[n].append(emit)
            self.pending[n] = {}
        with nc.Block() as block:
            for n in self.ENG:
                ops = self.ops[n]
                if ops:
                    def body(e, ops=ops):
                        for o in ops:
                            o(e)
                    getattr(block, n)(body)
        self.ops = {n: [] for n in self.ENG}
        for r in self.dres:
            self.dfree.append(r.dsem)
            r.dsem = None
        self.dres = []
        for r in self.allres:
            r.w = None
            r.rs = {}
        self.allres = [r for r in self.allres]


class Pool:
    def __init__(self, S, stack, name, shape, dtype, n, psum=False):
        self.tiles = []
        for i in range(n):
            if psum:
                t = stack.enter_context(S.nc.psum_tensor(U("%s%d") % (name, i), shape, dtype))
            else:
                t = stack.enter_context(S.nc.sbuf_tensor(U("%s%d") % (name, i), shape, dtype))
            self.tiles.append((t, S.res("%s%d" % (name, i))))
        self.i = 0

    def next(self):
        t = self.tiles[self.i % len(self.tiles)]
        self.i += 1
        return t


def mk_helpers(S):
    def mm(out, lhsT, rhs, start, stop, reads, writes):
        S.op("tensor", lambda e: e.matmul(out, lhsT=lhsT, rhs=rhs, start=start, stop=stop), reads=reads, writes=writes)

    def tr(out, in_, ident, reads, writes):
        S.op("tensor", lambda e: e.transpose(out=out, in_=in_, identity=ident), reads=reads, writes=writes)

    def act(out, in_, func, reads, writes, **kw):
        S.op("scalar", lambda e: e.activation(out=out, in_=in_, func=func, **kw), reads=reads, writes=writes)

    def tt(out, in0, in1, op, reads, writes, eng="vector"):
        S.op(eng, lambda e: e.tensor_tensor(out=out, in0=in0, in1=in1, op=op), reads=reads, writes=writes)

    def ts(out, in0, s1, s2, op0, op1, reads, writes, eng="vector"):
        S.op(eng, lambda e: e.tensor_scalar(out=out, in0=in0, scalar1=s1, scalar2=s2, op0=op0, op1=op1),
             reads=reads, writes=writes)

    def stt(out, in0, scalar, in1, op0, op1, reads, writes):
        S.op("vector", lambda e: e.scalar_tensor_tensor(out=out, in0=in0, scalar=scalar, in1=in1, op0=op0, op1=op1),
             reads=reads, writes=writes)

    def cp(out, in_, reads, writes, eng="vector"):
        S.op(eng, lambda e: e.tensor_copy(out=out, in_=in_), reads=reads, writes=writes)

    def red(out, in_, reads, writes, op=ALU.add):
        S.op("vector", lambda e: e.tensor_reduce(out=out, in_=in_, axis=AX.X, op=op), reads=reads, writes=writes)

    def rcp(out, in_, reads, writes):
        S.op("vector", lambda e: e.reciprocal(out=out, in_=in_), reads=reads, writes=writes)

    def dma(eng, out, in_, sres, reads=(), writes=()):
        S.dma(eng, lambda e: e.dma_start(out=out, in_=in_), sres, reads=reads, writes=writes)

    return mm, tr, act, tt, ts, stt, cp, red, rcp, dma


def bcast_rows(dram_ap_1d, n, parts=128):
    return bass.AP(dram_ap_1d.tensor, dram_ap_1d.offset, [[0, parts], [1, n]])


def build(debug=()):
    nc = bass.Bass("TRN2", target_bir_lowering=False)
    S = Sched(nc)
    dbg = {}

    def din(name, shape, dt=F32):
        return nc.dram_tensor(name, list(shape), dt, kind="ExternalInput").ap()

    x = din("x", [SEQ, D])
    ctx = din("ctx", [CTX, D])
    ccol = din("ccol", [128, 16])
    w_mod = din("w_mod", [D, 6 * D])
    b_mod = din("b_mod", [6 * D])
    gains = din("gains", [4, D])
    ident_d = din("ident", [128, 128])
    out = nc.dram_tensor("out", [SEQ, D], F32, kind="ExternalOutput").ap()

    def dbg_out(name, shape, dt=F32):
        t = nc.dram_tensor("dbg_" + name, list(shape), dt, kind="ExternalOutput").ap()
        dbg[name] = t
        return t

    glob = ExitStack()
    ident = glob.enter_context(nc.sbuf_tensor(U("ident_sb"), [128, 128], F32))
    identb = glob.enter_context(nc.sbuf_tensor(U("identb_sb"), [128, 128], BF16))
    ident_r = S.res("ident")
    rowmod_d = nc.dram_tensor("rowmod_d", [4 * D], F32, kind="Internal").ap()
    colmod = glob.enter_context(nc.sbuf_tensor(U("colmod"), [128, 4, 8], F32))
    colmod_r = S.res("colmod")
    epsc = glob.enter_context(nc.sbuf_tensor(U("epsc"), [128, 1], F32))
    epsc_r = S.res("epsc")
    aff_all = glob.enter_context(nc.sbuf_tensor(U("aff_all"), [128, 32, 16], F32))
    aff_r = S.res("aff_all")
    if "hT" in debug:
        hT_d = dbg_out("hT", [128, 8, NT], BF16)
    else:
        hT_d = nc.dram_tensor("hT_scr", [128, 8, NT], BF16, kind="Internal").ap()

    def run_interleaved(gens, skew=0):
        gens = list(gens)
        next(gens[0])
        for g in gens[1:]:
            next(g)
        for _ in range(skew):
            try:
                next(gens[-1])
            except StopIteration:
                break
        while gens:
            alive = []
            for g in gens:
                try:
                    next(g)
                    alive.append(g)
                except StopIteration:
                    pass
            gens = alive

    with ExitStack() as st:
        rowmod = st.enter_context(nc.sbuf_tensor(U("rowmod"), [128, 4, D], F32))
        rowmod_r = S.res("rowmod")
        csb = st.enter_context(nc.sbuf_tensor(U("csb"), [128, 16], F32))
        csil = st.enter_context(nc.sbuf_tensor(U("csil"), [128, 16], F32))
        cbc = st.enter_context(nc.sbuf_tensor(U("cbc"), [128, 16, 128], F32))
        c_r = S.res("c")
        gb = st.enter_context(nc.sbuf_tensor(U("gb"), [128, 4, D], F32))
        gb_r = S.res("gb")
        bmb = st.enter_context(nc.sbuf_tensor(U("bmb"), [128, 6 * D], F32))
        bmb_r = S.res("bmb")
        rowc = st.enter_context(nc.sbuf_tensor(U("rowc"), [128, 6 * D], F32))
        rowx = st.enter_context(nc.sbuf_tensor(U("rowx"), [128, 2 * D], F32))
        row_r = S.res("row")
        gcol = st.enter_context(nc.sbuf_tensor(U("gcol"), [128, 8], F32))
        junk = st.enter_context(nc.sbuf_tensor(U("junk0"), [128, 128], F32))
        junk_r = S.res("junk0")
        wpool = Pool(S, st, "wmod", [128, 8, 512], F32, 2)
        pp = Pool(S, st, "ps0", [128, 512], F32, 2, psum=True)

        S.dma("sync", lambda e: e.dma_start(out=csb[:], in_=ccol), c_r, writes=[c_r])
        S.dma("sync", lambda e: e.dma_start(out=ident[:], in_=ident_d), ident_r, writes=[ident_r])
        S.op("vector", lambda e: e.tensor_copy(out=identb[:], in_=ident[:]), reads=[ident_r], writes=[ident_r])
        S.op("vector", lambda e: e.memset(epsc[:], EPS), writes=[epsc_r])
        S.dma("gpsimd", lambda e: e.dma_start(out=gb[:].rearrange("p a d -> p (a d)"),
                                               in_=bcast_rows(gains.rearrange("a d -> (a d)"), 4 * D)),
              gb_r, writes=[gb_r])
        S.dma("gpsimd", lambda e: e.dma_start(out=bmb[:], in_=bcast_rows(b_mod, 6 * D)), bmb_r, writes=[bmb_r])
        S.op("scalar", lambda e: e.activation(out=csil[:], in_=csb[:], func=AF.Silu), reads=[c_r], writes=[c_r])
        S.op("vector", lambda e: e.tensor_copy(out=cbc[:], in_=csil[:].unsqueeze(2).to_broadcast([128, 16, 128])),
             reads=[c_r], writes=[c_r])
        wv = w_mod.rearrange("(k p) n -> p k n", p=128)
        for blk in range(12):
            wt, wr = wpool.next()
            S.dma("sync", lambda e, wt=wt, blk=blk: e.dma_start(out=wt[:], in_=wv[:, :, blk * 512:(blk + 1) * 512]),
                  wr, writes=[wr])
            for which in range(2 if blk < 4 else 1):
                pt, pr = pp.next()
                for k in range(8):
                    S.op("tensor", lambda e, pt=pt, wt=wt, k=k, which=which: e.matmul(
                        pt[:], lhsT=cbc[:, which * 8 + k, :], rhs=wt[:, k, :], start=(k == 0), stop=(k == 7)),
                        reads=[c_r, wr], writes=[pr])
                dst = rowc if which == 0 else rowx
                S.op("vector", lambda e, pt=pt, dst=dst, blk=blk: e.tensor_tensor(
                    out=dst[:, blk * 512:(blk + 1) * 512], in0=pt[:], in1=bmb[:, blk * 512:(blk + 1) * 512],
                    op=ALU.add), reads=[pr, bmb_r], writes=[row_r])
        S.op("vector", lambda e: e.tensor_tensor(out=rowmod[:, 0, :], in0=rowc[:, 2 * D:3 * D], in1=gb[:, 1, :],
                                                 op=ALU.mult), reads=[row_r, gb_r], writes=[rowmod_r])
        S.op("vector", lambda e: e.scalar_tensor_tensor(out=rowmod[:, 1, :], in0=rowc[:, 4 * D:5 * D], scalar=1.0,
                                                        in1=gb[:, 2, :], op0=ALU.add, op1=ALU.mult),
             reads=[row_r, gb_r], writes=[rowmod_r])
        S.op("vector", lambda e: e.tensor_copy(out=rowmod[:, 2, :], in_=rowc[:, 3 * D:4 * D]),
             reads=[row_r], writes=[rowmod_r])
        S.op("vector", lambda e: e.tensor_tensor(out=rowmod[:, 3, :], in0=rowc[:, 5 * D:6 * D], in1=gb[:, 3, :],
                                                 op=ALU.mult), reads=[row_r, gb_r], writes=[rowmod_r])
        S.dma("sync", lambda e: e.dma_start(out=bass.AP(rowmod_d.tensor, 0, [[0, 1], [1, 4 * D]]),
                                            in_=rowmod[0:1].rearrange("p a d -> p (a d)")), rowmod_r, reads=[rowmod_r])
        for src in (rowc, rowx):
            S.op("vector", lambda e, src=src: e.scalar_tensor_tensor(
                out=src[:, D:2 * D], in0=src[:, D:2 * D], scalar=1.0, in1=gb[:, 0, :], op0=ALU.add, op1=ALU.mult),
                reads=[gb_r], writes=[row_r])
        for ci, (src, off) in enumerate([(rowc, D), (rowc, 0), (rowx, D), (rowx, 0)]):
            for j in range(8):
                S.op("vector", lambda e, src=src, off=off, j=j: e.tensor_tensor(
                    out=junk[:], in0=src[:, off + j * 128: off + (j + 1) * 128], in1=ident[:], op=ALU.mult),
                    reads=[row_r, ident_r], writes=[junk_r])
                S.op("vector", lambda e, j=j, ci=ci: e.tensor_reduce(
                    out=colmod[:, ci, j:j + 1], in_=junk[:], axis=AX.X, op=ALU.add),
                    reads=[junk_r], writes=[colmod_r])
        S.flush()

    mm, tr, act, tt, ts, stt, cp, red, rcp, dma = mk_helpers(S)
    w_in = din("w_in", [D, 4544])
    prot_d = din("prot", [128, 128])
    cosT_d = din("cosT", [128, NT])
    sinT_d = din("sinT", [128, NT])
    qdec_d = din("qdec", [2, 128, 512])
    kdec_d = din("kdec", [2, 128, 512])
    dmask_d = din("dmask", [2, 128, 1024])
    gC_d = din("gC", [128, 512])
    if "lat_f" in debug:
        lat_d = [dbg_out("lat_f", [SEQ, D], BF16), dbg_out("lat_b", [SEQ, D], BF16)]
    else:
        lat_d = [nc.dram_tensor("lat_f", [SEQ, D], BF16, kind="Internal").ap(),
                 nc.dram_tensor("lat_b", [SEQ, D], BF16, kind="Internal").ap()]

    def load_w_bf16(st_pool, dst, dst_r, src_ap, ncols):
        v = src_ap.rearrange("(k p) n -> p k n", p=128)
        for c0 in range(0, ncols, 256):
            cw = min(256, ncols - c0)
            stg, sr = st_pool.next()
            dma("sync", stg[:, :, :cw], v[:, :, c0:c0 + cw], sr, writes=[sr])
            act(dst[:, :, c0:c0 + cw], stg[:, :, :cw], AF.Copy, [sr], [dst_r])


    RC = 2560
    rw_wstack = ExitStack()
    Wrw = rw_wstack.enter_context(nc.sbuf_tensor(U("Wrw"), [128, 8, 1536 + 512], BF16))
    Wrw_r = S.res("Wrw")
    ret_wstack = ExitStack()
    wsb = ret_wstack.enter_context(nc.sbuf_tensor(U("ret_w"), [128, 5, 8, 512], BF16))
    wsb_r = S.res("ret_w")

    with ExitStack() as st:
        stg_pool1 = Pool(S, st, "wstg", [128, 8, 256], F32, 2)
        pf1 = []
        for wi, c0w in enumerate([0, 512, 1024, 1536, 2048]):
            for hh_ in range(2):
                pf1.append((wi, c0w, hh_))

        def prefetch1():
            if not pf1:
                return
            wi, c0w, hh_ = pf1.pop(0)
            stg, sr = stg_pool1.next()
            vv = w_in[:, c0w + hh_ * 256:c0w + hh_ * 256 + 256].rearrange("(k p) n -> p k n", p=128)
            dma("gpsimd", stg[:], vv, sr, writes=[sr])
            cp(wsb[:, wi, :, hh_ * 256:(hh_ + 1) * 256], stg[:], [sr], [wsb_r])
        zp = st.enter_context(nc.sbuf_tensor(U("zpad"), [128, 8, 2], BF16))
        zp_r = S.res("zpad")
        S.op("gpsimd", lambda e: e.memset(zp[:], 0.0), writes=[zp_r])
        with nc.allow_non_contiguous_dma(reason="tiny zero pad columns"):
            pass
        for col in (0, CTX0 + CTX, CTX0 + CTX + 1, NT - 1):
            S.dma("gpsimd", lambda e, col=col: e.dma_start(out=hT_d[:, :, col:col + 1], in_=zp[:, :, 0:1],
                                                           allow_slow_non_contiguous=True), zp_r, reads=[zp_r])
        tiles = [(ctx, i, CTX0 + i * 128, 2) for i in range(CTX // 128)] + \
                [(x, i, LAT0 + i * 128, 0) for i in range(SEQ // 128)]
        groups = [tiles[0:2]] + [tiles[2 + 4 * g_:2 + 4 * g_ + 4] for g_ in range(SEQ // 512)]

        def p1_gen(par):
            hTp = Pool(S, st, "hTt", [128, 8, 512], BF16, 2)
            xp = Pool(S, st, "xin", [128, D], F32, 2)
            xnp = Pool(S, st, "xn", [128, D], BF16, 1)
            sqp = Pool(S, st, "sqj", [128, D], BF16, 1)
            stp = Pool(S, st, "stat", [128, 4], F32, 2)
            tp = Pool(S, st, "pst", [128, 8, 128], BF16, 1, psum=True)
            yield
            for grp in groups[par::2]:
                for (src, i, c0, ci) in grp:
                    if ci == 0 and i >= 1:
                        prefetch1()
                    xt, xr = xp.next()
                    S.dma("sync", lambda e, xt=xt, src=src, i=i: e.dma_start(out=xt[:], in_=src[i * 128:(i + 1) * 128, :]),
                          xr, writes=[xr])
                    sq, sqr = sqp.next()
                    stq, sr = stp.next()
                    S.op("scalar", lambda e, sq=sq, xt=xt, stq=stq: e.activation(
                        out=sq[:], in_=xt[:], func=AF.Square, accum_out=stq[:, 0:1]), reads=[xr], writes=[sqr, sr])
                    S.op("scalar", lambda e, stq=stq: e.activation(
                        out=stq[:, 1:2], in_=stq[:, 0:1], func=AF.Sqrt, bias=epsc[:], scale=1.0 / D),
                        reads=[sr, epsc_r], writes=[sr])
                    S.op("vector", lambda e, stq=stq: e.reciprocal(out=stq[:, 2:3], in_=stq[:, 1:2]), reads=[sr], writes=[sr])
                    xn, xnr = xnp.next()
                    S.op("scalar", lambda e, xn=xn, xt=xt, stq=stq: e.activation(
                        out=xn[:], in_=xt[:], func=AF.Copy, scale=stq[:, 2:3]), reads=[xr, sr], writes=[xnr])
                    yield
                    pt, pr = tp.next()
                    for j in range(8):
                        S.op("tensor", lambda e, pt=pt, xn=xn, j=j: e.transpose(
                            out=pt[:, j, :], in_=xn[:, j * 128:(j + 1) * 128], identity=identb[:]),
                            reads=[xnr, ident_r], writes=[pr])
                    gsz = 2 if ci == 2 else 4
                    gi = i % gsz
                    if gi == 0:
                        ht, htr = hTp.next()
                    for j in range(8):
                        S.op("vector", lambda e, pt=pt, j=j, ht=ht, ci=ci, gi=gi: e.tensor_scalar(
                            out=ht[:, j, gi * 128:(gi + 1) * 128], in0=pt[:, j, :], scalar1=colmod[:, ci, j:j + 1],
                            scalar2=colmod[:, ci + 1, j:j + 1], op0=ALU.mult, op1=ALU.add),
                            reads=[pr, colmod_r], writes=[htr])
                    if gi == gsz - 1:
                        cb = c0 - gi * 128
                        S.dma("sync", lambda e, ht=ht, cb=cb, gsz=gsz: e.dma_start(
                            out=hT_d[:, :, cb:cb + gsz * 128], in_=ht[:, :, 0:gsz * 128]), htr, reads=[htr])
                    yield

        run_interleaved([p1_gen(0), p1_gen(1)])
        while pf1:
            prefetch1()
        S.flush()

    def dscr(name, shape, dt):
        if name in debug:
            return dbg_out(name, shape, dt)
        return nc.dram_tensor(name + "_scr", list(shape), dt, kind="Internal").ap()

    x1_d = dscr("x1", [SEQ, D], F32)
    h2_d = dscr("h2", [SEQ, D], BF16)
    ymoe_d = dscr("moe", [SEQ, D], F32)
    NTILE = SEQ // 128

    def ret_all():
        with ExitStack() as st:
            stg_pool = Pool(S, st, "wstg2", [128, 8, 256], F32, 2)
            pieces = [(0, RC, 1536)]
            for dr in range(2):
                b0 = 1536 + 256 * dr
                pieces += [(b0, RC + 1536 + 64 * dr, 64), (b0 + 64, RC + 1664, 64), (b0 + 128, RC + 1728 + 128 * dr, 128)]
            vfull = w_in.rearrange("(k p) n -> p k n", p=128)
            pf2 = []
            for (d0, s0, n) in pieces:
                for o in range(0, n, 256):
                    pf2.append((d0 + o, s0 + o, min(256, n - o)))

            def prefetch2():
                if not pf2:
                    return
                dd, ss, cwd = pf2.pop(0)
                stg, sr = stg_pool.next()
                dma("gpsimd", stg[:, :, :cwd], vfull[:, :, ss:ss + cwd], sr, writes=[sr])
                cp(Wrw[:, :, dd:dd + cwd], stg[:, :, :cwd], [sr], [Wrw_r])

            cst_r = S.res("ret_consts")
            protf = st.enter_context(nc.sbuf_tensor(U("protf"), [128, 128], F32))
            prot = st.enter_context(nc.sbuf_tensor(U("prot"), [128, 128], BF16))
            gC = st.enter_context(nc.sbuf_tensor(U("gC"), [128, 512], F32))
            for (dst, srcap) in [(protf[:], prot_d), (gC[:], gC_d)]:
                r_ = S.res("cst")
                dma("gpsimd", dst, srcap, r_, writes=[r_, cst_r])
            cp(prot[:], protf[:], [cst_r], [cst_r])
            zt, zt_r = st.enter_context(nc.sbuf_tensor(U("zt"), [128, D], F32)), S.res("zt")
            S.op("vector", lambda e: e.memset(zt[:], 0.0), writes=[zt_r])
            zf = list(range(NTILE))

            def zerofill():
                if zf:
                    i = zf.pop(0)
                    dma("gpsimd", ymoe_d[i * 128:(i + 1) * 128, :], zt[:], zt_r, reads=[zt_r])

            def ret_gen(dirn):
                csp = Pool(S, st, "cs", [128, 2, 128], F32, 2)
                hsp = Pool(S, st, "hsr", [128, 8, 128], BF16, 2)
                qdec = st.enter_context(nc.sbuf_tensor(U("qdec"), [128, 4, 128], F32))
                kdec = st.enter_context(nc.sbuf_tensor(U("kdec"), [128, 512], F32))
                dmask = st.enter_context(nc.sbuf_tensor(U("dmask"), [128, 8, 128], F32))
                dc_r = S.res("ret_dconsts")
                for (dst, srcap) in [(qdec[:].rearrange("p a t -> p (a t)"), qdec_d[dirn]), (kdec[:], kdec_d[dirn]),
                                     (dmask[:].rearrange("p a t -> p (a t)"), dmask_d[dirn])]:
                    r_ = S.res("cst")
                    dma("gpsimd", dst, srcap, r_, writes=[r_, dc_r])
                S32 = st.enter_context(nc.sbuf_tensor(U("S32"), [128, 512], F32))
                S16 = st.enter_context(nc.sbuf_tensor(U("S16"), [128, 512], BF16))
                S_r = S.res("S")
                S16_r = S.res("S16")
                S.op("vector", lambda e: e.memset(S32[:], 0.0), writes=[S_r])
                S.op("vector", lambda e: e.memset(S16[:], 0.0), writes=[S16_r])
                PA = Pool(S, st, "retPA", [128, 512], F32, 2, psum=True)
                PYp = Pool(S, st, "retPY", [128, 512], F32, 1, psum=True)
                qk_sb = Pool(S, st, "qk_sb", [128, 4, 128], BF16, 2)
                t1p = Pool(S, st, "t1", [128, 4, 128], F32, 1)
                t2p = Pool(S, st, "t2", [128, 4, 128], F32, 1)
                krp = Pool(S, st, "kr", [128, 3, 4, 128], BF16, 2)
                for (t_, r_) in krp.tiles:
                    S.op("gpsimd", lambda e, t_=t_: e.memset(t_[:], 0.0), writes=[r_])
                qrp = Pool(S, st, "qr", [128, 4, 128], BF16, 2)
                qpp = Pool(S, st, "qp", [128, 4, 128], BF16, 2)
                kptp = Pool(S, st, "kpt", [128, 512], BF16, 2)
                vsp = Pool(S, st, "vsb", [128, 512], BF16, 2)
                sTp = Pool(S, st, "sT", [128, 8, 128], BF16, 2)
                sqp2 = Pool(S, st, "sq2", [128, 8, 64], F32, 1)
                ynp = Pool(S, st, "yn", [128, 8, 64], F32, 1)
                sgp = Pool(S, st, "sg", [128, 512], F32, 2)
                latp = Pool(S, st, "lat", [128, 512], BF16, 2)
                st8 = Pool(S, st, "st8", [128, 3, 8], F32, 2)
                yield

                def proj_feat(wi, hs, hsr):
                    pt, pr = PA.next()
                    for p in range(4):
                        for kc in range(8):
                            mm(pt[:, p * 128:(p + 1) * 128], wsb[:, wi, kc, p * 128:(p + 1) * 128], hs[:, kc, :],
                               kc == 0, kc == 7, [wsb_r, hsr], [pr])
                    return pt, pr

                def proj_tok(wi, hs, hsr):
                    pt, pr = PA.next()
                    for kc in range(8):
                        mm(pt[:], hs[:, kc, :], wsb[:, wi, kc, :], kc == 0, kc == 7, [wsb_r, hsr], [pr])
                    return pt, pr

                def rope(wi, hs, hsr, outp, cs, csr):
                    pt, pr = proj_feat(wi, hs, hsr)
                    sb, sbr = qk_sb.next()
                    act(sb[:].rearrange("p a t -> p (a t)"), pt[:], AF.Copy, [pr], [sbr])
                    yield
                    rt_, rr = PA.next()
                    mm(rt_[:], prot[:], sb[:].rearrange("p a t -> p (a t)"), True, True, [cst_r, sbr], [rr])
                    t1, t1r = t1p.next()
                    t2, t2r = t2p.next()
                    cosb = cs[:, 0:1, :].to_broadcast([128, 4, 128])
                    sinb = cs[:, 1:2, :].to_broadcast([128, 4, 128])
                    tt(t1[:], sb[:], cosb, ALU.mult, [sbr, csr], [t1r])
                    yield
                    tt(t2[:], rt_[:].rearrange("p (a t) -> p a t", a=4), sinb, ALU.mult, [rr, csr], [t2r])
                    o, orr = outp.next()
                    if wi == 1:
                        tt(o[:, 0], t1[:], t2[:], ALU.add, [t1r, t2r], [orr])
                        act(o[0:64, 1], o[0:64, 0], AF.Copy, [], [orr])
                        act(o[64:128, 2], o[64:128, 0], AF.Copy, [], [orr])
                    else:
                        tt(o[:], t1[:], t2[:], ALU.add, [t1r, t2r], [orr])
                    yield
                    return o, orr

                PKV = Pool(S, st, "retPKV", [128, 512], F32, 1, psum=True)

                def stage_a(is_ctx, ci):
                    c0 = (CTX0 if is_ctx else LAT0) + ci * 128
                    hs, hsr = hsp.next()
                    dma("sync", hs[:], hT_d[:, :, c0:c0 + 128], hsr, writes=[hsr])
                    if dirn == 0:
                        zerofill()
                    else:
                        prefetch2()
                    cs, csr = csp.next()
                    dma("gpsimd", cs[:, 0, :], cosT_d[:, c0:c0 + 128], csr, writes=[csr])
                    dma("gpsimd", cs[:, 1, :], sinT_d[:, c0:c0 + 128], csr, writes=[csr])
                    kr, krr = yield from rope(1, hs, hsr, krp, cs, csr)
                    pk, pkr = PA.next()
                    pkb = pk.bitcast(BF16)
                    for p in range(4):
                        tr(pkb[:, p * 128:(p + 1) * 128], kr[:, 0, p, :], identb[:], [krr, ident_r], [pkr])
                    kpt, kptr = kptp.next()
                    tt(kpt[:], pkb[:, 0:512], kdec[:], ALU.mult, [pkr, dc_r], [kptr])
                    yield
                    pv, pvr = proj_tok(2, hs, hsr)
                    vs, vsr = vsp.next()
                    act(vs[:], pv[:], AF.Copy, [pvr], [vsr])
                    yield
                    ctx_ = dict(is_ctx=is_ctx, ci=ci, kpt=kpt, kptr=kptr, vs=vs, vsr=vsr)
                    if not is_ctx:
                        qr, qrr = yield from rope(0, hs, hsr, qrp, cs, csr)
                        qp, qpr = qpp.next()
                        tt(qp[:], qr[:], qdec[:], ALU.mult, [qrr, dc_r], [qpr])
                        sT, sTr = sTp.next()
                        for b_ in range(2):
                            ps, psr = PA.next()
                            for hh in range(4):
                                h = 4 * b_ + hh
                                p, hf = h // 2, h % 2
                                mm(ps[:, hh * 128:(hh + 1) * 128], kr[:, 1 + hf, p, :], qr[:, p, :], True, True,
                                   [krr, qrr], [psr])
                            tt(sT[:, 4 * b_:4 * b_ + 4, :], ps[:].rearrange("p (a t) -> p a t", a=4),
                               dmask[:, 4 * b_:4 * b_ + 4, :], ALU.mult, [psr, dc_r], [sTr])
                            yield
                        pg, pgr = proj_tok(3 + dirn, hs, hsr)
                        sg, sgr = sgp.next()
                        act(sg[:], pg[:], AF.Silu, [pgr], [sgr])
                        yield
                        ctx_.update(qp=qp, qpr=qpr, sT=sT, sTr=sTr, sg=sg, sgr=sgr)
                    return ctx_

                def stage_b(cx):
                    kpt, kptr, vs, vsr = cx["kpt"], cx["kptr"], cx["vs"], cx["vsr"]
                    if not cx["is_ctx"]:
                        qp, qpr, sT, sTr, sg, sgr, ci = cx["qp"], cx["qpr"], cx["sT"], cx["sTr"], cx["sg"], cx["sgr"], cx["ci"]
                        py, pyr = PYp.next()
                        for p in range(4):
                            mm(py[:, p * 128:(p + 1) * 128], qp[:, p, :], S16[:, p * 128:(p + 1) * 128], True, False,
                               [qpr, S16_r], [pyr])
                            for hf in range(2):
                                h = 2 * p + hf
                                mm(py[:, h * 64:(h + 1) * 64], sT[:, h, :], vs[:, h * 64:(h + 1) * 64], False, hf == 1,
                                   [sTr, vsr], [pyr])
                        yield
                    kv, kvr = PKV.next()
                    for p in range(4):
                        mm(kv[:, p * 128:(p + 1) * 128], kpt[:, p * 128:(p + 1) * 128], vs[:, p * 128:(p + 1) * 128],
                           True, True, [kptr, vsr], [kvr])
                    tt(S32[:], kv[:], S32[:], ALU.add, [kvr], [S_r])
                    tt(S32[:], S32[:], gC[:], ALU.mult, [cst_r], [S_r])
                    act(S16[:], S32[:], AF.Copy, [S_r], [S16_r])
                    yield
                    if not cx["is_ctx"]:
                        sq, sqr = sqp2.next()
                        py3 = py[:].rearrange("p (h e) -> p h e", h=8)
                        act(sq[:], py3, AF.Square, [pyr], [sqr])
                        yield
                        s8, s8r = st8.next()
                        red(s8[:, 0, :], sq[:], [sqr], [s8r])
                        act(s8[:, 1, :], s8[:, 0, :], AF.Sqrt, [s8r, epsc_r], [s8r], bias=epsc[:], scale=1.0 / 64)
                        rcp(s8[:, 2, :], s8[:, 1, :], [s8r], [s8r])
                        yield
                        yn, ynr = ynp.next()
                        tt(yn[:], py3, s8[:, 2, :].unsqueeze(2).to_broadcast([128, 8, 64]), ALU.mult, [pyr, s8r], [ynr])
                        lt, ltr = latp.next()
                        tt(lt[:], yn[:].rearrange("p h e -> p (h e)"), sg[:], ALU.mult, [ynr, sgr], [ltr])
                        dma("sync", lat_d[dirn][ci * 128:(ci + 1) * 128, 0:512], lt[:], ltr, reads=[ltr])
                        yield

                if dirn == 0:
                    order = [(True, 0), (True, 1)] + [(False, i) for i in range(SEQ // 128)]
                else:
                    order = [(True, 1), (True, 0)] + [(False, i) for i in reversed(range(SEQ // 128))]
                order = order[:KLIM]
                cx = yield from stage_a(*order[0])
                for k in range(len(order)):
                    gA = stage_a(*order[k + 1]) if k + 1 < len(order) else None
                    gB = stage_b(cx)
                    nxt = None
                    while gA is not None or gB is not None:
                        if gB is not None:
                            try:
                                next(gB)
                            except StopIteration:
                                gB = None
                        if gA is not None:
                            try:
                                next(gA)
                            except StopIteration as e_:
                                nxt = e_.value
                                gA = None
                        yield
                    cx = nxt

            run_interleaved([ret_gen(0), ret_gen(1)], skew=KSKEW_RET)
            while pf2:
                prefetch2()
            while zf:
                zerofill()
            S.flush()

    if KLIM >= 0:
        ret_all()
    ret_wstack.close()


    cw_d = din("cwT", [2, 128, 14 * 3])
    w2pad_d = din("w2pad", [2, 128, 512])
    a2pad_d = din("a2pad", [128, 512])
    g2_d = din("g2", [2, 128, 512])
    rwcols_d = din("rwcols", [128, 4 * 8])
    lnx_d = din("lnx", [2, 512])
    tri_d = din("tri", [4, 128, 128])
    bdm_d = din("bdmask", [128, 512])
    rmask_d = din("rmask", [128, 512])
    ind2_d = din("ind2", [128, 2])
    bones_d = din("bones", [128, 128])
    lvm_d = din("lvlmask", [14, 128, 128], U32)
    CDEC = float(np.exp(-0.5))
    GN_EPS = 64e-5

    def rwkv_all():
        with ExitStack() as st:
            cst_r = S.res("rw_consts")

            def cload(name, shape, dt, src_ap, cast_from=None):
                t = st.enter_context(nc.sbuf_tensor(U(name), shape, dt))
                r_ = S.res(name)
                if cast_from is None:
                    dma("gpsimd", t[:], src_ap, r_, writes=[r_, cst_r])
                    return t
                with ExitStack() as stc:
                    tf = st.enter_context(nc.sbuf_tensor(U(name + "f"), shape, cast_from))
                    dma("gpsimd", tf[:], src_ap, r_, writes=[r_])
                    cp(t[:], tf[:], [r_], [cst_r])
                return t

            cwl = [cload("cw%d" % d_, [128, 14 * 3], F32, cw_d[d_]) for d_ in range(2)]
            a2p = cload("a2p", [128, 512], BF16, a2pad_d, F32)
            w2pl = [cload("w2p%d" % d_, [128, 512], BF16, w2pad_d[d_], F32) for d_ in range(2)]
            g2sl = [cload("g2s%d" % d_, [128, 512], BF16, g2_d[d_], F32) for d_ in range(2)]
            rwc = cload("rwc", [128, 4, 8], F32, rwcols_d.rearrange("p (a b) -> p a b", a=4))
            lnxw = cload("lnxw", [128, 512], F32, bcast_rows(lnx_d[0], 512))
            lnxb = cload("lnxb", [128, 512], F32, bcast_rows(lnx_d[1], 512))
            tril = [cload("tri%d" % i_, [128, 128], BF16, tri_d[i_], F32) for i_ in range(4)]
            bdm = cload("bdm", [128, 4, 128], F32, bdm_d.rearrange("p (a b) -> p a b", a=4))
            rmask = cload("rmask", [128, 512], F32, rmask_d)
            ind2 = cload("ind2", [128, 2], BF16, ind2_d, F32)
            bones = cload("bones", [128, 128], BF16, bones_d, F32)
            lvm = cload("lvm", [128, 14, 128], U32, lvm_d.rearrange("a p t -> p a t"))
            tinyc = st.enter_context(nc.sbuf_tensor(U("tinyc"), [128, 2], F32))
            S.op("vector", lambda e: e.memset(tinyc[:, 0:1], 1e-24), writes=[cst_r])
            S.op("vector", lambda e: e.memset(tinyc[:, 1:2], GN_EPS), writes=[cst_r])
            omka = st.enter_context(nc.sbuf_tensor(U("omka"), [128, 4, 1], F32))
            ts(omka[:], rwc[:, :, 4:5], -1.0, 1.0, ALU.mult, ALU.add, [cst_r], [cst_r])
            S.flush()
            print("RWKV sbuf remaining after shared", nc.sbuf_bytes_remaining)

            def rwkv_gen(dirn):
                cw, w2p, g2s = cwl[dirn], w2pl[dirn], g2sl[dirn]
                sk = 0 if dirn == 0 else 2
                m_strict, m_incl, m_strictT = tril[sk], tril[sk + 1], tril[2 - sk]
                w0col = lambda blk: rwc[:, blk, dirn:dirn + 1]
                a0col = lambda blk: rwc[:, blk, 2:3]

                def wcols(blk):
                    if blk < 12:
                        return slice(blk * 128, (blk + 1) * 128)
                    o_ = 1536 + 256 * dirn + (blk - 12) * 128
                    return slice(o_, o_ + 128)

                def T(name, shape, dt):
                    return st.enter_context(nc.sbuf_tensor(U(name), shape, dt)), S.res(name)

                hsp = Pool(S, st, "hsw", [128, 8, 130], BF16, 2)
                H32, H32_r = T("H32", [128, 4, 128], F32)
                H16, H16_r = T("H16", [128, 4, 128], BF16)
                S.op("vector", lambda e: e.memset(H32[:], 0.0), writes=[H32_r])
                rw, rw_r = T("rw", [128, 14, 128], F32)
                lr, lr_r = T("lr", [128, 128], BF16)
                sgl, sgl_r = T("sgl", [128, 128], BF16)
                sig, sig_r = T("sig", [128, 4, 128], F32)
                icl, icl_r = T("icl", [128, 4, 128], F32)
                cum, cum_r = T("cum", [128, 4, 128], F32)
                pex, pex_r = T("pex", [128, 4, 128], F32)
                Er, Er_r = T("Er", [128, 512], F32)
                Ea, Ea_r = T("Ea", [128, 512], F32)
                Ek, Ek_r = T("Ek", [128, 512], F32)
                v3 = lambda t_: t_[:].rearrange("p (a t) -> p a t", a=4)
                v8 = lambda t_: t_[:].rearrange("p (h e) -> p h e", h=8)
                WC, WC_r = T("WC", [128, 4, 1], F32)
                kk, kk_r = sig, sig_r
                rn, rn_r = cum, cum_r
                t1, t1_r = pex, pex_r
                htmp, htmp_r = sig, sig_r
                kk2, kk2_r = T("kk2", [128, 4, 128], BF16)
                rkr, rkr_r = T("rkr", [128, 4, 128], BF16)
                rt, rt_r = T("rt", [128, 3, 4, 128], BF16)
                at, at_r = T("at", [128, 3, 4, 128], BF16)
                S.op("gpsimd", lambda e: e.memset(rt[:], 0.0), writes=[rt_r])
                S.op("gpsimd", lambda e: e.memset(at[:], 0.0), writes=[at_r])
                kt, kt_r = T("kt", [128, 4, 128], BF16)
                bt, bt_r = T("bt", [128, 4, 128], BF16)
                vbf, vbf_r = T("vbf", [128, 4, 128], BF16)
                vtok, vtok_r = T("vtok", [128, 512], BF16)
                ktok, ktok_r = T("ktok", [128, 512], BF16)
                btok, btok_r = T("btok", [128, 512], BF16)

                def T2g(name):
                    t_ = st.enter_context(nc.sbuf_tensor(U(name), [128, 8, 128], BF16))
                    return t_, [S.res(name + "0"), S.res(name + "1")]

                N0, N0g = T2g("N0")
                M0, M0g = T2g("M0")
                AakT, AakTg = T2g("AakT")
                ArbT, ArbTg = T2g("ArbT")
                ArkT, ArkTg = T2g("ArkT")
                Pd, Pdg = T2g("Pd")
                PdT, PdTg = T2g("PdT")
                T1s, T1sg = T2g("T1s")
                T2s, T2sg = T2g("T2s")
                rhs_sb, rhs_r = T("rhs_sb", [128, 512], BF16)
                u_sb, u_r = T("u_sb", [128, 512], BF16)
                s8, s8_r = T("s8", [128, 6, 8], F32)
                latp = Pool(S, st, "latr", [128, 512], BF16, 2)
                P1 = Pool(S, st, "P1", [128, 512], F32, 2, psum=True)
                P2 = Pool(S, st, "P2", [128, 4, 128], F32, 2, psum=True)
                print("RWKV sbuf remaining after gen alloc", dirn, nc.sbuf_bytes_remaining)
                yield

                if dirn == 0:
                    order = [(True, 0), (True, 1)] + [(False, i) for i in range(SEQ // 128)]
                else:
                    order = [(True, 1), (True, 0)] + [(False, i) for i in reversed(range(SEQ // 128))]
                ident_b4 = identb[:].unsqueeze(1).to_broadcast([128, 4, 128])
                fM, fN = (0, 1) if dirn == 0 else (1, 0)
                G = lambda t_, g: t_[:, 4 * g:4 * g + 4, :]
                def stage1(is_ctx, ci):
                    c0 = (CTX0 if is_ctx else LAT0) + ci * 128
                    hs, hsr = hsp.next()
                    dma("sync", hs[:], hT_d[:, :, c0 - 1:c0 + 129], hsr, writes=[hsr])
                    for blk in range(14):
                        if is_ctx and blk == 13:
                            continue
                        pt, pr = P1.next()
                        for kc in range(8):
                            mm(pt[:, 0:130], Wrw[:, kc, wcols(blk)], hs[:, kc, :], kc == 0, kc == 7, [Wrw_r, hsr], [pr])
                        act(rw[:, blk, :], pt[:, 1:129], AF.Copy, [pr, cst_r], [rw_r],
                            scale=cw[:, 3 * blk + 1:3 * blk + 2])
                        stt(rw[:, blk, :], pt[:, 0:128], cw[:, 3 * blk:3 * blk + 1], rw[:, blk, :], ALU.mult, ALU.add,
                            [pr, cst_r], [rw_r])
                        stt(rw[:, blk, :], pt[:, 2:130], cw[:, 3 * blk + 2:3 * blk + 3], rw[:, blk, :], ALU.mult,
                            ALU.add, [pr, cst_r], [rw_r])
                        if blk % 2 == 1:
                            yield

                order = order[:KLIM]
                yield from stage1(*order[0])
                for k_, (is_ctx, ci) in enumerate(order):
                    gS1 = [None]

                    def step_s1():
                        if gS1[0] is not None:
                            try:
                                next(gS1[0])
                            except StopIteration:
                                gS1[0] = None
                    rr_, kk_, vv_ = rw[:, 0:4, :], rw[:, 4:8, :], rw[:, 8:12, :]
                    act(lr[0:64, :], rw[0:64, 12, :], AF.Tanh, [rw_r], [lr_r])
                    act(lr[64:128, :], rw[64:128, 12, :], AF.Copy, [rw_r], [lr_r])
                    if not is_ctx:
                        act(sgl[:], rw[:, 13, :], AF.Sigmoid, [rw_r], [sgl_r])
                    act(vbf[:], vv_, AF.Copy, [rw_r], [vbf_r])
                    pz, pzr = P1.next()
                    for blk in range(4):
                        mm(pz[:, blk * 128:(blk + 1) * 128], w2p[:, blk * 128:(blk + 1) * 128], lr[:], True, True,
                           [cst_r, lr_r], [pzr])
                    for blk in range(4):
                        act(sig[:, blk, :], pz[:, blk * 128:(blk + 1) * 128], AF.Sigmoid, [pzr, cst_r], [sig_r],
                            bias=w0col(blk))
                    yield
                    pi_, pir = P1.next()
                    for blk in range(4):
                        mm(pi_[:, blk * 128:(blk + 1) * 128], a2p[:, blk * 128:(blk + 1) * 128], lr[:], True, True,
                           [cst_r, lr_r], [pir])
                    for blk in range(4):
                        act(icl[:, blk, :], pi_[:, blk * 128:(blk + 1) * 128], AF.Sigmoid, [pir, cst_r], [icl_r],
                            bias=a0col(blk))
                    flat = lambda t_: t_[:].rearrange("p a t -> p (a t)")
                    S.op("vector", lambda e: e.tensor_tensor_scan(out=flat(cum), data0=rmask[:], data1=flat(sig),
                                                                  initial=0.0, op0=ALU.mult, op1=ALU.add),
                         reads=[cst_r, sig_r], writes=[cum_r])
                    yield
                    tt(pex[:], cum[:], sig[:], ALU.subtract, [cum_r, sig_r], [pex_r])
                    if dirn == 0:
                        act(v3(Er), cum[:], AF.Exp, [cum_r], [Er_r], scale=-CDEC)
                        act(v3(Ea), pex[:], AF.Exp, [pex_r], [Ea_r], scale=-CDEC)
                        act(v3(Ek), cum[:], AF.Exp, [cum_r], [Ek_r], scale=CDEC)
                    else:
                        act(v3(Er), pex[:], AF.Exp, [pex_r], [Er_r], scale=CDEC)
                        act(v3(Ea), cum[:], AF.Exp, [cum_r], [Ea_r], scale=CDEC)
                        act(v3(Ek), pex[:], AF.Exp, [pex_r], [Ek_r], scale=-CDEC)
                    act(WC[:], cum[:, :, 127:128], AF.Exp, [cum_r], [WC_r], scale=-CDEC)
                    yield
                    tt(kk[:], kk_, rwc[:, :, 3:4].to_broadcast([128, 4, 128]), ALU.mult, [rw_r, cst_r], [kk_r])
                    act(kk2[:], kk[:], AF.Square, [kk_r], [kk2_r])
                    pss, pssr = P1.next()
                    for blk in range(4):
                        mm(pss[:, blk * 128:(blk + 1) * 128], bones[:], kk2[:, blk, :], True, True, [cst_r, kk2_r], [pssr])
                    act(rn[:].rearrange("p a t -> p (a t)"), pss[:], AF.Ln, [pssr, cst_r], [rn_r], bias=tinyc[:, 0:1])
                    act(rn[:], rn[:], AF.Exp, [], [rn_r], scale=-0.5)
                    yield
                    tt(kk[:], kk[:], rn[:], ALU.mult, [rn_r], [kk_r])
                    tt(t1[:], icl[:], rwc[:, :, 4:5].to_broadcast([128, 4, 128]), ALU.mult, [icl_r, cst_r], [t1_r])
                    tt(t1[:], t1[:], omka[:].to_broadcast([128, 4, 128]), ALU.add, [cst_r], [t1_r])
                    tt(t1[:], kk_, t1[:], ALU.mult, [rw_r], [t1_r])
                    tt(icl[:], kk[:], icl[:], ALU.mult, [kk_r], [icl_r])
                    yield
                    if not is_ctx:
                        tt(rn[:], rr_, rwc[:, :, 5:6].to_broadcast([128, 4, 128]), ALU.mult, [rw_r, cst_r], [rn_r])
                        tt(rkr[:], rn[:], t1[:], ALU.mult, [rn_r, t1_r], [rkr_r])
                        tt(rt[:, 0], rr_, v3(Er), ALU.mult, [rw_r, Er_r], [rt_r])
                        act(rt[0:64, 1], rt[0:64, 0], AF.Copy, [], [rt_r])
                        act(rt[64:128, 2], rt[64:128, 0], AF.Copy, [], [rt_r])
                    stt(at[:, 0], kk[:], -1.0, v3(Ea), ALU.mult, ALU.mult, [kk_r, Ea_r], [at_r])
                    act(at[0:64, 1], at[0:64, 0], AF.Copy, [], [at_r])
                    act(at[64:128, 2], at[64:128, 0], AF.Copy, [], [at_r])
                    tt(kt[:], t1[:], v3(Ek), ALU.mult, [t1_r, Ek_r], [kt_r])
                    tt(bt[:], icl[:], v3(Ek), ALU.mult, [icl_r, Ek_r], [bt_r])
                    yield
                    if k_ + 1 < len(order):
                        gS1[0] = stage1(*order[k_ + 1])
                    for (srcT, srcr, dst, dstr) in ((vbf, vbf_r, vtok, vtok_r), (kt, kt_r, ktok, ktok_r),
                                                    (bt, bt_r, btok, btok_r)):
                        pt, pr = P1.next()
                        ptb = pt.bitcast(BF16)
                        for blk in range(4):
                            tr(ptb[:, blk * 128:(blk + 1) * 128], srcT[:, blk, :], identb[:], [srcr, ident_r], [pr])
                        act(dst[:], ptb[:, 0:512], AF.Copy, [pr], [dstr])
                    step_s1()
                    yield

                    def scores(g, lhs, lhs_r, rhsm, rhsm_r, mask, dst, dst_rg, lhs_masked=False):
                        p2, p2r = P2.next()
                        for hh in range(4):
                            h = 4 * g + hh
                            p, hf = h // 2, h % 2
                            if lhs_masked:
                                mm(p2[:, hh, :], lhs[:, 1 + hf, p, :], rhsm[:, p, :], True, True, [lhs_r, rhsm_r], [p2r])
                            else:
                                mm(p2[:, hh, :], lhs[:, p, :], rhsm[:, 1 + hf, p, :], True, True, [lhs_r, rhsm_r], [p2r])
                        tt(G(dst, g), p2[:], mask[:].unsqueeze(1).to_broadcast([128, 4, 128]), ALU.mult,
                           [p2r, cst_r], [dst_rg[g]])

                    for g in range(2):
                        scores(g, bt, bt_r, at, at_r, m_strict, N0, N0g)
                        scores(g, at, at_r, bt, bt_r, m_strictT, M0, M0g, lhs_masked=True)
                        step_s1()
                        yield
                    for g in range(2):
                        scores(g, kt, kt_r, at, at_r, m_strict, AakT, AakTg)
                        if not is_ctx:
                            scores(g, bt, bt_r, rt, rt_r, m_incl, ArbT, ArbTg)
                            scores(g, kt, kt_r, rt, rt_r, m_incl, ArkT, ArkTg)
                        step_s1()
                        yield

                    def pred(dst, dst_rg, g, lvl, form, data_ap, data_r):
                        mk = lvm[:, form * 7 + lvl - 1, :].unsqueeze(1).to_broadcast([128, 4, 128])
                        S.op("vector", lambda e: e.copy_predicated(out=G(dst, g), mask=mk, data=data_ap),
                             reads=[cst_r, data_r], writes=[dst_rg[g]])

                    for g in range(2):
                        cp(G(Pd, g), ident_b4, [ident_r], [Pdg[g]])
                        cp(G(PdT, g), ident_b4, [ident_r], [PdTg[g]])
                        pred(Pd, Pdg, g, 1, fM, G(M0, g), M0g[g])
                        pred(PdT, PdTg, g, 1, fN, G(N0, g), N0g[g])
                    step_s1()
                    yield
                    for lvl in range(2, 8):
                        last = lvl == 7
                        for g in range(2):
                            if not last:
                                pT1, pT1r = P2.next()
                                for hh in range(4):
                                    h = 4 * g + hh
                                    mm(pT1[:, hh, :], N0[:, h, :], Pd[:, h, :], True, True, [N0g[g], Pdg[g]], [pT1r])
                                act(G(T1s, g), pT1[:], AF.Copy, [pT1r], [T1sg[g]])
                            pT2, pT2r = P2.next()
                            for hh in range(4):
                                h = 4 * g + hh
                                mm(pT2[:, hh, :], M0[:, h, :], PdT[:, h, :], True, True, [M0g[g], PdTg[g]], [pT2r])
                            act(G(T2s, g), pT2[:], AF.Copy, [pT2r], [T2sg[g]])
                            step_s1()
                            yield
                            if not last:
                                pX, pXr = P2.next()
                                for hh in range(4):
                                    h = 4 * g + hh
                                    mm(pX[:, hh, :], PdT[:, h, :], T1s[:, h, :], True, True, [PdTg[g], T1sg[g]], [pXr])
                            pXT, pXTr = P2.next()
                            for hh in range(4):
                                h = 4 * g + hh
                                mm(pXT[:, hh, :], Pd[:, h, :], T2s[:, h, :], True, True, [Pdg[g], T2sg[g]], [pXTr])
                            if not last:
                                pred(Pd, Pdg, g, lvl, fM, pX[:], pXr)
                            pred(PdT, PdTg, g, lvl, fN, pXT[:], pXTr)
                            step_s1()
                            yield
                    Q16, Q16g = PdT, PdTg
                    wcb = WC[:].to_broadcast([128, 4, 128])
                    if dirn == 1:
                        tt(H32[:], H32[:], wcb, ALU.mult, [WC_r], [H32_r])
                    act(H16[:], H32[:], AF.Copy, [H32_r], [H16_r])
                    pr_, prr = P1.next()
                    for p in range(4):
                        mm(pr_[:, p * 128:(p + 1) * 128], at[:, 0, p, :], H16[:, p, :], True, False, [at_r, H16_r], [prr])
                        for hf in range(2):
                            h = 2 * p + hf
                            mm(pr_[:, h * 64:(h + 1) * 64], AakT[:, h, :], vtok[:, h * 64:(h + 1) * 64], False, hf == 1,
                               [AakTg[h // 4], vtok_r], [prr])
                    act(rhs_sb[:], pr_[:], AF.Copy, [prr], [rhs_r])
                    step_s1()
                    yield
                    pu, pur = P1.next()
                    for h in range(8):
                        mm(pu[:, h * 64:(h + 1) * 64], Q16[:, h, :], rhs_sb[:, h * 64:(h + 1) * 64], True, True,
                           [Q16g[h // 4], rhs_r], [pur])
                    act(u_sb[:], pu[:], AF.Copy, [pur], [u_r])
                    step_s1()
                    yield
                    if not is_ctx:
                        py_, pyr = P2.next()
                        py = py_[:].rearrange("p a t -> p (a t)")
                        for p in range(4):
                            mm(py[:, p * 128:(p + 1) * 128], rt[:, 0, p, :], H16[:, p, :], True, False, [rt_r, H16_r],
                               [pyr])
                            for hf in range(2):
                                h = 2 * p + hf
                                mm(py[:, h * 64:(h + 1) * 64], ArbT[:, h, :], u_sb[:, h * 64:(h + 1) * 64], False, False,
                                   [ArbTg[h // 4], u_r], [pyr])
                                mm(py[:, h * 64:(h + 1) * 64], ArkT[:, h, :], vtok[:, h * 64:(h + 1) * 64], False,
                                   hf == 1, [ArkTg[h // 4], vtok_r], [pyr])
                    p2h, p2hr = P2.next()
                    for p in range(4):
                        mm(p2h[:, p, :], btok[:, p * 128:(p + 1) * 128], u_sb[:, p * 128:(p + 1) * 128], True, False,
                           [btok_r, u_r], [p2hr])
                        mm(p2h[:, p, :], ktok[:, p * 128:(p + 1) * 128], vtok[:, p * 128:(p + 1) * 128], False, True,
                           [ktok_r, vtok_r], [p2hr])
                    tt(htmp[:], p2h[:], bdm[:], ALU.mult, [p2hr, cst_r], [htmp_r])
                    tt(H32[:], htmp[:], H32[:], ALU.add, [htmp_r], [H32_r])
                    if dirn == 0:
                        tt(H32[:], H32[:], wcb, ALU.mult, [WC_r], [H32_r])
                    step_s1()
                    yield
                    if is_ctx:
                        while gS1[0] is not None:
                            step_s1()
                            yield
                        continue
                    py3 = py.rearrange("p (h e) -> p h e", h=8)
                    red(s8[:, 0, :], py3, [pyr], [s8_r])
                    act(v8(Er), py3, AF.Square, [pyr], [Er_r])
                    red(s8[:, 1, :], v8(Er), [Er_r], [s8_r])
                    ts(s8[:, 2, :], s8[:, 0, :], 1.0 / 64, None, ALU.mult, ALU.bypass, [], [s8_r])
                    tt(s8[:, 3, :], s8[:, 2, :], s8[:, 2, :], ALU.mult, [], [s8_r])
                    stt(s8[:, 4, :], s8[:, 1, :], 1.0 / 64, s8[:, 3, :], ALU.mult, ALU.subtract, [], [s8_r])
                    act(s8[:, 5, :], s8[:, 4, :], AF.Sqrt, [cst_r], [s8_r], bias=tinyc[:, 1:2])
                    rcp(s8[:, 5, :], s8[:, 5, :], [], [s8_r])
                    tt(v8(Ea), py3, s8[:, 2, :].unsqueeze(2).to_broadcast([128, 8, 64]), ALU.subtract, [pyr, s8_r],
                       [Ea_r])
                    step_s1()
                    yield
                    prk, prkr = P1.next()
                    for blk in range(4):
                        mm(prk[:, 2 * blk:2 * blk + 2], rkr[:, blk, :], ind2[:], True, True, [rkr_r, cst_r], [prkr])
                    pg, pgr = P1.next()
                    mm(pg[:], sgl[:], g2s[:], True, True, [sgl_r, cst_r], [pgr])
                    tt(v8(Ea), v8(Ea), s8[:, 5, :].unsqueeze(2).to_broadcast([128, 8, 64]), ALU.mult, [s8_r], [Ea_r])
                    tt(Ea[:], Ea[:], lnxw[:], ALU.mult, [cst_r], [Ea_r])
                    tt(Ea[:], Ea[:], lnxb[:], ALU.add, [cst_r], [Ea_r])
                    tt(v8(Ek), vtok[:].rearrange("p (h e) -> p h e", h=8),
                       prk[:, 0:8].unsqueeze(2).to_broadcast([128, 8, 64]), ALU.mult, [vtok_r, prkr], [Ek_r])
                    tt(Ea[:], Ea[:], Ek[:], ALU.add, [Ek_r], [Ea_r])
                    lt, ltr = latp.next()
                    tt(lt[:], Ea[:], pg[:], ALU.mult, [Ea_r, pgr], [ltr])
                    dma("sync", lat_d[dirn][ci * 128:(ci + 1) * 128, 512:1024], lt[:], ltr, reads=[ltr])
                    step_s1()
                    yield
                    while gS1[0] is not None:
                        step_s1()
                        yield

            run_interleaved([rwkv_gen(0), rwkv_gen(1)], skew=KSKEW_RW)
            S.flush()

    if KLIM >= 0 and KRW:
        rwkv_all()
    rw_wstack.close()


    w_out = din("w_out", [D, D])
    w_router = din("w_router", [D, 16])
    w_gate = din("w_gate", [16, D, D])
    w_up = din("w_up", [16, D, D])
    w_down = din("w_down", [16, D, D])
    tid_d = din("tidc", [128, 32 * 2])
    iota_d = din("iota512", [128, 512])
    rm16_d = din("rmask16", [128, 512])
    ones_d = din("onesc", [128, 128])


    with ExitStack() as st:
        wout = st.enter_context(nc.sbuf_tensor(U("wout"), [128, 8, D], BF16))
        wout_r = S.res("wout")
        with ExitStack() as st0:
            stg_pool = Pool(S, st0, "wstg4", [128, 8, 256], F32, 2)
            load_w_bf16(stg_pool, wout, wout_r, w_out, D)
            S.flush()
        c4 = S.res("c4")
        wr = st.enter_context(nc.sbuf_tensor(U("wr"), [128, 8, 16], F32))
        rows = st.enter_context(nc.sbuf_tensor(U("rows4"), [128, 3, D], F32))
        r_ = S.res("c4a")
        dma("gpsimd", wr[:], w_router.rearrange("(k p) e -> p k e", p=128), r_, writes=[r_, c4])
        r_ = S.res("c4b")
        dma("gpsimd", rows[:].rearrange("p a d -> p (a d)"), bcast_rows(rowmod_d, 3 * D), r_, writes=[r_, c4])
        def p4_gen(parity):
            lfp = Pool(S, st, "lf", [128, D], BF16, 2)
            lbp = Pool(S, st, "lb", [128, D], BF16, 2)
            ltp = Pool(S, st, "l4", [128, D], BF16, 1)
            lTp = Pool(S, st, "lT", [128, 8, 128], BF16, 1)
            xip = Pool(S, st, "xi4", [128, D], F32, 2)
            tmp_ = Pool(S, st, "tm4", [128, D], F32, 1)
            x1p = Pool(S, st, "x14", [128, D], F32, 1)
            h2p = Pool(S, st, "h24", [128, D], F32, 1)
            h2bp = Pool(S, st, "h2b4", [128, D], BF16, 1)
            h2Tp = Pool(S, st, "h2T4", [128, 8, 128], F32, 1)
            jkp = Pool(S, st, "jk4", [128, D], BF16, 1)
            stp4 = Pool(S, st, "st4", [128, 12], F32, 2)
            exp_ = Pool(S, st, "ex4", [128, 16], F32, 2)
            pbig = Pool(S, st, "pbig4", [128, D], F32, 1, psum=True)
            psml = Pool(S, st, "psml4", [128, 512], F32, 1, psum=True)
            yield
            for i in range(parity, NTILE if KLIM > 100 else min(NTILE, max(KLIM, 0)), 2):
                r0 = i * 128
                lf, lfr = lfp.next()
                lb, lbr = lbp.next()
                dma("gpsimd", lf[:], lat_d[0][r0:r0 + 128, :], lfr, writes=[lfr])
                dma("gpsimd", lb[:], lat_d[1][r0:r0 + 128, :], lbr, writes=[lbr])
                lt, ltr = ltp.next()
                tt(lt[:], lf[:], lb[:], ALU.add, [lfr, lbr], [ltr])
                pt_, ptr_ = psml.next()
                pt = pt_.bitcast(BF16)[:, 0:1024].rearrange("p (j t) -> p j t", j=8)
                for j in range(8):
                    tr(pt[:, j, :], lt[:, j * 128:(j + 1) * 128], identb[:], [ltr, ident_r], [ptr_])
                lT, lTr = lTp.next()
                act(lT[:], pt, AF.Copy, [ptr_], [lTr])
                yield
                pm, pmr = pbig.next()
                for dh in range(2):
                    for fc in range(8):
                        mm(pm[:, dh * 512:(dh + 1) * 512], lT[:, fc, :], wout[:, fc, dh * 512:(dh + 1) * 512],
                           fc == 0, fc == 7, [lTr, wout_r], [pmr])
                s4, s4r = stp4.next()
                jk, jkr = jkp.next()
                act(jk[:], pm[:], AF.Square, [pmr], [jkr, s4r], accum_out=s4[:, 0:1])
                act(s4[:, 1:2], s4[:, 0:1], AF.Sqrt, [s4r, epsc_r], [s4r], bias=epsc[:], scale=1.0 / D)
                rcp(s4[:, 2:3], s4[:, 1:2], [s4r], [s4r])
                yield
                tm, tmr = tmp_.next()
                tt(tm[:], pm[:], rows[:, 0, :], ALU.mult, [pmr, c4], [tmr])
                xi, xir = xip.next()
                dma("sync", xi[:], x[r0:r0 + 128, :], xir, writes=[xir])
                x1, x1r = x1p.next()
                stt(x1[:], tm[:], s4[:, 2:3], xi[:], ALU.mult, ALU.add, [tmr, s4r, xir], [x1r])
                dma("sync", x1_d[r0:r0 + 128, :], x1[:], x1r, reads=[x1r])
                yield
                act(jk[:], x1[:], AF.Square, [x1r], [jkr, s4r], accum_out=s4[:, 3:4])
                act(s4[:, 4:5], s4[:, 3:4], AF.Sqrt, [s4r, epsc_r], [s4r], bias=epsc[:], scale=1.0 / D)
                rcp(s4[:, 5:6], s4[:, 4:5], [s4r], [s4r])
                h2, h2r = h2p.next()
                stt(h2[:], x1[:], s4[:, 5:6], rows[:, 1, :], ALU.mult, ALU.mult, [x1r, s4r, c4], [h2r])
                tt(h2[:], h2[:], rows[:, 2, :], ALU.add, [c4], [h2r])
                h2b, h2br = h2bp.next()
                act(h2b[:], h2[:], AF.Copy, [h2r], [h2br])
                dma("sync", h2_d[r0:r0 + 128, :], h2b[:], h2br, reads=[h2br])
                yield
                p2t_, p2tr = pbig.next()
                p2t = p2t_[:].rearrange("p (j t) -> p j t", j=8)
                for j in range(8):
                    tr(p2t[:, j, :], h2[:, j * 128:(j + 1) * 128], ident[:], [h2r, ident_r], [p2tr])
                h2T, h2Tr = h2Tp.next()
                cp(h2T[:], p2t, [p2tr], [h2Tr])
                yield
                pl_, plr = psml.next()
                pl = pl_[:, 0:16]
                for kc in range(8):
                    mm(pl, h2T[:, kc, :], wr[:, kc, :], kc == 0, kc == 7, [h2Tr, c4], [plr])
                red(s4[:, 6:7], pl, [plr], [s4r], op=ALU.max)
                ts(s4[:, 7:8], s4[:, 6:7], -1.0, None, ALU.mult, ALU.bypass, [], [s4r])
                ex, exr = exp_.next()
                act(ex[:], pl, AF.Exp, [plr, s4r], [exr, s4r], bias=s4[:, 7:8], accum_out=s4[:, 8:9])
                rcp(s4[:, 9:10], s4[:, 8:9], [s4r], [s4r])
                ts(aff_all[:, i, :], ex[:], s4[:, 9:10], None, ALU.mult, ALU.bypass, [exr, s4r], [aff_r])
        run_interleaved([p4_gen(0), p4_gen(1)])
        if "aff" in debug:
            d_ = dbg_out("aff", [SEQ, 16], F32)
            dma("sync", d_.rearrange("(a p) e -> p a e", p=128), aff_all[:], aff_r, reads=[aff_r])
        S.flush()

    idxu = glob.enter_context(nc.sbuf_tensor(U("idxu"), [128, 16, 4], U32))
    gate = glob.enter_context(nc.sbuf_tensor(U("gate"), [128, 16, 4], F32))
    route_r = S.res("route")
    wstack = ExitStack()
    NEXP = 16 if KLIM > 100 else 0
    wbuf = [[(wstack.enter_context(nc.sbuf_tensor(U("wexp"), [128, 8, D], BF16)), S.res("wexp")) for _ in range(3)]
            for _ in range(2)]
    cast_engs = ["scalar", "vector", "scalar", "vector"]
    ncast = [0]

    def load_expert(e_):
        for wi, wsrc in enumerate((w_gate, w_up, w_down)):
            wt, wtr = wbuf[e_ % 2][wi]
            v = wsrc[e_].rearrange("(k p) n -> p k n", p=128)
            for kh in range(2):
                dma("gpsimd", wt[:, kh * 4:(kh + 1) * 4, :], v[:, kh * 4:(kh + 1) * 4, :], wtr, writes=[wtr])


    with ExitStack() as st:
        c5 = S.res("c5")

        def cl5(name, shape, dt, src_ap, cast_from=None):
            t = st.enter_context(nc.sbuf_tensor(U(name), shape, dt))
            r_ = S.res(name)
            if cast_from is None:
                dma("gpsimd", t[:], src_ap, r_, writes=[r_, c5])
                return t
            tf = st.enter_context(nc.sbuf_tensor(U(name + "f"), shape, cast_from))
            dma("gpsimd", tf[:], src_ap, r_, writes=[r_])
            cp(t[:], tf[:], [r_], [c5])
            return t

        onesb = cl5("onesb", [128, 128], BF16, ones_d, F32)
        sutb = cl5("sutb", [128, 128], BF16, tri_d[0], F32)
        iota = cl5("iota", [128, 512], F32, iota_d)
        rm16 = cl5("rm16", [128, 512], F32, rm16_d)
        tidc = cl5("tidc", [128, 32, 2], F32, tid_d.rearrange("p (a c) -> p a c", c=2))

        def T5(name, shape, dt):
            return st.enter_context(nc.sbuf_tensor(U(name), shape, dt)), S.res(name)

        lo, lo_r = T5("lo", [128, 16], F32)
        cand, cand_r = T5("cand", [128, 16], F32)
        cmpb, cmp_r = T5("cmpb", [128, 32, 16], BF16)
        cnt, cnt_r = T5("cnt", [128, 16], F32)
        incr, incr_r = T5("incr", [128, 16], F32)
        maskf, maskf_r = T5("maskf", [128, 32, 16], F32)
        cs5, cs5_r = T5("cs5", [128, 16, 32], F32)
        inc5, inc5_r = T5("inc5", [128, 16, 32], F32)
        pos, pos_r = T5("pos", [128, 32, 16], F32)
        pos2 = st.enter_context(nc.sbuf_tensor(U("pos2"), [128, 32, 16], F32))
        iotab = st.enter_context(nc.sbuf_tensor(U("iotab"), [128, 256], BF16))
        tv, tv_r = T5("tv", [128, 32, 16, 5], BF16)
        tmp5, tmp5_r = T5("tmp5", [128, 32, 16], F32)
        a1, a1_r = T5("a1", [128, 32, 16], F32)
        racc, racc_r = T5("racc", [128, 16, 4, 5], F32)
        idxf, idxf_r = T5("idxf", [128, 16, 4], F32)
        selp = Pool(S, st, "sel", [128, 512], BF16, 4)
        pcp = Pool(S, st, "pc5", [128, 512], F32, 2, psum=True)
        pap = Pool(S, st, "pa5", [128, 512], F32, 4, psum=True)
        S.op("vector", lambda e: e.memset(lo[:], 0.0), writes=[lo_r])
        for e0_ in range(min(2, NEXP)):
            load_expert(e0_)
        flat5 = lambda t_: t_[:].rearrange("p a e -> p (a e)")
        for it in range(27):
            cst = float(2.0 ** -(it + 1))
            ts(cand[:], lo[:], cst, None, ALU.add, ALU.bypass, [lo_r], [cand_r])
            tt(cmpb[:], aff_all[:], cand[:].unsqueeze(1).to_broadcast([128, 32, 16]), ALU.is_ge, [aff_r, cand_r],
               [cmp_r])
            pc, pcr = pcp.next()
            mm(pc[:], onesb[:], flat5(cmpb), True, True, [c5, cmp_r], [pcr])
            red(cnt[:], pc[:].rearrange("p (a e) -> p e a", e=16), [pcr], [cnt_r])
            ts(incr[:], cnt[:], 512.0, cst, ALU.is_ge, ALU.mult, [cnt_r], [incr_r])
            tt(lo[:], lo[:], incr[:], ALU.add, [incr_r], [lo_r])
        lob = lo[:].unsqueeze(1).to_broadcast([128, 32, 16])
        tt(cmpb[:], aff_all[:], lob, ALU.is_ge, [aff_r, lo_r], [cmp_r])
        tt(maskf[:], aff_all[:], lob, ALU.is_ge, [aff_r, lo_r], [maskf_r])
        pw, pwr = pcp.next()
        mm(pw[:], sutb[:], flat5(cmpb), True, True, [c5, cmp_r], [pwr])
        pcs, pcsr = pcp.next()
        mm(pcs[:], onesb[:], flat5(cmpb), True, True, [c5, cmp_r], [pcsr])
        cp(cs5[:], pcs[:].rearrange("p (a e) -> p e a", e=16), [pcsr], [cs5_r])
        S.op("vector", lambda e: e.tensor_tensor_scan(out=inc5[:].rearrange("p e a -> p (e a)"), data0=rm16[:],
                                                      data1=cs5[:].rearrange("p e a -> p (e a)"), initial=0.0,
                                                      op0=ALU.mult, op1=ALU.add),
             reads=[c5, cs5_r], writes=[inc5_r])
        tt(inc5[:], inc5[:], cs5[:], ALU.subtract, [cs5_r], [inc5_r])
        tt(pos[:], pw[:].rearrange("p (a e) -> p a e", e=16), inc5[:].rearrange("p e a -> p a e"), ALU.add,
           [pwr, inc5_r], [pos_r])
        ts(tmp5[:], maskf[:], -1.0e4, 1.0e4, ALU.mult, ALU.add, [maskf_r], [tmp5_r])
        tt(pos[:], pos[:], tmp5[:], ALU.add, [tmp5_r], [pos_r])
        ts(pos2[:], pos[:], -256.0, None, ALU.add, ALU.bypass, [pos_r], [pos_r])
        cp(iotab[:], iota[:, 0:256], [c5], [c5])
        for c_ in range(2):
            cp(tv[:, :, :, c_], tidc[:, :, c_:c_ + 1].to_broadcast([128, 32, 16]), [c5], [tv_r])
        cp(tv[:, :, :, 2], aff_all[:], [aff_r], [tv_r])
        tt(a1[:], aff_all[:], tv[:, :, :, 2], ALU.subtract, [aff_r, tv_r], [a1_r])
        cp(tv[:, :, :, 3], a1[:], [a1_r], [tv_r])
        tt(a1[:], a1[:], tv[:, :, :, 3], ALU.subtract, [tv_r], [a1_r])
        cp(tv[:, :, :, 4], a1[:], [a1_r], [tv_r])
        posA = st.enter_context(nc.sbuf_tensor(U("posA"), [128, 32, 16], BF16))
        posB = st.enter_context(nc.sbuf_tensor(U("posB"), [128, 32, 16], BF16))
        pk_r = S.res("poskeys")
        ts(tmp5[:], pos[:], 256.0, None, ALU.is_lt, ALU.bypass, [pos_r], [tmp5_r])
        stt(a1[:], pos[:], 1.0, tmp5[:], ALU.add, ALU.mult, [pos_r, tmp5_r], [a1_r])
        ts(posA[:], a1[:], -1.0, None, ALU.add, ALU.bypass, [a1_r], [pk_r])
        ts(tmp5[:], pos[:], 256.0, None, ALU.is_ge, ALU.bypass, [pos_r], [tmp5_r])
        ts(a1[:], pos[:], 512.0, None, ALU.is_lt, ALU.bypass, [pos_r], [a1_r])
        tt(tmp5[:], tmp5[:], a1[:], ALU.mult, [a1_r], [tmp5_r])
        stt(a1[:], pos[:], -255.0, tmp5[:], ALU.add, ALU.mult, [pos_r, tmp5_r], [a1_r])
        ts(posB[:], a1[:], -1.0, None, ALU.add, ALU.bypass, [a1_r], [pk_r])
        selAp = Pool(S, st, "selA", [128, 32, 256], BF16, 2)
        selBp = Pool(S, st, "selB", [128, 32, 256], BF16, 2)
        iob = iotab[:].unsqueeze(1).to_broadcast([128, 32, 256])
        for e_ in range(16 if KLIM > 100 else 0):
            pa0, par0 = pap.next()
            pa1, par1 = pap.next()
            pav = [pa0[:, 0:320].rearrange("p (j a c) -> p j a c", j=2, a=32),
                   pa1[:, 0:320].rearrange("p (j a c) -> p j a c", j=2, a=32)]
            parr = [par0, par1]
            sA, sAr = selAp.next()
            sB, sBr = selBp.next()
            tt(sA[:], iob, posA[:, :, e_:e_ + 1].to_broadcast([128, 32, 256]), ALU.is_equal, [c5, pk_r], [sAr])
            tt(sB[:], iob, posB[:, :, e_:e_ + 1].to_broadcast([128, 32, 256]), ALU.is_equal, [c5, pk_r], [sBr])
            for a_ in range(32):
                for j in range(4):
                    sel_, selr_ = (sA, sAr) if j < 2 else (sB, sBr)
                    mm(pav[j // 2][:, j % 2, a_, :], sel_[:, a_, (j % 2) * 128:(j % 2 + 1) * 128], tv[:, a_, e_, :],
                       True, True, [selr_, tv_r], [parr[j // 2]])
            for jj in range(2):
                red(racc[:, e_, 2 * jj:2 * jj + 2], pav[jj].rearrange("p j a c -> p j c a"), [parr[jj]], [racc_r])
        stt(idxf[:], racc[:, :, :, 0], 64.0, racc[:, :, :, 1], ALU.mult, ALU.add, [racc_r], [idxf_r])
        cp(idxu[:], idxf[:], [idxf_r], [route_r])
        tt(gate[:], racc[:, :, :, 2], racc[:, :, :, 3], ALU.add, [racc_r], [route_r])
        tt(gate[:], gate[:], racc[:, :, :, 4], ALU.add, [racc_r], [route_r])
        if "route" in debug:
            d_ = dbg_out("idx", [128, 64], U32)
            dma("sync", d_, idxu[:].rearrange("p e j -> p (e j)"), route_r, reads=[route_r])
            d2_ = dbg_out("gate", [128, 64], F32)
            r2_ = S.res("gdbg")
            dma("sync", d2_, gate[:].rearrange("p e j -> p (e j)"), r2_, reads=[route_r])
        S.flush()

    with ExitStack() as st:
        ym_r = S.res("ymoe")
        xsTp = Pool(S, st, "xsT", [128, 8, 512], BF16, 2)
        hidp = Pool(S, st, "hidT", [128, 8, 512], BF16, 1)
        silp = Pool(S, st, "sil", [128, 512], F32, 2)
        yep = Pool(S, st, "ye", [128, D], F32, 2)
        ptx = Pool(S, st, "ptx", [128, 8, 128], BF16, 2, psum=True)
        pgu = Pool(S, st, "pgu", [128, 512], F32, 4, psum=True)
        pdn = Pool(S, st, "pdn", [128, 512], F32, 2, psum=True)
        xs_slots = [(st.enter_context(nc.sbuf_tensor(U("xsg"), [128, 4, D], BF16)), [S.res("xsg") for _ in range(4)])
                    for _ in range(2)]

        def gather(e_):
            xs, xsrs = xs_slots[e_ % 2]
            for j in range(4):
                S.dma("gpsimd", lambda e, xs=xs, j=j, e_=e_: e.indirect_dma_start(
                    out=xs[:, j, :], out_offset=None, in_=h2_d,
                    in_offset=bass.IndirectOffsetOnAxis(ap=idxu[:, e_, j:j + 1], axis=0)),
                    xsrs[j], reads=[route_r], writes=[xsrs[j]])

        if NEXP:
            gather(0)
        for e_ in range(NEXP):
            if e_ + 1 < NEXP:
                if e_ + 1 >= 2:
                    load_expert(e_ + 1)
                gather(e_ + 1)
            (wg, wgr), (wu, wur), (wd, wdr) = wbuf[e_ % 2]
            xs, xsrs = xs_slots[e_ % 2]
            xsT, xsTr = xsTp.next()
            for j in range(4):
                pt, ptr_ = ptx.next()
                for kc in range(8):
                    tr(pt[:, kc, :], xs[:, j, kc * 128:(kc + 1) * 128], identb[:], [xsrs[j], ident_r], [ptr_])
                act(xsT[:, :, j * 128:(j + 1) * 128], pt[:], AF.Copy, [ptr_], [xsTr])
            hid, hidr = hidp.next()
            for fc in range(8):
                pg_, pgr_ = pgu.next()
                pu_, pur_ = pgu.next()
                for kc in range(8):
                    mm(pg_[:], wg[:, kc, fc * 128:(fc + 1) * 128], xsT[:, kc, :], kc == 0, kc == 7, [wgr, xsTr], [pgr_])
                for kc in range(8):
                    mm(pu_[:], wu[:, kc, fc * 128:(fc + 1) * 128], xsT[:, kc, :], kc == 0, kc == 7, [wur, xsTr], [pur_])
                sl, slr = silp.next()
                act(sl[:], pg_[:], AF.Silu, [pgr_], [slr])
                tt(hid[:, fc, :], pu_[:], sl[:], ALU.mult, [pur_, slr], [hidr])
            for j in range(4):
                ye, yer = yep.next()
                for dh in range(2):
                    pd_, pdr_ = pdn.next()
                    for fc in range(8):
                        mm(pd_[:], hid[:, fc, j * 128:(j + 1) * 128], wd[:, fc, dh * 512:(dh + 1) * 512], fc == 0, fc == 7,
                           [hidr, wdr], [pdr_])
                    if dh == 0:
                        act(ye[:, 0:512], pd_[:], AF.Copy, [pdr_, route_r], [yer], scale=gate[:, e_, j:j + 1])
                    else:
                        ts(ye[:, 512:1024], pd_[:], gate[:, e_, j:j + 1], None, ALU.mult, ALU.bypass, [pdr_, route_r],
                           [yer])
                S.dma("gpsimd", lambda e, ye=ye, j=j, e_=e_: e.indirect_dma_start(
                    out=ymoe_d, out_offset=bass.IndirectOffsetOnAxis(ap=idxu[:, e_, j:j + 1], axis=0),
                    in_=ye[:], in_offset=None, compute_op=ALU.add),
                    yer, reads=[yer, route_r], writes=[ym_r])
        S.flush()

    wstack.close()
    with ExitStack() as st:
        g2row = st.enter_context(nc.sbuf_tensor(U("g2row"), [128, D], F32))
        g2r = S.res("g2row")
        dma("gpsimd", g2row[:], bcast_rows(rowmod_d[3 * D:4 * D], D), g2r, writes=[g2r])
        x1p = Pool(S, st, "x17", [128, D], F32, 3)
        ymp = Pool(S, st, "ym7", [128, D], F32, 3)
        jk7 = Pool(S, st, "jk7", [128, D], BF16, 1)
        t7p = Pool(S, st, "t7", [128, D], F32, 2)
        o7p = Pool(S, st, "o7", [128, D], F32, 3)
        s7p = Pool(S, st, "s7", [128, 4], F32, 3)
        for i in range(NTILE):
            r0 = i * 128
            x1, x1r = x1p.next()
            ym, ymr = ymp.next()
            dma("sync", x1[:], x1_d[r0:r0 + 128, :], x1r, writes=[x1r])
            dma("gpsimd", ym[:], ymoe_d[r0:r0 + 128, :], ymr, writes=[ymr])
            s7, s7r = s7p.next()
            jk, jkr = jk7.next()
            act(jk[:], ym[:], AF.Square, [ymr], [jkr, s7r], accum_out=s7[:, 0:1])
            act(s7[:, 1:2], s7[:, 0:1], AF.Sqrt, [s7r, epsc_r], [s7r], bias=epsc[:], scale=1.0 / D)
            rcp(s7[:, 2:3], s7[:, 1:2], [s7r], [s7r])
            t7, t7r = t7p.next()
            tt(t7[:], ym[:], g2row[:], ALU.mult, [ymr, g2r], [t7r])
            o7, o7r = o7p.next()
            stt(o7[:], t7[:], s7[:, 2:3], x1[:], ALU.mult, ALU.add, [t7r, s7r, x1r], [o7r])
            dma("scalar" if i % 2 == 0 else "sync", out[r0:r0 + 128, :], o7[:], o7r, reads=[o7r])
        S.flush()

    glob.close()
    return nc, dbg


def make_consts():
    c = {}
    c["ident"] = np.eye(128, dtype=np.float32)
    prot = np.zeros((128, 128), np.float32)
    for o in (0, 64):
        for i in range(32):
            prot[o + i + 32, o + i] = -1.0
            prot[o + i, o + 32 + i] = 1.0
    c["prot"] = prot
    t = np.arange(SEQ)
    row = (t // 64).astype(np.float64)
    col = (t % 64).astype(np.float64)
    freq = 10000.0 ** (-np.arange(16, dtype=np.float64) / 16)
    ang = np.concatenate([row[:, None] * freq, col[:, None] * freq], axis=-1)
    cosT = np.ones((128, NT), np.float64)
    sinT = np.zeros((128, NT), np.float64)
    for p in range(128):
        f = p % 32
        cosT[p, LAT0:LAT0 + SEQ] = np.cos(ang[:, f].astype(np.float32).astype(np.float64))
        sinT[p, LAT0:LAT0 + SEQ] = np.sin(ang[:, f].astype(np.float32).astype(np.float64))
    c["cosT"] = cosT.astype(np.float32)
    c["sinT"] = sinT.astype(np.float32)
    lg = np.log1p(-np.exp2(-5.0 - np.arange(8, dtype=np.float64)))
    i = np.arange(128, dtype=np.float64)
    qdec = np.zeros((2, 128, 4, 128))
    kdec = np.zeros((2, 128, 8, 64))
    dmask = np.zeros((2, 128, 8, 128))
    gC = np.zeros((128, 4, 128))
    for h in range(8):
        p, hf = h // 2, h % 2
        qdec[0, hf * 64:(hf + 1) * 64, p, :] = np.exp(lg[h] * (i + 1))[None, :]
        qdec[1, hf * 64:(hf + 1) * 64, p, :] = np.exp(lg[h] * (128 - i))[None, :]
        kdec[0, :, h, :] = 0.125 * np.exp(-lg[h] * (i + 1))[:, None]
        kdec[1, :, h, :] = 0.125 * np.exp(-lg[h] * (128 - i))[:, None]
        jj, ii = np.meshgrid(i, i, indexing="ij")
        dmask[0, :, h, :] = 0.125 * np.where(ii >= jj, np.exp(lg[h] * np.maximum(ii - jj, 0)), 0.0)
        dmask[1, :, h, :] = 0.125 * np.where(jj >= ii, np.exp(lg[h] * np.maximum(jj - ii, 0)), 0.0)
        gC[hf * 64:(hf + 1) * 64, p, hf * 64:(hf + 1) * 64] = np.exp(lg[h] * 128)
    c["qdec"] = qdec.reshape(2, 128, 512).astype(np.float32)
    c["kdec"] = kdec.reshape(2, 128, 512).astype(np.float32)
    c["dmask"] = dmask.reshape(2, 128, 1024).astype(np.float32)
    c["gC"] = gC.reshape(128, 512).astype(np.float32)

    tri = np.zeros((4, 128, 128), np.float32)
    s_, t_ = np.meshgrid(np.arange(128), np.arange(128), indexing="ij")
    tri[0] = (s_ < t_); tri[1] = (s_ <= t_); tri[2] = (s_ > t_); tri[3] = (s_ >= t_)
    c["tri"] = tri
    bd = np.zeros((128, 4, 128), np.float32)
    bd[0:64, :, 0:64] = 1.0
    bd[64:128, :, 64:128] = 1.0
    c["bdmask"] = bd.reshape(128, 512)
    rm = np.ones((128, 4, 128), np.float32)
    rm[:, :, 0] = 0.0
    c["rmask"] = rm.reshape(128, 512)
    ind2 = np.zeros((128, 2), np.float32)
    ind2[0:64, 0] = 1.0
    ind2[64:128, 1] = 1.0
    c["ind2"] = ind2
    bo = np.zeros((128, 128), np.float32)
    bo[0:64, 0:64] = 1.0
    bo[64:128, 64:128] = 1.0
    c["bones"] = bo
    lv = np.zeros((14, 128, 128), np.uint32)
    for L in range(1, 8):
        B = 2 ** L
        hb = B // 2
        mk = (s_ // B == t_ // B) & (s_ % B >= hb) & (t_ % B < hb)
        lv[L - 1] = mk
        lv[7 + L - 1] = mk.T
    c["lvlmask"] = lv
    tid = np.zeros((128, 32, 2), np.float32)
    tok = np.arange(32)[None, :] * 128 + np.arange(128)[:, None]
    tid[:, :, 0] = tok // 64
    tid[:, :, 1] = tok % 64
    c["tidc"] = tid.reshape(128, 64)
    c["iota512"] = np.tile(np.arange(512, dtype=np.float32)[None, :], (128, 1))
    rm = np.ones((128, 16, 32), np.float32)
    rm[:, :, 0] = 0.0
    c["rmask16"] = rm.reshape(128, 512)
    c["onesc"] = np.ones((128, 128), np.float32)
    return c


def make_in_maps(inputs):
    consts = make_consts()
    maps = []
    for b in range(NCORES):
        m = {}
        m["x"] = np.ascontiguousarray(inputs["x"][b])
        m["ctx"] = np.ascontiguousarray(inputs["ctx"][b])
        cc = np.concatenate([inputs["c"][b].reshape(8, 128).T, inputs["c_ctx"].reshape(8, 128).T], axis=1)
        m["ccol"] = np.ascontiguousarray(cc.astype(np.float32))
        m["w_mod"] = np.ascontiguousarray(inputs["w_mod"][0])
        m["b_mod"] = np.ascontiguousarray(inputs["b_mod"][0])
        m["gains"] = np.ascontiguousarray(inputs["norm_gains"][0])
        m["w_in"] = np.ascontiguousarray(inputs["w_in"][0])
        RC_ = 2560
        conv = inputs["rwkv_conv"][0]
        cwT = np.zeros((2, 128, 14, 3), np.float32)
        for dr in range(2):
            cols = list(range(0, 1536)) + list(range(1536 + 64 * dr, 1600 + 64 * dr)) + list(range(1664, 1728)) + \
                list(range(1728 + 128 * dr, 1856 + 128 * dr))
            cwT[dr] = conv[:, cols].T.reshape(14, 128, 3).transpose(1, 0, 2)
        m["cwT"] = np.ascontiguousarray(cwT.reshape(2, 128, 42))
        w2pad = np.zeros((2, 128, 512), np.float32)
        w2pad[:, 0:64, :] = inputs["rwkv_w2"][0]
        m["w2pad"] = w2pad
        a2pad = np.zeros((128, 512), np.float32)
        a2pad[64:128, :] = inputs["rwkv_a2"][0]
        m["a2pad"] = a2pad
        m["g2"] = np.ascontiguousarray(inputs["rwkv_g2"][0])
        colv = lambda v: v.reshape(4, 128).T
        rwcols = np.zeros((128, 4, 8), np.float32)
        rwcols[:, :, 0] = colv(inputs["rwkv_w0"][0, 0])
        rwcols[:, :, 1] = colv(inputs["rwkv_w0"][0, 1])
        rwcols[:, :, 2] = colv(inputs["rwkv_a0"][0])
        rwcols[:, :, 3] = colv(inputs["rwkv_k_k"][0])
        rwcols[:, :, 4] = colv(inputs["rwkv_k_a"][0])
        rwcols[:, :, 5] = colv(inputs["rwkv_r_k"][0])
        m["rwcols"] = np.ascontiguousarray(rwcols.reshape(128, 32))
        m["w_out"] = np.ascontiguousarray(inputs["w_out"][0])
        m["w_router"] = np.ascontiguousarray(inputs["w_router"][0])
        m["w_gate"] = np.ascontiguousarray(inputs["w_gate"][0])
        m["w_up"] = np.ascontiguousarray(inputs["w_up"][0])
        m["w_down"] = np.ascontiguousarray(inputs["w_down"][0])
        m["lnx"] = np.ascontiguousarray(np.stack([inputs["rwkv_lnx_w"][0], inputs["rwkv_lnx_b"][0]], axis=0))
        m.update(consts)
        maps.append(m)
    return maps


def kernel(**inputs):
    inputs = {k: np.asarray(v) for k, v in inputs.items()}
    nc, _ = build()
    maps = make_in_maps(inputs)
    res = run_bass_kernel_spmd(nc, maps, core_ids=list(range(NCORES)))
    return np.stack([r["out"] for r in res.results], axis=0).astype(np.float32)
```

```python
import numpy as np
from contextlib import ExitStack
import concourse.bass as bass
import concourse.mybir as mybir
from concourse.bass_utils import run_bass_kernel_spmd

F32 = mybir.dt.float32
BF16 = mybir.dt.bfloat16
U32 = mybir.dt.uint32
I32 = mybir.dt.int32
AF = mybir.ActivationFunctionType
ALU = mybir.AluOpType
AX = mybir.AxisListType

D = 1024
SEQ = 4096
CTX = 256
NCORES = 8
CTX0 = 1
LAT0 = 259
NT = 4356
EPS = 1e-6
import os
KLIM = int(os.environ.get('KLIM', '999'))
KW = int(os.environ.get('KW', '4'))
KRW = int(os.environ.get('KRW', '1'))
KSKEW_RET = int(os.environ.get('KSKEW_RET', '0'))
KSKEW_RW = int(os.environ.get('KSKEW_RW', '0'))
KSTOP = float(os.environ.get('KSTOP', '99'))
KC = int(os.environ.get('KC', '9'))


_UID = [0]


def U(name):
    _UID[0] += 1
    return "%s_u%d" % (name, _UID[0])


class Res:
    __slots__ = ("name", "w", "rs", "dsem")

    def __init__(self, name=""):
        self.name = name
        self.w = None
        self.rs = {}
        self.dsem = None


class Sched:
    ENG = ["tensor", "vector", "scalar", "gpsimd", "sync"]

    def __init__(self, nc):
        self.nc = nc
        self.sem = {n: nc.alloc_semaphore("sem_" + n) for n in self.ENG}
        self.cnt = {n: 0 for n in self.ENG}
        self.waited = {n: {} for n in self.ENG}
        self.ops = {n: [] for n in self.ENG}
        self.pending = {n: {} for n in self.ENG}
        self.dfree = []
        self.dcnt = {}
        self.dres = []
        self.allres = []
        self.nwaits = 0
        self.nops = 0
        self.mute = False

    def stage(self, n):
        self.mute = n > KSTOP

    def res(self, name=""):
        r = Res(name)
        self.allres.append(r)
        return r

    def _need(self, eng, reads, writes):
        evs = []
        for r in reads:
            if r.w is not None:
                evs.append(r.w)
        for w in writes:
            if w.w is not None:
                evs.append(w.w)
            evs.extend(w.rs.values())
        need = {}
        own = self.sem[eng].num
        wd = self.waited[eng]
        for (s, v) in evs:
            if eng == "tensor" and s.num == own:
                continue
            if wd.get(s.num, 0) >= v:
                continue
            if s.num not in need or need[s.num][1] < v:
                need[s.num] = (s, v)
        for k, (s, v) in need.items():
            wd[k] = v
        self.nwaits += len(need)
        return list(need.values())

    def _mark(self, ev, reads, writes):
        for r in reads:
            if r in writes:
                continue
            k = ev[0].num
            if k not in r.rs or r.rs[k][1] < ev[1]:
                r.rs[k] = ev
        for w in writes:
            w.w = ev
            w.rs = {}

    def op(self, eng, fn, reads=(), writes=()):
        if self.mute:
            return
        need = self._need(eng, reads, writes)
        self.cnt[eng] += 1
        sem = self.sem[eng]
        ev = (sem, self.cnt[eng])
        self._mark(ev, reads, writes)
        self.nops += 1

        def emit(e, need=need, fn=fn, sem=sem):
            for (s, v) in need:
                e.wait_ge(s, v)
            fn(e).then_inc(sem, 1)

        self.ops[eng].append(emit)

    def dma(self, eng, fn, sres, reads=(), writes=()):
        if self.mute:
            return
        need = self._need(eng, reads, writes)
        if sres.dsem is None:
            if self.dfree:
                sres.dsem = self.dfree.pop()
            else:
                sres.dsem = self.nc.alloc_semaphore("dsem%d" % len(self.dcnt))
                self.dcnt[sres.dsem.num] = 0
            self.dres.append(sres)
        ds = sres.dsem
        self.dcnt[ds.num] += 16
        ev = (ds, self.dcnt[ds.num])
        self._mark(ev, reads, writes)
        self.pending[eng][ds.num] = ev
        self.nops += 1

        def emit(e, need=need, fn=fn, ds=ds):
            for (s, v) in need:
                e.wait_ge(s, v)
            fn(e).then_inc(ds, 16)

        self.ops[eng].append(emit)

    def flush(self):
        nc = self.nc
        self.mute = False
        for n in self.ENG:
            pend = list(self.pending[n].values())
            if pend:
                def emit(e, pend=pend):
                    for (s, v) in pend:
                        e.wait_ge(s, v)
                self.ops[n].append(emit)
            self.pending[n] = {}
        with nc.Block() as block:
            for n in self.ENG:
                ops = self.ops[n]
                if ops:
                    def body(e, ops=ops):
                        for o in ops:
                            o(e)
                    getattr(block, n)(body)
        self.ops = {n: [] for n in self.ENG}
        for r in self.dres:
            self.dfree.append(r.dsem)
            r.dsem = None
        self.dres = []
        for r in self.allres:
            r.w = None
            r.rs = {}
        self.allres = [r for r in self.allres]


class Pool:
    def __init__(self, S, stack, name, shape, dtype, n, psum=False):
        self.tiles = []
        for i in range(n):
            if psum:
                t = stack.enter_context(S.nc.psum_tensor(U("%s%d") % (name, i), shape, dtype))
            else:
                t = stack.enter_context(S.nc.sbuf_tensor(U("%s%d") % (name, i), shape, dtype))
            self.tiles.append((t, S.res("%s%d" % (name, i))))
        self.i = 0

    def next(self):
        t = self.tiles[self.i % len(self.tiles)]
        self.i += 1
        return t


def mk_helpers(S):
    def mm(out, lhsT, rhs, start, stop, reads, writes):
        S.op("tensor", lambda e: e.matmul(out, lhsT=lhsT, rhs=rhs, start=start, stop=stop), reads=reads, writes=writes)

    def tr(out, in_, ident, reads, writes):
        S.op("tensor", lambda e: e.transpose(out=out, in_=in_, identity=ident), reads=reads, writes=writes)

    def act(out, in_, func, reads, writes, **kw):
        S.op("scalar", lambda e: e.activation(out=out, in_=in_, func=func, **kw), reads=reads, writes=writes)

    def tt(out, in0, in1, op, reads, writes, eng="vector"):
        S.op(eng, lambda e: e.tensor_tensor(out=out, in0=in0, in1=in1, op=op), reads=reads, writes=writes)

    def ts(out, in0, s1, s2, op0, op1, reads, writes, eng="vector"):
        S.op(eng, lambda e: e.tensor_scalar(out=out, in0=in0, scalar1=s1, scalar2=s2, op0=op0, op1=op1),
             reads=reads, writes=writes)

    def stt(out, in0, scalar, in1, op0, op1, reads, writes):
        S.op("vector", lambda e: e.scalar_tensor_tensor(out=out, in0=in0, scalar=scalar, in1=in1, op0=op0, op1=op1),
             reads=reads, writes=writes)

    def cp(out, in_, reads, writes, eng="vector"):
        S.op(eng, lambda e: e.tensor_copy(out=out, in_=in_), reads=reads, writes=writes)

    def red(out, in_, reads, writes, op=ALU.add):
        S.op("vector", lambda e: e.tensor_reduce(out=out, in_=in_, axis=AX.X, op=op), reads=reads, writes=writes)

    def rcp(out, in_, reads, writes):
        S.op("vector", lambda e: e.reciprocal(out=out, in_=in_), reads=reads, writes=writes)

    def dma(eng, out, in_, sres, reads=(), writes=()):
        S.dma(eng, lambda e: e.dma_start(out=out, in_=in_), sres, reads=reads, writes=writes)

    return mm, tr, act, tt, ts, stt, cp, red, rcp, dma


def bcast_rows(dram_ap_1d, n, parts=128):
    return bass.AP(dram_ap_1d.tensor, dram_ap_1d.offset, [[0, parts], [1, n]])


def build(debug=()):
    nc = bass.Bass("TRN2", target_bir_lowering=False)
    S = Sched(nc)
    dbg = {}

    def din(name, shape, dt=F32):
        return nc.dram_tensor(name, list(shape), dt, kind="ExternalInput").ap()

    x = din("x", [SEQ, D])
    ctx = din("ctx", [CTX, D])
    ccol = din("ccol", [128, 16])
    w_mod = din("w_mod", [D, 6 * D])
    b_mod = din("b_mod", [6 * D])
    gains = din("gains", [4, D])
    ident_d = din("ident", [128, 128])
    out = nc.dram_tensor("out", [SEQ, D], F32, kind="ExternalOutput").ap()

    def dbg_out(name, shape, dt=F32):
        t = nc.dram_tensor("dbg_" + name, list(shape), dt, kind="ExternalOutput").ap()
        dbg[name] = t
        return t

    glob = ExitStack()
    ident = glob.enter_context(nc.sbuf_tensor(U("ident_sb"), [128, 128], F32))
    identb = glob.enter_context(nc.sbuf_tensor(U("identb_sb"), [128, 128], BF16))
    ident_r = S.res("ident")
    rowmod_d = nc.dram_tensor("rowmod_d", [4 * D], F32, kind="Internal").ap()
    colmod = glob.enter_context(nc.sbuf_tensor(U("colmod"), [128, 4, 8], F32))
    colmod_r = S.res("colmod")
    epsc = glob.enter_context(nc.sbuf_tensor(U("epsc"), [128, 1], F32))
    epsc_r = S.res("epsc")
    aff_all = glob.enter_context(nc.sbuf_tensor(U("aff_all"), [128, 32, 16], F32))
    aff_r = S.res("aff_all")
    if "hT" in debug:
        hT_d = dbg_out("hT", [128, 8, NT], BF16)
    else:
        hT_d = nc.dram_tensor("hT_scr", [128, 8, NT], BF16, kind="Internal").ap()

    def run_interleaved(gens, skew=0):
        gens = list(gens)
        next(gens[0])
        for g in gens[1:]:
            next(g)
        for _ in range(skew):
            try:
                next(gens[-1])
            except StopIteration:
                break
        while gens:
            alive = []
            for g in gens:
                try:
                    next(g)
                    alive.append(g)
                except StopIteration:
                    pass
            gens = alive

    with ExitStack() as st:
        rowmod = st.enter_context(nc.sbuf_tensor(U("rowmod"), [128, 4, D], F32))
        rowmod_r = S.res("rowmod")
        csb = st.enter_context(nc.sbuf_tensor(U("csb"), [128, 16], F32))
        csil = st.enter_context(nc.sbuf_tensor(U("csil"), [128, 16], F32))
        cbc = st.enter_context(nc.sbuf_tensor(U("cbc"), [128, 16, 128], F32))
        c_r = S.res("c")
        gb = st.enter_context(nc.sbuf_tensor(U("gb"), [128, 4, D], F32))
        gb_r = S.res("gb")
        bmb = st.enter_context(nc.sbuf_tensor(U("bmb"), [128, 6 * D], F32))
        bmb_r = S.res("bmb")
        rowc = st.enter_context(nc.sbuf_tensor(U("rowc"), [128, 6 * D], F32))
        rowx = st.enter_context(nc.sbuf_tensor(U("rowx"), [128, 2 * D], F32))
        row_r = S.res("row")
        gcol = st.enter_context(nc.sbuf_tensor(U("gcol"), [128, 8], F32))
        junk = st.enter_context(nc.sbuf_tensor(U("junk0"), [128, 128], F32))
        junk_r = S.res("junk0")
        wpool = Pool(S, st, "wmod", [128, 8, 512], F32, 2)
        pp = Pool(S, st, "ps0", [128, 512], F32, 2, psum=True)

        S.dma("sync", lambda e: e.dma_start(out=csb[:], in_=ccol), c_r, writes=[c_r])
        S.dma("sync", lambda e: e.dma_start(out=ident[:], in_=ident_d), ident_r, writes=[ident_r])
        S.op("vector", lambda e: e.tensor_copy(out=identb[:], in_=ident[:]), reads=[ident_r], writes=[ident_r])
        S.op("vector", lambda e: e.memset(epsc[:], EPS), writes=[epsc_r])
        S.dma("gpsimd", lambda e: e.dma_start(out=gb[:].rearrange("p a d -> p (a d)"),
                                               in_=bcast_rows(gains.rearrange("a d -> (a d)"), 4 * D)),
              gb_r, writes=[gb_r])
        S.dma("gpsimd", lambda e: e.dma_start(out=bmb[:], in_=bcast_rows(b_mod, 6 * D)), bmb_r, writes=[bmb_r])
        S.op("scalar", lambda e: e.activation(out=csil[:], in_=csb[:], func=AF.Silu), reads=[c_r], writes=[c_r])
        S.op("vector", lambda e: e.tensor_copy(out=cbc[:], in_=csil[:].unsqueeze(2).to_broadcast([128, 16, 128])),
             reads=[c_r], writes=[c_r])
        wv = w_mod.rearrange("(k p) n -> p k n", p=128)
        for blk in range(12):
            wt, wr = wpool.next()
            S.dma("sync" if blk % 2 == 0 else "gpsimd",
                  lambda e, wt=wt, blk=blk: e.dma_start(out=wt[:], in_=wv[:, :, blk * 512:(blk + 1) * 512]),
                  wr, writes=[wr])
            for which in range(2 if blk < 4 else 1):
                pt, pr = pp.next()
                for k in range(8):
                    S.op("tensor", lambda e, pt=pt, wt=wt, k=k, which=which: e.matmul(
                        pt[:], lhsT=cbc[:, which * 8 + k, :], rhs=wt[:, k, :], start=(k == 0), stop=(k == 7)),
                        reads=[c_r, wr], writes=[pr])
                dst = rowc if which == 0 else rowx
                S.op("vector", lambda e, pt=pt, dst=dst, blk=blk: e.tensor_tensor(
                    out=dst[:, blk * 512:(blk + 1) * 512], in0=pt[:], in1=bmb[:, blk * 512:(blk + 1) * 512],
                    op=ALU.add), reads=[pr, bmb_r], writes=[row_r])
        S.op("vector", lambda e: e.tensor_tensor(out=rowmod[:, 0, :], in0=rowc[:, 2 * D:3 * D], in1=gb[:, 1, :],
                                                 op=ALU.mult), reads=[row_r, gb_r], writes=[rowmod_r])
        S.op("vector", lambda e: e.scalar_tensor_tensor(out=rowmod[:, 1, :], in0=rowc[:, 4 * D:5 * D], scalar=1.0,
                                                        in1=gb[:, 2, :], op0=ALU.add, op1=ALU.mult),
             reads=[row_r, gb_r], writes=[rowmod_r])
        S.op("vector", lambda e: e.tensor_copy(out=rowmod[:, 2, :], in_=rowc[:, 3 * D:4 * D]),
             reads=[row_r], writes=[rowmod_r])
        S.op("vector", lambda e: e.tensor_tensor(out=rowmod[:, 3, :], in0=rowc[:, 5 * D:6 * D], in1=gb[:, 3, :],
                                                 op=ALU.mult), reads=[row_r, gb_r], writes=[rowmod_r])
        S.dma("sync", lambda e: e.dma_start(out=bass.AP(rowmod_d.tensor, 0, [[0, 1], [1, 4 * D]]),
                                            in_=rowmod[0:1].rearrange("p a d -> p (a d)")), rowmod_r, reads=[rowmod_r])
        for src in (rowc, rowx):
            S.op("vector", lambda e, src=src: e.scalar_tensor_tensor(
                out=src[:, D:2 * D], in0=src[:, D:2 * D], scalar=1.0, in1=gb[:, 0, :], op0=ALU.add, op1=ALU.mult),
                reads=[gb_r], writes=[row_r])
        for ci, (src, off) in enumerate([(rowc, D), (rowc, 0), (rowx, D), (rowx, 0)]):
            for j in range(8):
                S.op("vector", lambda e, src=src, off=off, j=j: e.tensor_tensor(
                    out=junk[:], in0=src[:, off + j * 128: off + (j + 1) * 128], in1=ident[:], op=ALU.mult),
                    reads=[row_r, ident_r], writes=[junk_r])
                S.op("vector", lambda e, j=j, ci=ci: e.tensor_reduce(
                    out=colmod[:, ci, j:j + 1], in_=junk[:], axis=AX.X, op=ALU.add),
                    reads=[junk_r], writes=[colmod_r])
        S.flush()

    mm, tr, act, tt, ts, stt, cp, red, rcp, dma = mk_helpers(S)
    w_in = din("w_in", [D, 4544])
    prot_d = din("prot", [128, 128])
    cosT_d = din("cosT", [128, NT])
    sinT_d = din("sinT", [128, NT])
    qdec_d = din("qdec", [2, 128, 512])
    kdec_d = din("kdec", [2, 128, 512])
    dmask_d = din("dmask", [2, 128, 1024])
    gC_d = din("gC", [128, 512])
    if "lat_f" in debug:
        lat_d = [dbg_out("lat_f", [SEQ, D], BF16), dbg_out("lat_b", [SEQ, D], BF16)]
    else:
        lat_d = [nc.dram_tensor("lat_f", [SEQ, D], BF16, kind="Internal").ap(),
                 nc.dram_tensor("lat_b", [SEQ, D], BF16, kind="Internal").ap()]

    def load_w_bf16(st_pool, dst, dst_r, src_ap, ncols):
        v = src_ap.rearrange("(k p) n -> p k n", p=128)
        for c0 in range(0, ncols, 256):
            cw = min(256, ncols - c0)
            stg, sr = st_pool.next()
            dma("sync", stg[:, :, :cw], v[:, :, c0:c0 + cw], sr, writes=[sr])
            act(dst[:, :, c0:c0 + cw], stg[:, :, :cw], AF.Copy, [sr], [dst_r])


    RC = 2560
    rw_wstack = ExitStack()
    Wrw = rw_wstack.enter_context(nc.sbuf_tensor(U("Wrw"), [128, 8, 1536 + 512], BF16))
    Wrw_r = S.res("Wrw")
    ret_wstack = ExitStack()
    wsb = ret_wstack.enter_context(nc.sbuf_tensor(U("ret_w"), [128, 5, 8, 512], BF16))
    wsb_r = S.res("ret_w")

    with ExitStack() as st:
        stg_pool1 = Pool(S, st, "wstg", [128, 8, 256], F32, 2)
        pf1 = []
        for wi, c0w in enumerate([0, 512, 1024, 1536, 2048]):
            for hh_ in range(2):
                pf1.append((wi, c0w, hh_))

        def prefetch1():
            if not pf1:
                return
            wi, c0w, hh_ = pf1.pop(0)
            stg, sr = stg_pool1.next()
            vv = w_in[:, c0w + hh_ * 256:c0w + hh_ * 256 + 256].rearrange("(k p) n -> p k n", p=128)
            dma("gpsimd", stg[:], vv, sr, writes=[sr])
            cp(wsb[:, wi, :, hh_ * 256:(hh_ + 1) * 256], stg[:], [sr], [wsb_r])
        zp = st.enter_context(nc.sbuf_tensor(U("zpad"), [128, 8, 2], BF16))
        zp_r = S.res("zpad")
        S.op("gpsimd", lambda e: e.memset(zp[:], 0.0), writes=[zp_r])
        with nc.allow_non_contiguous_dma(reason="tiny zero pad columns"):
            pass
        for col in (0, CTX0 + CTX, CTX0 + CTX + 1, NT - 1):
            S.dma("gpsimd", lambda e, col=col: e.dma_start(out=hT_d[:, :, col:col + 1], in_=zp[:, :, 0:1],
                                                           allow_slow_non_contiguous=True), zp_r, reads=[zp_r])
        tiles = [(ctx, i, CTX0 + i * 128, 2) for i in range(CTX // 128)] + \
                [(x, i, LAT0 + i * 128, 0) for i in range(SEQ // 128)]
        groups = [tiles[0:2]] + [tiles[2 + 4 * g_:2 + 4 * g_ + 4] for g_ in range(SEQ // 512)]

        def p1_gen(par):
            hTp = Pool(S, st, "hTt", [128, 8, 512], BF16, 2)
            xp = Pool(S, st, "xin", [128, D], F32, 2)
            xnp = Pool(S, st, "xn", [128, D], BF16, 1)
            sqp = Pool(S, st, "sqj", [128, D], BF16, 1)
            stp = Pool(S, st, "stat", [128, 4], F32, 2)
            tp = Pool(S, st, "pst", [128, 8, 128], BF16, 1, psum=True)
            yield
            for grp in groups[par::2]:
                for (src, i, c0, ci) in grp:
                    if ci == 0 and i >= 1:
                        prefetch1()
                    xt, xr = xp.next()
                    S.dma("sync", lambda e, xt=xt, src=src, i=i: e.dma_start(out=xt[:], in_=src[i * 128:(i + 1) * 128, :]),
                          xr, writes=[xr])
                    sq, sqr = sqp.next()
                    stq, sr = stp.next()
                    S.op("scalar", lambda e, sq=sq, xt=xt, stq=stq: e.activation(
                        out=sq[:], in_=xt[:], func=AF.Square, accum_out=stq[:, 0:1]), reads=[xr], writes=[sqr, sr])
                    S.op("scalar", lambda e, stq=stq: e.activation(
                        out=stq[:, 1:2], in_=stq[:, 0:1], func=AF.Sqrt, bias=epsc[:], scale=1.0 / D),
                        reads=[sr, epsc_r], writes=[sr])
                    S.op("vector", lambda e, stq=stq: e.reciprocal(out=stq[:, 2:3], in_=stq[:, 1:2]), reads=[sr], writes=[sr])
                    xn, xnr = xnp.next()
                    S.op("scalar", lambda e, xn=xn, xt=xt, stq=stq: e.activation(
                        out=xn[:], in_=xt[:], func=AF.Copy, scale=stq[:, 2:3]), reads=[xr, sr], writes=[xnr])
                    yield
                    pt, pr = tp.next()
                    for j in range(8):
                        S.op("tensor", lambda e, pt=pt, xn=xn, j=j: e.transpose(
                            out=pt[:, j, :], in_=xn[:, j * 128:(j + 1) * 128], identity=identb[:]),
                            reads=[xnr, ident_r], writes=[pr])
                    gsz = 2 if ci == 2 else 4
                    gi = i % gsz
                    if gi == 0:
                        ht, htr = hTp.next()
                    for j in range(8):
                        S.op("vector", lambda e, pt=pt, j=j, ht=ht, ci=ci, gi=gi: e.tensor_scalar(
                            out=ht[:, j, gi * 128:(gi + 1) * 128], in0=pt[:, j, :], scalar1=colmod[:, ci, j:j + 1],
                            scalar2=colmod[:, ci + 1, j:j + 1], op0=ALU.mult, op1=ALU.add),
                            reads=[pr, colmod_r], writes=[htr])
                    if gi == gsz - 1:
                        cb = c0 - gi * 128
                        S.dma("sync", lambda e, ht=ht, cb=cb, gsz=gsz: e.dma_start(
                            out=hT_d[:, :, cb:cb + gsz * 128], in_=ht[:, :, 0:gsz * 128]), htr, reads=[htr])
                    yield

        run_interleaved([p1_gen(0), p1_gen(1)])
        while pf1:
            prefetch1()
        S.flush()

    def dscr(name, shape, dt):
        if name in debug:
            return dbg_out(name, shape, dt)
        return nc.dram_tensor(name + "_scr", list(shape), dt, kind="Internal").ap()

    x1_d = dscr("x1", [SEQ, D], F32)
    h2_d = dscr("h2", [SEQ, D], BF16)
    ymoe_d = dscr("moe", [SEQ, D], F32)
    NTILE = SEQ // 128

    def ret_all():
        with ExitStack() as st:
            stg_pool = Pool(S, st, "wstg2", [128, 8, 256], F32, 2)
            pieces = [(0, RC, 1536)]
            for dr in range(2):
                b0 = 1536 + 256 * dr
                pieces += [(b0, RC + 1536 + 64 * dr, 64), (b0 + 64, RC + 1664, 64), (b0 + 128, RC + 1728 + 128 * dr, 128)]
            vfull = w_in.rearrange("(k p) n -> p k n", p=128)
            pf2 = []
            for (d0, s0, n) in pieces:
                for o in range(0, n, 256):
                    pf2.append((d0 + o, s0 + o, min(256, n - o)))

            def prefetch2():
                if not pf2:
                    return
                dd, ss, cwd = pf2.pop(0)
                stg, sr = stg_pool.next()
                dma("gpsimd", stg[:, :, :cwd], vfull[:, :, ss:ss + cwd], sr, writes=[sr])
                cp(Wrw[:, :, dd:dd + cwd], stg[:, :, :cwd], [sr], [Wrw_r])

            cst_r = S.res("ret_consts")
            protf = st.enter_context(nc.sbuf_tensor(U("protf"), [128, 128], F32))
            prot = st.enter_context(nc.sbuf_tensor(U("prot"), [128, 128], BF16))
            gC = st.enter_context(nc.sbuf_tensor(U("gC"), [128, 512], F32))
            for (dst, srcap) in [(protf[:], prot_d), (gC[:], gC_d)]:
                r_ = S.res("cst")
                dma("gpsimd", dst, srcap, r_, writes=[r_, cst_r])
            cp(prot[:], protf[:], [cst_r], [cst_r])
            zt, zt_r = st.enter_context(nc.sbuf_tensor(U("zt"), [128, D], F32)), S.res("zt")
            S.op("vector", lambda e: e.memset(zt[:], 0.0), writes=[zt_r])
            zf = list(range(NTILE))

            def zerofill():
                if zf:
                    i = zf.pop(0)
                    dma("gpsimd", ymoe_d[i * 128:(i + 1) * 128, :], zt[:], zt_r, reads=[zt_r])

            def ret_gen(dirn):
                csp = Pool(S, st, "cs", [128, 2, 128], F32, 2)
                hsp = Pool(S, st, "hsr", [128, 8, 128], BF16, 2)
                qdec = st.enter_context(nc.sbuf_tensor(U("qdec"), [128, 4, 128], F32))
                kdec = st.enter_context(nc.sbuf_tensor(U("kdec"), [128, 512], F32))
                dmask = st.enter_context(nc.sbuf_tensor(U("dmask"), [128, 8, 128], F32))
                dc_r = S.res("ret_dconsts")
                for (dst, srcap) in [(qdec[:].rearrange("p a t -> p (a t)"), qdec_d[dirn]), (kdec[:], kdec_d[dirn]),
                                     (dmask[:].rearrange("p a t -> p (a t)"), dmask_d[dirn])]:
                    r_ = S.res("cst")
                    dma("gpsimd", dst, srcap, r_, writes=[r_, dc_r])
                S32 = st.enter_context(nc.sbuf_tensor(U("S32"), [128, 512], F32))
                S16 = st.enter_context(nc.sbuf_tensor(U("S16"), [128, 512], BF16))
                S_r = S.res("S")
                S16_r = S.res("S16")
                S.op("vector", lambda e: e.memset(S32[:], 0.0), writes=[S_r])
                S.op("vector", lambda e: e.memset(S16[:], 0.0), writes=[S16_r])
                PA = Pool(S, st, "retPA", [128, 512], F32, 2, psum=True)
                PYp = Pool(S, st, "retPY", [128, 512], F32, 1, psum=True)
                qk_sb = Pool(S, st, "qk_sb", [128, 4, 128], BF16, 2)
                t1p = Pool(S, st, "t1", [128, 4, 128], F32, 1)
                t2p = Pool(S, st, "t2", [128, 4, 128], F32, 1)
                krp = Pool(S, st, "kr", [128, 3, 4, 128], BF16, 2)
                for (t_, r_) in krp.tiles:
                    S.op("gpsimd", lambda e, t_=t_: e.memset(t_[:], 0.0), writes=[r_])
                qrp = Pool(S, st, "qr", [128, 4, 128], BF16, 2)
                qpp = Pool(S, st, "qp", [128, 4, 128], BF16, 2)
                kptp = Pool(S, st, "kpt", [128, 512], BF16, 2)
                vsp = Pool(S, st, "vsb", [128, 512], BF16, 2)
                sTp = Pool(S, st, "sT", [128, 8, 128], BF16, 2)
                sqp2 = Pool(S, st, "sq2", [128, 8, 64], F32, 1)
                ynp = Pool(S, st, "yn", [128, 8, 64], F32, 1)
                sgp = Pool(S, st, "sg", [128, 512], F32, 2)
                latp = Pool(S, st, "lat", [128, 512], BF16, 2)
                st8 = Pool(S, st, "st8", [128, 3, 8], F32, 2)
                yield

                def proj_feat(wi, hs, hsr):
                    pt, pr = PA.next()
                    for p in range(4):
                        for kc in range(8):
                            mm(pt[:, p * 128:(p + 1) * 128], wsb[:, wi, kc, p * 128:(p + 1) * 128], hs[:, kc, :],
                               kc == 0, kc == 7, [wsb_r, hsr], [pr])
                    return pt, pr

                def proj_tok(wi, hs, hsr):
                    pt, pr = PA.next()
                    for kc in range(8):
                        mm(pt[:], hs[:, kc, :], wsb[:, wi, kc, :], kc == 0, kc == 7, [wsb_r, hsr], [pr])
                    return pt, pr

                def rope(wi, hs, hsr, outp, cs, csr):
                    pt, pr = proj_feat(wi, hs, hsr)
                    sb, sbr = qk_sb.next()
                    act(sb[:].rearrange("p a t -> p (a t)"), pt[:], AF.Copy, [pr], [sbr])
                    yield
                    rt_, rr = PA.next()
                    mm(rt_[:], prot[:], sb[:].rearrange("p a t -> p (a t)"), True, True, [cst_r, sbr], [rr])
                    t1, t1r = t1p.next()
                    t2, t2r = t2p.next()
                    cosb = cs[:, 0:1, :].to_broadcast([128, 4, 128])
                    sinb = cs[:, 1:2, :].to_broadcast([128, 4, 128])
                    tt(t1[:], sb[:], cosb, ALU.mult, [sbr, csr], [t1r])
                    yield
                    tt(t2[:], rt_[:].rearrange("p (a t) -> p a t", a=4), sinb, ALU.mult, [rr, csr], [t2r])
                    o, orr = outp.next()
                    if wi == 1:
                        tt(o[:, 0], t1[:], t2[:], ALU.add, [t1r, t2r], [orr])
                        act(o[0:64, 1], o[0:64, 0], AF.Copy, [], [orr])
                        act(o[64:128, 2], o[64:128, 0], AF.Copy, [], [orr])
                    else:
                        tt(o[:], t1[:], t2[:], ALU.add, [t1r, t2r], [orr])
                    yield
                    return o, orr

                PKV = Pool(S, st, "retPKV", [128, 512], F32, 1, psum=True)

                def stage_a(is_ctx, ci):
                    c0 = (CTX0 if is_ctx else LAT0) + ci * 128
                    hs, hsr = hsp.next()
                    dma("sync", hs[:], hT_d[:, :, c0:c0 + 128], hsr, writes=[hsr])
                    if dirn == 0:
                        zerofill()
                    else:
                        prefetch2()
                    cs, csr = csp.next()
                    dma("gpsimd", cs[:, 0, :], cosT_d[:, c0:c0 + 128], csr, writes=[csr])
                    dma("gpsimd", cs[:, 1, :], sinT_d[:, c0:c0 + 128], csr, writes=[csr])
                    kr, krr = yield from rope(1, hs, hsr, krp, cs, csr)
                    pk, pkr = PA.next()
                    pkb = pk.bitcast(BF16)
                    for p in range(4):
                        tr(pkb[:, p * 128:(p + 1) * 128], kr[:, 0, p, :], identb[:], [krr, ident_r], [pkr])
                    kpt, kptr = kptp.next()
                    tt(kpt[:], pkb[:, 0:512], kdec[:], ALU.mult, [pkr, dc_r], [kptr])
                    yield
                    pv, pvr = proj_tok(2, hs, hsr)
                    vs, vsr = vsp.next()
                    act(vs[:], pv[:], AF.Copy, [pvr], [vsr])
                    yield
                    ctx_ = dict(is_ctx=is_ctx, ci=ci, kpt=kpt, kptr=kptr, vs=vs, vsr=vsr)
                    if not is_ctx:
                        qr, qrr = yield from rope(0, hs, hsr, qrp, cs, csr)
                        qp, qpr = qpp.next()
                        tt(qp[:], qr[:], qdec[:], ALU.mult, [qrr, dc_r], [qpr])
                        sT, sTr = sTp.next()
                        for b_ in range(2):
                            ps, psr = PA.next()
                            for hh in range(4):
                                h = 4 * b_ + hh
                                p, hf = h // 2, h % 2
                                mm(ps[:, hh * 128:(hh + 1) * 128], kr[:, 1 + hf, p, :], qr[:, p, :], True, True,
                                   [krr, qrr], [psr])
                            tt(sT[:, 4 * b_:4 * b_ + 4, :], ps[:].rearrange("p (a t) -> p a t", a=4),
                               dmask[:, 4 * b_:4 * b_ + 4, :], ALU.mult, [psr, dc_r], [sTr])
                            yield
                        pg, pgr = proj_tok(3 + dirn, hs, hsr)
                        sg, sgr = sgp.next()
                        act(sg[:], pg[:], AF.Silu, [pgr], [sgr])
                        yield
                        ctx_.update(qp=qp, qpr=qpr, sT=sT, sTr=sTr, sg=sg, sgr=sgr)
                    return ctx_

                def stage_b(cx):
                    kpt, kptr, vs, vsr = cx["kpt"], cx["kptr"], cx["vs"], cx["vsr"]
                    if not cx["is_ctx"]:
                        qp, qpr, sT, sTr, sg, sgr, ci = cx["qp"], cx["qpr"], cx["sT"], cx["sTr"], cx["sg"], cx["sgr"], cx["ci"]
                        py, pyr = PYp.next()
                        for p in range(4):
                            mm(py[:, p * 128:(p + 1) * 128], qp[:, p, :], S16[:, p * 128:(p + 1) * 128], True, False,
                               [qpr, S16_r], [pyr])
                            for hf in range(2):
                                h = 2 * p + hf
                                mm(py[:, h * 64:(h + 1) * 64], sT[:, h, :], vs[:, h * 64:(h + 1) * 64], False, hf == 1,
                                   [sTr, vsr], [pyr])
                        yield
                    kv, kvr = PKV.next()
                    for p in range(4):
                        mm(kv[:, p * 128:(p + 1) * 128], kpt[:, p * 128:(p + 1) * 128], vs[:, p * 128:(p + 1) * 128],
                           True, True, [kptr, vsr], [kvr])
                    tt(S32[:], kv[:], S32[:], ALU.add, [kvr], [S_r])
                    tt(S32[:], S32[:], gC[:], ALU.mult, [cst_r], [S_r])
                    act(S16[:], S32[:], AF.Copy, [S_r], [S16_r])
                    yield
                    if not cx["is_ctx"]:
                        sq, sqr = sqp2.next()
                        py3 = py[:].rearrange("p (h e) -> p h e", h=8)
                        act(sq[:], py3, AF.Square, [pyr], [sqr])
                        yield
                        s8, s8r = st8.next()
                        red(s8[:, 0, :], sq[:], [sqr], [s8r])
                        act(s8[:, 1, :], s8[:, 0, :], AF.Sqrt, [s8r, epsc_r], [s8r], bias=epsc[:], scale=1.0 / 64)
                        rcp(s8[:, 2, :], s8[:, 1, :], [s8r], [s8r])
                        yield
                        yn, ynr = ynp.next()
                        tt(yn[:], py3, s8[:, 2, :].unsqueeze(2).to_broadcast([128, 8, 64]), ALU.mult, [pyr, s8r], [ynr])
                        lt, ltr = latp.next()
                        tt(lt[:], yn[:].rearrange("p h e -> p (h e)"), sg[:], ALU.mult, [ynr, sgr], [ltr])
                        dma("sync", lat_d[dirn][ci * 128:(ci + 1) * 128, 0:512], lt[:], ltr, reads=[ltr])
                        yield

                if dirn == 0:
                    order = [(True, 0), (True, 1)] + [(False, i) for i in range(SEQ // 128)]
                else:
                    order = [(True, 1), (True, 0)] + [(False, i) for i in reversed(range(SEQ // 128))]
                order = order[:KLIM]
                cx = yield from stage_a(*order[0])
                for k in range(len(order)):
                    gA = stage_a(*order[k + 1]) if k + 1 < len(order) else None
                    gB = stage_b(cx)
                    nxt = None
                    while gA is not None or gB is not None:
                        if gB is not None:
                            try:
                                next(gB)
                            except StopIteration:
                                gB = None
                        if gA is not None:
                            try:
                                next(gA)
                            except StopIteration as e_:
                                nxt = e_.value
                                gA = None
                        yield
                    cx = nxt

            run_interleaved([ret_gen(0), ret_gen(1)], skew=KSKEW_RET)
            while pf2:
                prefetch2()
            while zf:
                zerofill()
            S.flush()

    if KLIM >= 0:
        ret_all()
    ret_wstack.close()


    cw_d = din("cwT", [2, 128, 14 * 3])
    w2pad_d = din("w2pad", [2, 128, 512])
    a2pad_d = din("a2pad", [128, 512])
    g2_d = din("g2", [2, 128, 512])
    rwcols_d = din("rwcols", [128, 4 * 8])
    lnx_d = din("lnx", [2, 512])
    tri_d = din("tri", [4, 128, 128])
    bdm_d = din("bdmask", [128, 512])
    rmask_d = din("rmask", [128, 512])
    ind2_d = din("ind2", [128, 2])
    bones_d = din("bones", [128, 128])
    lvm_d = din("lvlmask", [14, 128, 128], U32)
    CDEC = float(np.exp(-0.5))
    GN_EPS = 64e-5

    def rwkv_all():
        with ExitStack() as st:
            cst_r = S.res("rw_consts")

            def cload(name, shape, dt, src_ap, cast_from=None):
                t = st.enter_context(nc.sbuf_tensor(U(name), shape, dt))
                r_ = S.res(name)
                if cast_from is None:
                    dma("gpsimd", t[:], src_ap, r_, writes=[r_, cst_r])
                    return t
                with ExitStack() as stc:
                    tf = st.enter_context(nc.sbuf_tensor(U(name + "f"), shape, cast_from))
                    dma("gpsimd", tf[:], src_ap, r_, writes=[r_])
                    cp(t[:], tf[:], [r_], [cst_r])
                return t

            cwl = [cload("cw%d" % d_, [128, 14 * 3], F32, cw_d[d_]) for d_ in range(2)]
            a2p = cload("a2p", [128, 512], BF16, a2pad_d, F32)
            w2pl = [cload("w2p%d" % d_, [128, 512], BF16, w2pad_d[d_], F32) for d_ in range(2)]
            g2sl = [cload("g2s%d" % d_, [128, 512], BF16, g2_d[d_], F32) for d_ in range(2)]
            rwc = cload("rwc", [128, 4, 8], F32, rwcols_d.rearrange("p (a b) -> p a b", a=4))
            lnxw = cload("lnxw", [128, 512], F32, bcast_rows(lnx_d[0], 512))
            lnxb = cload("lnxb", [128, 512], F32, bcast_rows(lnx_d[1], 512))
            tril = [cload("tri%d" % i_, [128, 128], BF16, tri_d[i_], F32) for i_ in range(4)]
            bdm = cload("bdm", [128, 4, 128], F32, bdm_d.rearrange("p (a b) -> p a b", a=4))
            rmask = cload("rmask", [128, 512], F32, rmask_d)
            ind2 = cload("ind2", [128, 2], BF16, ind2_d, F32)
            bones = cload("bones", [128, 128], BF16, bones_d, F32)
            lvm = cload("lvm", [128, 14, 128], U32, lvm_d.rearrange("a p t -> p a t"))
            tinyc = st.enter_context(nc.sbuf_tensor(U("tinyc"), [128, 2], F32))
            S.op("vector", lambda e: e.memset(tinyc[:, 0:1], 1e-24), writes=[cst_r])
            S.op("vector", lambda e: e.memset(tinyc[:, 1:2], GN_EPS), writes=[cst_r])
            omka = st.enter_context(nc.sbuf_tensor(U("omka"), [128, 4, 1], F32))
            ts(omka[:], rwc[:, :, 4:5], -1.0, 1.0, ALU.mult, ALU.add, [cst_r], [cst_r])
            S.flush()
            print("RWKV sbuf remaining after shared", nc.sbuf_bytes_remaining)

            def rwkv_gen(dirn):
                cw, w2p, g2s = cwl[dirn], w2pl[dirn], g2sl[dirn]
                sk = 0 if dirn == 0 else 2
                m_strict, m_incl, m_strictT = tril[sk], tril[sk + 1], tril[2 - sk]
                w0col = lambda blk: rwc[:, blk, dirn:dirn + 1]
                a0col = lambda blk: rwc[:, blk, 2:3]

                def wcols(blk):
                    if blk < 12:
                        return slice(blk * 128, (blk + 1) * 128)
                    o_ = 1536 + 256 * dirn + (blk - 12) * 128
                    return slice(o_, o_ + 128)

                def T(name, shape, dt):
                    return st.enter_context(nc.sbuf_tensor(U(name), shape, dt)), S.res(name)

                hsp = Pool(S, st, "hsw", [128, 8, 130], BF16, 2)
                H32, H32_r = T("H32", [128, 4, 128], F32)
                H16, H16_r = T("H16", [128, 4, 128], BF16)
                S.op("vector", lambda e: e.memset(H32[:], 0.0), writes=[H32_r])
                rw, rw_r = T("rw", [128, 14, 128], F32)
                lr, lr_r = T("lr", [128, 128], BF16)
                sgl, sgl_r = T("sgl", [128, 128], BF16)
                sig, sig_r = T("sig", [128, 4, 128], F32)
                icl, icl_r = T("icl", [128, 4, 128], F32)
                cum, cum_r = T("cum", [128, 4, 128], F32)
                pex, pex_r = T("pex", [128, 4, 128], F32)
                Er, Er_r = T("Er", [128, 512], F32)
                Ea, Ea_r = T("Ea", [128, 512], F32)
                Ek, Ek_r = T("Ek", [128, 512], F32)
                v3 = lambda t_: t_[:].rearrange("p (a t) -> p a t", a=4)
                v8 = lambda t_: t_[:].rearrange("p (h e) -> p h e", h=8)
                WC, WC_r = T("WC", [128, 4, 1], F32)
                kk, kk_r = sig, sig_r
                rn, rn_r = cum, cum_r
                t1, t1_r = pex, pex_r
                htmp, htmp_r = sig, sig_r
                kk2, kk2_r = T("kk2", [128, 4, 128], BF16)
                rkr, rkr_r = T("rkr", [128, 4, 128], BF16)
                rt, rt_r = T("rt", [128, 3, 4, 128], BF16)
                at, at_r = T("at", [128, 3, 4, 128], BF16)
                S.op("gpsimd", lambda e: e.memset(rt[:], 0.0), writes=[rt_r])
                S.op("gpsimd", lambda e: e.memset(at[:], 0.0), writes=[at_r])
                kt, kt_r = T("kt", [128, 4, 128], BF16)
                bt, bt_r = T("bt", [128, 4, 128], BF16)
                vbf, vbf_r = T("vbf", [128, 4, 128], BF16)
                vtok, vtok_r = T("vtok", [128, 512], BF16)
                ktok, ktok_r = T("ktok", [128, 512], BF16)
                btok, btok_r = T("btok", [128, 512], BF16)

                def T2g(name):
                    t_ = st.enter_context(nc.sbuf_tensor(U(name), [128, 8, 128], BF16))
                    return t_, [S.res(name + "0"), S.res(name + "1")]

                N0, N0g = T2g("N0")
                M0, M0g = T2g("M0")
                AakT, AakTg = T2g("AakT")
                ArbT, ArbTg = T2g("ArbT")
                ArkT, ArkTg = T2g("ArkT")
                Pd, Pdg = T2g("Pd")
                PdT, PdTg = T2g("PdT")
                T1s, T1sg = T2g("T1s")
                T2s, T2sg = T2g("T2s")
                rhs_sb, rhs_r = T("rhs_sb", [128, 512], BF16)
                u_sb, u_r = T("u_sb", [128, 512], BF16)
                s8, s8_r = T("s8", [128, 6, 8], F32)
                latp = Pool(S, st, "latr", [128, 512], BF16, 2)
                P1 = Pool(S, st, "P1", [128, 512], F32, 2, psum=True)
                P2 = Pool(S, st, "P2", [128, 4, 128], F32, 2, psum=True)
                print("RWKV sbuf remaining after gen alloc", dirn, nc.sbuf_bytes_remaining)
                yield

                if dirn == 0:
                    order = [(True, 0), (True, 1)] + [(False, i) for i in range(SEQ // 128)]
                else:
                    order = [(True, 1), (True, 0)] + [(False, i) for i in reversed(range(SEQ // 128))]
                ident_b4 = identb[:].unsqueeze(1).to_broadcast([128, 4, 128])
                fM, fN = (0, 1) if dirn == 0 else (1, 0)
                G = lambda t_, g: t_[:, 4 * g:4 * g + 4, :]
                def stage1(is_ctx, ci):
                    c0 = (CTX0 if is_ctx else LAT0) + ci * 128
                    hs, hsr = hsp.next()
                    dma("sync", hs[:], hT_d[:, :, c0 - 1:c0 + 129], hsr, writes=[hsr])
                    for blk in range(14):
                        if is_ctx and blk == 13:
                            continue
                        pt, pr = P1.next()
                        for kc in range(8):
                            mm(pt[:, 0:130], Wrw[:, kc, wcols(blk)], hs[:, kc, :], kc == 0, kc == 7, [Wrw_r, hsr], [pr])
                        act(rw[:, blk, :], pt[:, 1:129], AF.Copy, [pr, cst_r], [rw_r],
                            scale=cw[:, 3 * blk + 1:3 * blk + 2])
                        stt(rw[:, blk, :], pt[:, 0:128], cw[:, 3 * blk:3 * blk + 1], rw[:, blk, :], ALU.mult, ALU.add,
                            [pr, cst_r], [rw_r])
                        stt(rw[:, blk, :], pt[:, 2:130], cw[:, 3 * blk + 2:3 * blk + 3], rw[:, blk, :], ALU.mult,
                            ALU.add, [pr, cst_r], [rw_r])
                        if blk % 2 == 1:
                            yield

                order = order[:KLIM]
                yield from stage1(*order[0])
                for k_, (is_ctx, ci) in enumerate(order):
                    gS1 = [None]

                    def step_s1():
                        if gS1[0] is not None:
                            try:
                                next(gS1[0])
                            except StopIteration:
                                gS1[0] = None
                    rr_, kk_, vv_ = rw[:, 0:4, :], rw[:, 4:8, :], rw[:, 8:12, :]
                    act(lr[0:64, :], rw[0:64, 12, :], AF.Tanh, [rw_r], [lr_r])
                    act(lr[64:128, :], rw[64:128, 12, :], AF.Copy, [rw_r], [lr_r])
                    if not is_ctx:
                        act(sgl[:], rw[:, 13, :], AF.Sigmoid, [rw_r], [sgl_r])
                    act(vbf[:], vv_, AF.Copy, [rw_r], [vbf_r])
                    pz, pzr = P1.next()
                    for blk in range(4):
                        mm(pz[:, blk * 128:(blk + 1) * 128], w2p[:, blk * 128:(blk + 1) * 128], lr[:], True, True,
                           [cst_r, lr_r], [pzr])
                    for blk in range(4):
                        act(sig[:, blk, :], pz[:, blk * 128:(blk + 1) * 128], AF.Sigmoid, [pzr, cst_r], [sig_r],
                            bias=w0col(blk))
                    yield
                    pi_, pir = P1.next()
                    for blk in range(4):
                        mm(pi_[:, blk * 128:(blk + 1) * 128], a2p[:, blk * 128:(blk + 1) * 128], lr[:], True, True,
                           [cst_r, lr_r], [pir])
                    for blk in range(4):
                        act(icl[:, blk, :], pi_[:, blk * 128:(blk + 1) * 128], AF.Sigmoid, [pir, cst_r], [icl_r],
                            bias=a0col(blk))
                    flat = lambda t_: t_[:].rearrange("p a t -> p (a t)")
                    S.op("vector", lambda e: e.tensor_tensor_scan(out=flat(cum), data0=rmask[:], data1=flat(sig),
                                                                  initial=0.0, op0=ALU.mult, op1=ALU.add),
                         reads=[cst_r, sig_r], writes=[cum_r])
                    yield
                    tt(pex[:], cum[:], sig[:], ALU.subtract, [cum_r, sig_r], [pex_r])
                    if dirn == 0:
                        act(v3(Er), cum[:], AF.Exp, [cum_r], [Er_r], scale=-CDEC)
                        act(v3(Ea), pex[:], AF.Exp, [pex_r], [Ea_r], scale=-CDEC)
                        act(v3(Ek), cum[:], AF.Exp, [cum_r], [Ek_r], scale=CDEC)
                    else:
                        act(v3(Er), pex[:], AF.Exp, [pex_r], [Er_r], scale=CDEC)
                        act(v3(Ea), cum[:], AF.Exp, [cum_r], [Ea_r], scale=CDEC)
                        act(v3(Ek), pex[:], AF.Exp, [pex_r], [Ek_r], scale=-CDEC)
                    act(WC[:], cum[:, :, 127:128], AF.Exp, [cum_r], [WC_r], scale=-CDEC)
                    yield
                    tt(kk[:], kk_, rwc[:, :, 3:4].to_broadcast([128, 4, 128]), ALU.mult, [rw_r, cst_r], [kk_r])
                    act(kk2[:], kk[:], AF.Square, [kk_r], [kk2_r])
                    pss, pssr = P1.next()
                    for blk in range(4):
                        mm(pss[:, blk * 128:(blk + 1) * 128], bones[:], kk2[:, blk, :], True, True, [cst_r, kk2_r], [pssr])
                    act(rn[:].rearrange("p a t -> p (a t)"), pss[:], AF.Ln, [pssr, cst_r], [rn_r], bias=tinyc[:, 0:1])
                    act(rn[:], rn[:], AF.Exp, [], [rn_r], scale=-0.5)
                    yield
                    tt(kk[:], kk[:], rn[:], ALU.mult, [rn_r], [kk_r])
                    tt(t1[:], icl[:], rwc[:, :, 4:5].to_broadcast([128, 4, 128]), ALU.mult, [icl_r, cst_r], [t1_r])
                    tt(t1[:], t1[:], omka[:].to_broadcast([128, 4, 128]), ALU.add, [cst_r], [t1_r])
                    tt(t1[:], kk_, t1[:], ALU.mult, [rw_r], [t1_r])
                    tt(icl[:], kk[:], icl[:], ALU.mult, [kk_r], [icl_r])
                    yield
                    if not is_ctx:
                        tt(rn[:], rr_, rwc[:, :, 5:6].to_broadcast([128, 4, 128]), ALU.mult, [rw_r, cst_r], [rn_r])
                        tt(rkr[:], rn[:], t1[:], ALU.mult, [rn_r, t1_r], [rkr_r])
                        tt(rt[:, 0], rr_, v3(Er), ALU.mult, [rw_r, Er_r], [rt_r])
                        act(rt[0:64, 1], rt[0:64, 0], AF.Copy, [], [rt_r])
                        act(rt[64:128, 2], rt[64:128, 0], AF.Copy, [], [rt_r])
                    stt(at[:, 0], kk[:], -1.0, v3(Ea), ALU.mult, ALU.mult, [kk_r, Ea_r], [at_r])
                    act(at[0:64, 1], at[0:64, 0], AF.Copy, [], [at_r])
                    act(at[64:128, 2], at[64:128, 0], AF.Copy, [], [at_r])
                    tt(kt[:], t1[:], v3(Ek), ALU.mult, [t1_r, Ek_r], [kt_r])
                    tt(bt[:], icl[:], v3(Ek), ALU.mult, [icl_r, Ek_r], [bt_r])
                    yield
                    if k_ + 1 < len(order):
                        gS1[0] = stage1(*order[k_ + 1])
                    for (srcT, srcr, dst, dstr) in ((vbf, vbf_r, vtok, vtok_r), (kt, kt_r, ktok, ktok_r),
                                                    (bt, bt_r, btok, btok_r)):
                        pt, pr = P1.next()
                        ptb = pt.bitcast(BF16)
                        for blk in range(4):
                            tr(ptb[:, blk * 128:(blk + 1) * 128], srcT[:, blk, :], identb[:], [srcr, ident_r], [pr])
                        act(dst[:], ptb[:, 0:512], AF.Copy, [pr], [dstr])
                    step_s1()
                    yield

                    def scores(g, lhs, lhs_r, rhsm, rhsm_r, mask, dst, dst_rg, lhs_masked=False):
                        p2, p2r = P2.next()
                        for hh in range(4):
                            h = 4 * g + hh
                            p, hf = h // 2, h % 2
                            if lhs_masked:
                                mm(p2[:, hh, :], lhs[:, 1 + hf, p, :], rhsm[:, p, :], True, True, [lhs_r, rhsm_r], [p2r])
                            else:
                                mm(p2[:, hh, :], lhs[:, p, :], rhsm[:, 1 + hf, p, :], True, True, [lhs_r, rhsm_r], [p2r])
                        tt(G(dst, g), p2[:], mask[:].unsqueeze(1).to_broadcast([128, 4, 128]), ALU.mult,
                           [p2r, cst_r], [dst_rg[g]])

                    for g in range(2):
                        scores(g, bt, bt_r, at, at_r, m_strict, N0, N0g)
                        scores(g, at, at_r, bt, bt_r, m_strictT, M0, M0g, lhs_masked=True)
                        step_s1()
                        yield
                    for g in range(2):
                        scores(g, kt, kt_r, at, at_r, m_strict, AakT, AakTg)
                        if not is_ctx:
                            scores(g, bt, bt_r, rt, rt_r, m_incl, ArbT, ArbTg)
                            scores(g, kt, kt_r, rt, rt_r, m_incl, ArkT, ArkTg)
                        step_s1()
                        yield

                    def pred(dst, dst_rg, g, lvl, form, data_ap, data_r):
                        mk = lvm[:, form * 7 + lvl - 1, :].unsqueeze(1).to_broadcast([128, 4, 128])
                        S.op("vector", lambda e: e.copy_predicated(out=G(dst, g), mask=mk, data=data_ap),
                             reads=[cst_r, data_r], writes=[dst_rg[g]])

                    for g in range(2):
                        cp(G(Pd, g), ident_b4, [ident_r], [Pdg[g]])
                        cp(G(PdT, g), ident_b4, [ident_r], [PdTg[g]])
                        pred(Pd, Pdg, g, 1, fM, G(M0, g), M0g[g])
                        pred(PdT, PdTg, g, 1, fN, G(N0, g), N0g[g])
                    step_s1()
                    yield
                    for lvl in range(2, 8):
                        last = lvl == 7
                        for g in range(2):
                            if not last:
                                pT1, pT1r = P2.next()
                                for hh in range(4):
                                    h = 4 * g + hh
                                    mm(pT1[:, hh, :], N0[:, h, :], Pd[:, h, :], True, True, [N0g[g], Pdg[g]], [pT1r])
                                act(G(T1s, g), pT1[:], AF.Copy, [pT1r], [T1sg[g]])
                            pT2, pT2r = P2.next()
                            for hh in range(4):
                                h = 4 * g + hh
                                mm(pT2[:, hh, :], M0[:, h, :], PdT[:, h, :], True, True, [M0g[g], PdTg[g]], [pT2r])
                            act(G(T2s, g), pT2[:], AF.Copy, [pT2r], [T2sg[g]])
                            step_s1()
                            yield
                            if not last:
                                pX, pXr = P2.next()
                                for hh in range(4):
                                    h = 4 * g + hh
                                    mm(pX[:, hh, :], PdT[:, h, :], T1s[:, h, :], True, True, [PdTg[g], T1sg[g]], [pXr])
                            pXT, pXTr = P2.next()
                            for hh in range(4):
                                h = 4 * g + hh
                                mm(pXT[:, hh, :], Pd[:, h, :], T2s[:, h, :], True, True, [Pdg[g], T2sg[g]], [pXTr])
                            if not last:
                                pred(Pd, Pdg, g, lvl, fM, pX[:], pXr)
                            pred(PdT, PdTg, g, lvl, fN, pXT[:], pXTr)
                            step_s1()
                            yield
                    Q16, Q16g = PdT, PdTg
                    wcb = WC[:].to_broadcast([128, 4, 128])
                    if dirn == 1:
                        tt(H32[:], H32[:], wcb, ALU.mult, [WC_r], [H32_r])
                    act(H16[:], H32[:], AF.Copy, [H32_r], [H16_r])
                    pr_, prr = P1.next()
                    for p in range(4):
                        mm(pr_[:, p * 128:(p + 1) * 128], at[:, 0, p, :], H16[:, p, :], True, False, [at_r, H16_r], [prr])
                        for hf in range(2):
                            h = 2 * p + hf
                            mm(pr_[:, h * 64:(h + 1) * 64], AakT[:, h, :], vtok[:, h * 64:(h + 1) * 64], False, hf == 1,
                               [AakTg[h // 4], vtok_r], [prr])
                    act(rhs_sb[:], pr_[:], AF.Copy, [prr], [rhs_r])
                    step_s1()
                    yield
                    pu, pur = P1.next()
                    for h in range(8):
                        mm(pu[:, h * 64:(h + 1) * 64], Q16[:, h, :], rhs_sb[:, h * 64:(h + 1) * 64], True, True,
                           [Q16g[h // 4], rhs_r], [pur])
                    act(u_sb[:], pu[:], AF.Copy, [pur], [u_r])
                    step_s1()
                    yield
                    if not is_ctx:
                        py_, pyr = P2.next()
                        py = py_[:].rearrange("p a t -> p (a t)")
                        for p in range(4):
                            mm(py[:, p * 128:(p + 1) * 128], rt[:, 0, p, :], H16[:, p, :], True, False, [rt_r, H16_r],
                               [pyr])
                            for hf in range(2):
                                h = 2 * p + hf
                                mm(py[:, h * 64:(h + 1) * 64], ArbT[:, h, :], u_sb[:, h * 64:(h + 1) * 64], False, False,
                                   [ArbTg[h // 4], u_r], [pyr])
                                mm(py[:, h * 64:(h + 1) * 64], ArkT[:, h, :], vtok[:, h * 64:(h + 1) * 64], False,
                                   hf == 1, [ArkTg[h // 4], vtok_r], [pyr])
                    p2h, p2hr = P2.next()
                    for p in range(4):
                        mm(p2h[:, p, :], btok[:, p * 128:(p + 1) * 128], u_sb[:, p * 128:(p + 1) * 128], True, False,
                           [btok_r, u_r], [p2hr])
                        mm(p2h[:, p, :], ktok[:, p * 128:(p + 1) * 128], vtok[:, p * 128:(p + 1) * 128], False, True,
                           [ktok_r, vtok_r], [p2hr])
                    tt(htmp[:], p2h[:], bdm[:], ALU.mult, [p2hr, cst_r], [htmp_r])
                    tt(H32[:], htmp[:], H32[:], ALU.add, [htmp_r], [H32_r])
                    if dirn == 0:
                        tt(H32[:], H32[:], wcb, ALU.mult, [WC_r], [H32_r])
                    step_s1()
                    yield
                    if is_ctx:
                        while gS1[0] is not None:
                            step_s1()
                            yield
                        continue
                    py3 = py.rearrange("p (h e) -> p h e", h=8)
                    red(s8[:, 0, :], py3, [pyr], [s8_r])
                    act(v8(Er), py3, AF.Square, [pyr], [Er_r])
                    red(s8[:, 1, :], v8(Er), [Er_r], [s8_r])
                    ts(s8[:, 2, :], s8[:, 0, :], 1.0 / 64, None, ALU.mult, ALU.bypass, [], [s8_r])
                    tt(s8[:, 3, :], s8[:, 2, :], s8[:, 2, :], ALU.mult, [], [s8_r])
                    stt(s8[:, 4, :], s8[:, 1, :], 1.0 / 64, s8[:, 3, :], ALU.mult, ALU.subtract, [], [s8_r])
                    act(s8[:, 5, :], s8[:, 4, :], AF.Sqrt, [cst_r], [s8_r], bias=tinyc[:, 1:2])
                    rcp(s8[:, 5, :], s8[:, 5, :], [], [s8_r])
                    tt(v8(Ea), py3, s8[:, 2, :].unsqueeze(2).to_broadcast([128, 8, 64]), ALU.subtract, [pyr, s8_r],
                       [Ea_r])
                    step_s1()
                    yield
                    prk, prkr = P1.next()
                    for blk in range(4):
                        mm(prk[:, 2 * blk:2 * blk + 2], rkr[:, blk, :], ind2[:], True, True, [rkr_r, cst_r], [prkr])
                    pg, pgr = P1.next()
                    mm(pg[:], sgl[:], g2s[:], True, True, [sgl_r, cst_r], [pgr])
                    tt(v8(Ea), v8(Ea), s8[:, 5, :].unsqueeze(2).to_broadcast([128, 8, 64]), ALU.mult, [s8_r], [Ea_r])
                    tt(Ea[:], Ea[:], lnxw[:], ALU.mult, [cst_r], [Ea_r])
                    tt(Ea[:], Ea[:], lnxb[:], ALU.add, [cst_r], [Ea_r])
                    tt(v8(Ek), vtok[:].rearrange("p (h e) -> p h e", h=8),
                       prk[:, 0:8].unsqueeze(2).to_broadcast([128, 8, 64]), ALU.mult, [vtok_r, prkr], [Ek_r])
                    tt(Ea[:], Ea[:], Ek[:], ALU.add, [Ek_r], [Ea_r])
                    lt, ltr = latp.next()
                    tt(lt[:], Ea[:], pg[:], ALU.mult, [Ea_r, pgr], [ltr])
                    dma("sync", lat_d[dirn][ci * 128:(ci + 1) * 128, 512:1024], lt[:], ltr, reads=[ltr])
                    step_s1()
                    yield
                    while gS1[0] is not None:
                        step_s1()
                        yield

            run_interleaved([rwkv_gen(0), rwkv_gen(1)], skew=KSKEW_RW)
            S.flush()

    if KLIM >= 0 and KRW:
        rwkv_all()
    rw_wstack.close()


    w_out = din("w_out", [D, D])
    w_router = din("w_router", [D, 16])
    w_gate = din("w_gate", [16, D, D])
    w_up = din("w_up", [16, D, D])
    w_down = din("w_down", [16, D, D])
    tid_d = din("tidc", [128, 32 * 2])
    iota_d = din("iota512", [128, 512])
    rm16_d = din("rmask16", [128, 512])
    ones_d = din("onesc", [128, 128])


    with ExitStack() as st:
        wout = st.enter_context(nc.sbuf_tensor(U("wout"), [128, 8, D], BF16))
        wout_r = S.res("wout")
        stg_pool = Pool(S, st, "wstg4", [128, 8, 256], F32, 2)
        load_w_bf16(stg_pool, wout, wout_r, w_out, D)
        c4 = S.res("c4")
        wr = st.enter_context(nc.sbuf_tensor(U("wr"), [128, 8, 16], F32))
        rows = st.enter_context(nc.sbuf_tensor(U("rows4"), [128, 3, D], F32))
        r_ = S.res("c4a")
        dma("gpsimd", wr[:], w_router.rearrange("(k p) e -> p k e", p=128), r_, writes=[r_, c4])
        r_ = S.res("c4b")
        dma("gpsimd", rows[:].rearrange("p a d -> p (a d)"), bcast_rows(rowmod_d, 3 * D), r_, writes=[r_, c4])
        def p4_gen(parity):
            lfp = Pool(S, st, "lf", [128, D], BF16, 2)
            lbp = Pool(S, st, "lb", [128, D], BF16, 2)
            ltp = Pool(S, st, "l4", [128, D], BF16, 1)
            lTp = Pool(S, st, "lT", [128, 8, 128], BF16, 1)
            xip = Pool(S, st, "xi4", [128, D], F32, 2)
            tmp_ = Pool(S, st, "tm4", [128, D], F32, 1)
            x1p = Pool(S, st, "x14", [128, D], F32, 1)
            h2p = Pool(S, st, "h24", [128, D], F32, 1)
            h2bp = Pool(S, st, "h2b4", [128, D], BF16, 1)
            h2Tp = Pool(S, st, "h2T4", [128, 8, 128], F32, 1)
            jkp = Pool(S, st, "jk4", [128, D], BF16, 1)
            stp4 = Pool(S, st, "st4", [128, 12], F32, 2)
            exp_ = Pool(S, st, "ex4", [128, 16], F32, 2)
            pbig = Pool(S, st, "pbig4", [128, D], F32, 1, psum=True)
            psml = Pool(S, st, "psml4", [128, 512], F32, 1, psum=True)
            yield
            for i in range(parity, NTILE if KLIM > 100 else min(NTILE, max(KLIM, 0)), 2):
                r0 = i * 128
                lf, lfr = lfp.next()
                lb, lbr = lbp.next()
                dma("sync", lf[:], lat_d[0][r0:r0 + 128, :], lfr, writes=[lfr])
                dma("sync", lb[:], lat_d[1][r0:r0 + 128, :], lbr, writes=[lbr])
                lt, ltr = ltp.next()
                tt(lt[:], lf[:], lb[:], ALU.add, [lfr, lbr], [ltr])
                pt_, ptr_ = psml.next()
                pt = pt_.bitcast(BF16)[:, 0:1024].rearrange("p (j t) -> p j t", j=8)
                for j in range(8):
                    tr(pt[:, j, :], lt[:, j * 128:(j + 1) * 128], identb[:], [ltr, ident_r], [ptr_])
                lT, lTr = lTp.next()
                act(lT[:], pt, AF.Copy, [ptr_], [lTr])
                yield
                pm, pmr = pbig.next()
                for dh in range(2):
                    for fc in range(8):
                        mm(pm[:, dh * 512:(dh + 1) * 512], lT[:, fc, :], wout[:, fc, dh * 512:(dh + 1) * 512],
                           fc == 0, fc == 7, [lTr, wout_r], [pmr])
                s4, s4r = stp4.next()
                jk, jkr = jkp.next()
                act(jk[:], pm[:], AF.Square, [pmr], [jkr, s4r], accum_out=s4[:, 0:1])
                act(s4[:, 1:2], s4[:, 0:1], AF.Sqrt, [s4r, epsc_r], [s4r], bias=epsc[:], scale=1.0 / D)
                rcp(s4[:, 2:3], s4[:, 1:2], [s4r], [s4r])
                yield
                tm, tmr = tmp_.next()
                tt(tm[:], pm[:], rows[:, 0, :], ALU.mult, [pmr, c4], [tmr])
                xi, xir = xip.next()
                dma("sync", xi[:], x[r0:r0 + 128, :], xir, writes=[xir])
                x1, x1r = x1p.next()
                stt(x1[:], tm[:], s4[:, 2:3], xi[:], ALU.mult, ALU.add, [tmr, s4r, xir], [x1r])
                dma("sync", x1_d[r0:r0 + 128, :], x1[:], x1r, reads=[x1r])
                yield
                act(jk[:], x1[:], AF.Square, [x1r], [jkr, s4r], accum_out=s4[:, 3:4])
                act(s4[:, 4:5], s4[:, 3:4], AF.Sqrt, [s4r, epsc_r], [s4r], bias=epsc[:], scale=1.0 / D)
                rcp(s4[:, 5:6], s4[:, 4:5], [s4r], [s4r])
                h2, h2r = h2p.next()
                stt(h2[:], x1[:], s4[:, 5:6], rows[:, 1, :], ALU.mult, ALU.mult, [x1r, s4r, c4], [h2r])
                tt(h2[:], h2[:], rows[:, 2, :], ALU.add, [c4], [h2r])
                h2b, h2br = h2bp.next()
                act(h2b[:], h2[:], AF.Copy, [h2r], [h2br])
                dma("sync", h2_d[r0:r0 + 128, :], h2b[:], h2br, reads=[h2br])
                yield
                p2t_, p2tr = pbig.next()
                p2t = p2t_[:].rearrange("p (j t) -> p j t", j=8)
                for j in range(8):
                    tr(p2t[:, j, :], h2[:, j * 128:(j + 1) * 128], ident[:], [h2r, ident_r], [p2tr])
                h2T, h2Tr = h2Tp.next()
                cp(h2T[:], p2t, [p2tr], [h2Tr])
                yield
                pl_, plr = psml.next()
                pl = pl_[:, 0:16]
                for kc in range(8):
                    mm(pl, h2T[:, kc, :], wr[:, kc, :], kc == 0, kc == 7, [h2Tr, c4], [plr])
                red(s4[:, 6:7], pl, [plr], [s4r], op=ALU.max)
                ts(s4[:, 7:8], s4[:, 6:7], -1.0, None, ALU.mult, ALU.bypass, [], [s4r])
                ex, exr = exp_.next()
                act(ex[:], pl, AF.Exp, [plr, s4r], [exr, s4r], bias=s4[:, 7:8], accum_out=s4[:, 8:9])
                rcp(s4[:, 9:10], s4[:, 8:9], [s4r], [s4r])
                ts(aff_all[:, i, :], ex[:], s4[:, 9:10], None, ALU.mult, ALU.bypass, [exr, s4r], [aff_r])
        run_interleaved([p4_gen(0), p4_gen(1)])
        if "aff" in debug:
            d_ = dbg_out("aff", [SEQ, 16], F32)
            dma("sync", d_.rearrange("(a p) e -> p a e", p=128), aff_all[:], aff_r, reads=[aff_r])
        S.flush()

    idxu = glob.enter_context(nc.sbuf_tensor(U("idxu"), [128, 16, 4], U32))
    gate = glob.enter_context(nc.sbuf_tensor(U("gate"), [128, 16, 4], F32))
    route_r = S.res("route")
    wstack = ExitStack()
    NEXP = 16 if KLIM > 100 else 0
    wbuf = [[(wstack.enter_context(nc.sbuf_tensor(U("wexp"), [128, 8, D], BF16)), S.res("wexp")) for _ in range(3)]
            for _ in range(2)]
    cast_engs = ["scalar", "vector", "scalar", "vector"]
    ncast = [0]

    def load_expert(e_):
        for wi, wsrc in enumerate((w_gate, w_up, w_down)):
            wt, wtr = wbuf[e_ % 2][wi]
            v = wsrc[e_].rearrange("(k p) n -> p k n", p=128)
            for kh in range(2):
                dma("gpsimd", wt[:, kh * 4:(kh + 1) * 4, :], v[:, kh * 4:(kh + 1) * 4, :], wtr, writes=[wtr])


    with ExitStack() as st:
        c5 = S.res("c5")

        def cl5(name, shape, dt, src_ap, cast_from=None):
            t = st.enter_context(nc.sbuf_tensor(U(name), shape, dt))
            r_ = S.res(name)
            if cast_from is None:
                dma("gpsimd", t[:], src_ap, r_, writes=[r_, c5])
                return t
            tf = st.enter_context(nc.sbuf_tensor(U(name + "f"), shape, cast_from))
            dma("gpsimd", tf[:], src_ap, r_, writes=[r_])
            cp(t[:], tf[:], [r_], [c5])
            return t

        onesb = cl5("onesb", [128, 128], BF16, ones_d, F32)
        sutb = cl5("sutb", [128, 128], BF16, tri_d[0], F32)
        iota = cl5("iota", [128, 512], F32, iota_d)
        rm16 = cl5("rm16", [128, 512], F32, rm16_d)
        tidc = cl5("tidc", [128, 32, 2], F32, tid_d.rearrange("p (a c) -> p a c", c=2))

        def T5(name, shape, dt):
            return st.enter_context(nc.sbuf_tensor(U(name), shape, dt)), S.res(name)

        lo, lo_r = T5("lo", [128, 16], F32)
        cand, cand_r = T5("cand", [128, 16], F32)
        cmpb, cmp_r = T5("cmpb", [128, 32, 16], BF16)
        cnt, cnt_r = T5("cnt", [128, 16], F32)
        incr, incr_r = T5("incr", [128, 16], F32)
        maskf, maskf_r = T5("maskf", [128, 32, 16], F32)
        cs5, cs5_r = T5("cs5", [128, 16, 32], F32)
        inc5, inc5_r = T5("inc5", [128, 16, 32], F32)
        pos, pos_r = T5("pos", [128, 32, 16], F32)
        pos2 = st.enter_context(nc.sbuf_tensor(U("pos2"), [128, 32, 16], F32))
        iotab = st.enter_context(nc.sbuf_tensor(U("iotab"), [128, 256], BF16))
        tv, tv_r = T5("tv", [128, 32, 16, 5], BF16)
        tmp5, tmp5_r = T5("tmp5", [128, 32, 16], F32)
        a1, a1_r = T5("a1", [128, 32, 16], F32)
        racc, racc_r = T5("racc", [128, 16, 4, 5], F32)
        idxf, idxf_r = T5("idxf", [128, 16, 4], F32)
        selp = Pool(S, st, "sel", [128, 512], BF16, 4)
        pcp = Pool(S, st, "pc5", [128, 512], F32, 2, psum=True)
        pap = Pool(S, st, "pa5", [128, 512], F32, 4, psum=True)
        S.op("vector", lambda e: e.memset(lo[:], 0.0), writes=[lo_r])
        for e0_ in range(min(2, NEXP)):
            load_expert(e0_)
        flat5 = lambda t_: t_[:].rearrange("p a e -> p (a e)")
        for it in range(27):
            cst = float(2.0 ** -(it + 1))
            ts(cand[:], lo[:], cst, None, ALU.add, ALU.bypass, [lo_r], [cand_r])
            tt(cmpb[:], aff_all[:], cand[:].unsqueeze(1).to_broadcast([128, 32, 16]), ALU.is_ge, [aff_r, cand_r],
               [cmp_r])
            pc, pcr = pcp.next()
            mm(pc[:], onesb[:], flat5(cmpb), True, True, [c5, cmp_r], [pcr])
            red(cnt[:], pc[:].rearrange("p (a e) -> p e a", e=16), [pcr], [cnt_r])
            ts(incr[:], cnt[:], 512.0, cst, ALU.is_ge, ALU.mult, [cnt_r], [incr_r])
            tt(lo[:], lo[:], incr[:], ALU.add, [incr_r], [lo_r])
        lob = lo[:].unsqueeze(1).to_broadcast([128, 32, 16])
        tt(cmpb[:], aff_all[:], lob, ALU.is_ge, [aff_r, lo_r], [cmp_r])
        tt(maskf[:], aff_all[:], lob, ALU.is_ge, [aff_r, lo_r], [maskf_r])
        pw, pwr = pcp.next()
        mm(pw[:], sutb[:], flat5(cmpb), True, True, [c5, cmp_r], [pwr])
        pcs, pcsr = pcp.next()
        mm(pcs[:], onesb[:], flat5(cmpb), True, True, [c5, cmp_r], [pcsr])
        cp(cs5[:], pcs[:].rearrange("p (a e) -> p e a", e=16), [pcsr], [cs5_r])
        S.op("vector", lambda e: e.tensor_tensor_scan(out=inc5[:].rearrange("p e a -> p (e a)"), data0=rm16[:],
                                                      data1=cs5[:].rearrange("p e a -> p (e a)"), initial=0.0,
                                                      op0=ALU.mult, op1=ALU.add),
             reads=[c5, cs5_r], writes=[inc5_r])
        tt(inc5[:], inc5[:], cs5[:], ALU.subtract, [cs5_r], [inc5_r])
        tt(pos[:], pw[:].rearrange("p (a e) -> p a e", e=16), inc5[:].rearrange("p e a -> p a e"), ALU.add,
           [pwr, inc5_r], [pos_r])
        ts(tmp5[:], maskf[:], -1.0e4, 1.0e4, ALU.mult, ALU.add, [maskf_r], [tmp5_r])
        tt(pos[:], pos[:], tmp5[:], ALU.add, [tmp5_r], [pos_r])
        ts(pos2[:], pos[:], -256.0, None, ALU.add, ALU.bypass, [pos_r], [pos_r])
        cp(iotab[:], iota[:, 0:256], [c5], [c5])
        for c_ in range(2):
            cp(tv[:, :, :, c_], tidc[:, :, c_:c_ + 1].to_broadcast([128, 32, 16]), [c5], [tv_r])
        cp(tv[:, :, :, 2], aff_all[:], [aff_r], [tv_r])
        tt(a1[:], aff_all[:], tv[:, :, :, 2], ALU.subtract, [aff_r, tv_r], [a1_r])
        cp(tv[:, :, :, 3], a1[:], [a1_r], [tv_r])
        tt(a1[:], a1[:], tv[:, :, :, 3], ALU.subtract, [tv_r], [a1_r])
        cp(tv[:, :, :, 4], a1[:], [a1_r], [tv_r])
        posA = st.enter_context(nc.sbuf_tensor(U("posA"), [128, 32, 16], BF16))
        posB = st.enter_context(nc.sbuf_tensor(U("posB"), [128, 32, 16], BF16))
        pk_r = S.res("poskeys")
        ts(tmp5[:], pos[:], 256.0, None, ALU.is_lt, ALU.bypass, [pos_r], [tmp5_r])
        stt(a1[:], pos[:], 1.0, tmp5[:], ALU.add, ALU.mult, [pos_r, tmp5_r], [a1_r])
        ts(posA[:], a1[:], -1.0, None, ALU.add, ALU.bypass, [a1_r], [pk_r])
        ts(tmp5[:], pos[:], 256.0, None, ALU.is_ge, ALU.bypass, [pos_r], [tmp5_r])
        ts(a1[:], pos[:], 512.0, None, ALU.is_lt, ALU.bypass, [pos_r], [a1_r])
        tt(tmp5[:], tmp5[:], a1[:], ALU.mult, [a1_r], [tmp5_r])
        stt(a1[:], pos[:], -255.0, tmp5[:], ALU.add, ALU.mult, [pos_r, tmp5_r], [a1_r])
        ts(posB[:], a1[:], -1.0, None, ALU.add, ALU.bypass, [a1_r], [pk_r])
        selAp = Pool(S, st, "selA", [128, 32, 256], BF16, 2)
        selBp = Pool(S, st, "selB", [128, 32, 256], BF16, 2)
        iob = iotab[:].unsqueeze(1).to_broadcast([128, 32, 256])
        for e_ in range(16 if KLIM > 100 else 0):
            pa0, par0 = pap.next()
            pa1, par1 = pap.next()
            pav = [pa0[:, 0:320].rearrange("p (j a c) -> p j a c", j=2, a=32),
                   pa1[:, 0:320].rearrange("p (j a c) -> p j a c", j=2, a=32)]
            parr = [par0, par1]
            sA, sAr = selAp.next()
            sB, sBr = selBp.next()
            tt(sA[:], iob, posA[:, :, e_:e_ + 1].to_broadcast([128, 32, 256]), ALU.is_equal, [c5, pk_r], [sAr])
            tt(sB[:], iob, posB[:, :, e_:e_ + 1].to_broadcast([128, 32, 256]), ALU.is_equal, [c5, pk_r], [sBr])
            for a_ in range(32):
                for j in range(4):
                    sel_, selr_ = (sA, sAr) if j < 2 else (sB, sBr)
                    mm(pav[j // 2][:, j % 2, a_, :], sel_[:, a_, (j % 2) * 128:(j % 2 + 1) * 128], tv[:, a_, e_, :],
                       True, True, [selr_, tv_r], [parr[j // 2]])
            for jj in range(2):
                red(racc[:, e_, 2 * jj:2 * jj + 2], pav[jj].rearrange("p j a c -> p j c a"), [parr[jj]], [racc_r])
        stt(idxf[:], racc[:, :, :, 0], 64.0, racc[:, :, :, 1], ALU.mult, ALU.add, [racc_r], [idxf_r])
        cp(idxu[:], idxf[:], [idxf_r], [route_r])
        tt(gate[:], racc[:, :, :, 2], racc[:, :, :, 3], ALU.add, [racc_r], [route_r])
        tt(gate[:], gate[:], racc[:, :, :, 4], ALU.add, [racc_r], [route_r])
        if "route" in debug:
            d_ = dbg_out("idx", [128, 64], U32)
            dma("sync", d_, idxu[:].rearrange("p e j -> p (e j)"), route_r, reads=[route_r])
            d2_ = dbg_out("gate", [128, 64], F32)
            r2_ = S.res("gdbg")
            dma("sync", d2_, gate[:].rearrange("p e j -> p (e j)"), r2_, reads=[route_r])
        S.flush()

    with ExitStack() as st:
        ym_r = S.res("ymoe")
        xsTp = Pool(S, st, "xsT", [128, 8, 512], BF16, 2)
        hidp = Pool(S, st, "hidT", [128, 8, 512], BF16, 1)
        silp = Pool(S, st, "sil", [128, 512], F32, 2)
        yep = Pool(S, st, "ye", [128, D], F32, 2)
        ptx = Pool(S, st, "ptx", [128, 8, 128], BF16, 2, psum=True)
        pgu = Pool(S, st, "pgu", [128, 512], F32, 4, psum=True)
        pdn = Pool(S, st, "pdn", [128, 512], F32, 2, psum=True)
        xs_slots = [(st.enter_context(nc.sbuf_tensor(U("xsg"), [128, 4, D], BF16)), [S.res("xsg") for _ in range(4)])
                    for _ in range(2)]

        def gather(e_):
            xs, xsrs = xs_slots[e_ % 2]
            for j in range(4):
                S.dma("gpsimd", lambda e, xs=xs, j=j, e_=e_: e.indirect_dma_start(
                    out=xs[:, j, :], out_offset=None, in_=h2_d,
                    in_offset=bass.IndirectOffsetOnAxis(ap=idxu[:, e_, j:j + 1], axis=0)),
                    xsrs[j], reads=[route_r], writes=[xsrs[j]])

        if NEXP:
            gather(0)
        for e_ in range(NEXP):
            if e_ + 1 < NEXP:
                if e_ + 1 >= 2:
                    load_expert(e_ + 1)
                gather(e_ + 1)
            (wg, wgr), (wu, wur), (wd, wdr) = wbuf[e_ % 2]
            xs, xsrs = xs_slots[e_ % 2]
            xsT, xsTr = xsTp.next()
            for j in range(4):
                pt, ptr_ = ptx.next()
                for kc in range(8):
                    tr(pt[:, kc, :], xs[:, j, kc * 128:(kc + 1) * 128], identb[:], [xsrs[j], ident_r], [ptr_])
                act(xsT[:, :, j * 128:(j + 1) * 128], pt[:], AF.Copy, [ptr_], [xsTr])
            hid, hidr = hidp.next()
            for fc in range(8):
                pg_, pgr_ = pgu.next()
                pu_, pur_ = pgu.next()
                for kc in range(8):
                    mm(pg_[:], wg[:, kc, fc * 128:(fc + 1) * 128], xsT[:, kc, :], kc == 0, kc == 7, [wgr, xsTr], [pgr_])
                for kc in range(8):
                    mm(pu_[:], wu[:, kc, fc * 128:(fc + 1) * 128], xsT[:, kc, :], kc == 0, kc == 7, [wur, xsTr], [pur_])
                sl, slr = silp.next()
                act(sl[:], pg_[:], AF.Silu, [pgr_], [slr])
                tt(hid[:, fc, :], pu_[:], sl[:], ALU.mult, [pur_, slr], [hidr])
            for j in range(4):
                ye, yer = yep.next()
                for dh in range(2):
                    pd_, pdr_ = pdn.next()
                    for fc in range(8):
                        mm(pd_[:], hid[:, fc, j * 128:(j + 1) * 128], wd[:, fc, dh * 512:(dh + 1) * 512], fc == 0, fc == 7,
                           [hidr, wdr], [pdr_])
                    if dh == 0:
                        act(ye[:, 0:512], pd_[:], AF.Copy, [pdr_, route_r], [yer], scale=gate[:, e_, j:j + 1])
                    else:
                        ts(ye[:, 512:1024], pd_[:], gate[:, e_, j:j + 1], None, ALU.mult, ALU.bypass, [pdr_, route_r],
                           [yer])
                S.dma("gpsimd", lambda e, ye=ye, j=j, e_=e_: e.indirect_dma_start(
                    out=ymoe_d, out_offset=bass.IndirectOffsetOnAxis(ap=idxu[:, e_, j:j + 1], axis=0),
                    in_=ye[:], in_offset=None, compute_op=ALU.add),
                    yer, reads=[yer, route_r], writes=[ym_r])
        S.flush()

    wstack.close()
    with ExitStack() as st:
        g2row = st.enter_context(nc.sbuf_tensor(U("g2row"), [128, D], F32))
        g2r = S.res("g2row")
        dma("gpsimd", g2row[:], bcast_rows(rowmod_d[3 * D:4 * D], D), g2r, writes=[g2r])
        x1p = Pool(S, st, "x17", [128, D], F32, 3)
        ymp = Pool(S, st, "ym7", [128, D], F32, 3)
        jk7 = Pool(S, st, "jk7", [128, D], BF16, 1)
        t7p = Pool(S, st, "t7", [128, D], F32, 2)
        o7p = Pool(S, st, "o7", [128, D], F32, 3)
        s7p = Pool(S, st, "s7", [128, 4], F32, 3)
        for i in range(NTILE):
            r0 = i * 128
            x1, x1r = x1p.next()
            ym, ymr = ymp.next()
            dma("sync", x1[:], x1_d[r0:r0 + 128, :], x1r, writes=[x1r])
            dma("gpsimd", ym[:], ymoe_d[r0:r0 + 128, :], ymr, writes=[ymr])
            s7, s7r = s7p.next()
            jk, jkr = jk7.next()
            act(jk[:], ym[:], AF.Square, [ymr], [jkr, s7r], accum_out=s7[:, 0:1])
            act(s7[:, 1:2], s7[:, 0:1], AF.Sqrt, [s7r, epsc_r], [s7r], bias=epsc[:], scale=1.0 / D)
            rcp(s7[:, 2:3], s7[:, 1:2], [s7r], [s7r])
            t7, t7r = t7p.next()
            tt(t7[:], ym[:], g2row[:], ALU.mult, [ymr, g2r], [t7r])
            o7, o7r = o7p.next()
            stt(o7[:], t7[:], s7[:, 2:3], x1[:], ALU.mult, ALU.add, [t7r, s7r, x1r], [o7r])
            dma("scalar" if i % 2 == 0 else "sync", out[r0:r0 + 128, :], o7[:], o7r, reads=[o7r])
        S.flush()

    glob.close()
    return nc, dbg


def make_consts():
    c = {}
    c["ident"] = np.eye(128, dtype=np.float32)
    prot = np.zeros((128, 128), np.float32)
    for o in (0, 64):
        for i in range(32):
            prot[o + i + 32, o + i] = -1.0
            prot[o + i, o + 32 + i] = 1.0
    c["prot"] = prot
    t = np.arange(SEQ)
    row = (t // 64).astype(np.float64)
    col = (t % 64).astype(np.float64)
    freq = 10000.0 ** (-np.arange(16, dtype=np.float64) / 16)
    ang = np.concatenate([row[:, None] * freq, col[:, None] * freq], axis=-1)
    cosT = np.ones((128, NT), np.float64)
    sinT = np.zeros((128, NT), np.float64)
    for p in range(128):
        f = p % 32
        cosT[p, LAT0:LAT0 + SEQ] = np.cos(ang[:, f].astype(np.float32).astype(np.float64))
        sinT[p, LAT0:LAT0 + SEQ] = np.sin(ang[:, f].astype(np.float32).astype(np.float64))
    c["cosT"] = cosT.astype(np.float32)
    c["sinT"] = sinT.astype(np.float32)
    lg = np.log1p(-np.exp2(-5.0 - np.arange(8, dtype=np.float64)))
    i = np.arange(128, dtype=np.float64)
    qdec = np.zeros((2, 128, 4, 128))
    kdec = np.zeros((2, 128, 8, 64))
    dmask = np.zeros((2, 128, 8, 128))
    gC = np.zeros((128, 4, 128))
    for h in range(8):
        p, hf = h // 2, h % 2
        qdec[0, hf * 64:(hf + 1) * 64, p, :] = np.exp(lg[h] * (i + 1))[None, :]
        qdec[1, hf * 64:(hf + 1) * 64, p, :] = np.exp(lg[h] * (128 - i))[None, :]
        kdec[0, :, h, :] = 0.125 * np.exp(-lg[h] * (i + 1))[:, None]
        kdec[1, :, h, :] = 0.125 * np.exp(-lg[h] * (128 - i))[:, None]
        jj, ii = np.meshgrid(i, i, indexing="ij")
        dmask[0, :, h, :] = 0.125 * np.where(ii >= jj, np.exp(lg[h] * np.maximum(ii - jj, 0)), 0.0)
        dmask[1, :, h, :] = 0.125 * np.where(jj >= ii, np.exp(lg[h] * np.maximum(jj - ii, 0)), 0.0)
        gC[hf * 64:(hf + 1) * 64, p, hf * 64:(hf + 1) * 64] = np.exp(lg[h] * 128)
    c["qdec"] = qdec.reshape(2, 128, 512).astype(np.float32)
    c["kdec"] = kdec.reshape(2, 128, 512).astype(np.float32)
    c["dmask"] = dmask.reshape(2, 128, 1024).astype(np.float32)
    c["gC"] = gC.reshape(128, 512).astype(np.float32)

    tri = np.zeros((4, 128, 128), np.float32)
    s_, t_ = np.meshgrid(np.arange(128), np.arange(128), indexing="ij")
    tri[0] = (s_ < t_); tri[1] = (s_ <= t_); tri[2] = (s_ > t_); tri[3] = (s_ >= t_)
    c["tri"] = tri
    bd = np.zeros((128, 4, 128), np.float32)
    bd[0:64, :, 0:64] = 1.0
    bd[64:128, :, 64:128] = 1.0
    c["bdmask"] = bd.reshape(128, 512)
    rm = np.ones((128, 4, 128), np.float32)
    rm[:, :, 0] = 0.0
    c["rmask"] = rm.reshape(128, 512)
    ind2 = np.zeros((128, 2), np.float32)
    ind2[0:64, 0] = 1.0
    ind2[64:128, 1] = 1.0
    c["ind2"] = ind2
    bo = np.zeros((128, 128), np.float32)
    bo[0:64, 0:64] = 1.0
    bo[64:128, 64:128] = 1.0
    c["bones"] = bo
    lv = np.zeros((14, 128, 128), np.uint32)
    for L in range(1, 8):
        B = 2 ** L
        hb = B // 2
        mk = (s_ // B == t_ // B) & (s_ % B >= hb) & (t_ % B < hb)
        lv[L - 1] = mk
        lv[7 + L - 1] = mk.T
    c["lvlmask"] = lv
    tid = np.zeros((128, 32, 2), np.float32)
    tok = np.arange(32)[None, :] * 128 + np.arange(128)[:, None]
    tid[:, :, 0] = tok // 64
    tid[:, :, 1] = tok % 64
    c["tidc"] = tid.reshape(128, 64)
    c["iota512"] = np.tile(np.arange(512, dtype=np.float32)[None, :], (128, 1))
    rm = np.ones((128, 16, 32), np.float32)
    rm[:, :, 0] = 0.0
    c["rmask16"] = rm.reshape(128, 512)
    c["onesc"] = np.ones((128, 128), np.float32)
    return c


def make_in_maps(inputs):
    consts = make_consts()
    maps = []
    for b in range(NCORES):
        m = {}
        m["x"] = np.ascontiguousarray(inputs["x"][b])
        m["ctx"] = np.ascontiguousarray(inputs["ctx"][b])
        cc = np.concatenate([inputs["c"][b].reshape(8, 128).T, inputs["c_ctx"].reshape(8, 128).T], axis=1)
        m["ccol"] = np.ascontiguousarray(cc.astype(np.float32))
        m["w_mod"] = np.ascontiguousarray(inputs["w_mod"][0])
        m["b_mod"] = np.ascontiguousarray(inputs["b_mod"][0])
        m["gains"] = np.ascontiguousarray(inputs["norm_gains"][0])
        m["w_in"] = np.ascontiguousarray(inputs["w_in"][0])
        RC_ = 2560
        conv = inputs["rwkv_conv"][0]
        cwT = np.zeros((2, 128, 14, 3), np.float32)
        for dr in range(2):
            cols = list(range(0, 1536)) + list(range(1536 + 64 * dr, 1600 + 64 * dr)) + list(range(1664, 1728)) + \
                list(range(1728 + 128 * dr, 1856 + 128 * dr))
            cwT[dr] = conv[:, cols].T.reshape(14, 128, 3).transpose(1, 0, 2)
        m["cwT"] = np.ascontiguousarray(cwT.reshape(2, 128, 42))
        w2pad = np.zeros((2, 128, 512), np.float32)
        w2pad[:, 0:64, :] = inputs["rwkv_w2"][0]
        m["w2pad"] = w2pad
        a2pad = np.zeros((128, 512), np.float32)
        a2pad[64:128, :] = inputs["rwkv_a2"][0]
        m["a2pad"] = a2pad
        m["g2"] = np.ascontiguousarray(inputs["rwkv_g2"][0])
        colv = lambda v: v.reshape(4, 128).T
        rwcols = np.zeros((128, 4, 8), np.float32)
        rwcols[:, :, 0] = colv(inputs["rwkv_w0"][0, 0])
        rwcols[:, :, 1] = colv(inputs["rwkv_w0"][0, 1])
        rwcols[:, :, 2] = colv(inputs["rwkv_a0"][0])
        rwcols[:, :, 3] = colv(inputs["rwkv_k_k"][0])
        rwcols[:, :, 4] = colv(inputs["rwkv_k_a"][0])
        rwcols[:, :, 5] = colv(inputs["rwkv_r_k"][0])
        m["rwcols"] = np.ascontiguousarray(rwcols.reshape(128, 32))
        m["w_out"] = np.ascontiguousarray(inputs["w_out"][0])
        m["w_router"] = np.ascontiguousarray(inputs["w_router"][0])
        m["w_gate"] = np.ascontiguousarray(inputs["w_gate"][0])
        m["w_up"] = np.ascontiguousarray(inputs["w_up"][0])
        m["w_down"] = np.ascontiguousarray(inputs["w_down"][0])
        m["lnx"] = np.ascontiguousarray(np.stack([inputs["rwkv_lnx_w"][0], inputs["rwkv_lnx_b"][0]], axis=0))
        m.update(consts)
        maps.append(m)
    return maps


def kernel(**inputs):
    inputs = {k: np.asarray(v) for k, v in inputs.items()}
    nc, _ = build()
    maps = make_in_maps(inputs)
    res = run_bass_kernel_spmd(nc, maps, core_ids=list(range(NCORES)))
    return np.stack([r["out"] for r in res.results], axis=0).astype(np.float32)
```

```python
import numpy as np
from contextlib import ExitStack
import concourse.bass as bass
import concourse.mybir as mybir
from concourse.bass_utils import run_bass_kernel_spmd

F32 = mybir.dt.float32
BF16 = mybir.dt.bfloat16
U32 = mybir.dt.uint32
I32 = mybir.dt.int32
AF = mybir.ActivationFunctionType
ALU = mybir.AluOpType
AX = mybir.AxisListType

D = 1024
SEQ = 4096
CTX = 256
NCORES = 8
CTX0 = 1
LAT0 = 259
NT = 4356
EPS = 1e-6
import os
KLIM = int(os.environ.get('KLIM', '999'))
KW = int(os.environ.get('KW', '4'))
KRW = int(os.environ.get('KRW', '1'))
KSKEW_RET = int(os.environ.get('KSKEW_RET', '0'))
KSKEW_RW = int(os.environ.get('KSKEW_RW', '0'))
KSTOP = float(os.environ.get('KSTOP', '99'))
KC = int(os.environ.get('KC', '9'))


_UID = [0]


def U(name):
    _UID[0] += 1
    return "%s_u%d" % (name, _UID[0])


class Res:
    __slots__ = ("name", "w", "rs", "dsem")

    def __init__(self, name=""):
        self.name = name
        self.w = None
        self.rs = {}
        self.dsem = None


class Sched:
    ENG = ["tensor", "vector", "scalar", "gpsimd", "sync"]

    def __init__(self, nc):
        self.nc = nc
        self.sem = {n: nc.alloc_semaphore("sem_" + n) for n in self.ENG}
        self.cnt = {n: 0 for n in self.ENG}
        self.waited = {n: {} for n in self.ENG}
        self.ops = {n: [] for n in self.ENG}
        self.pending = {n: {} for n in self.ENG}
        self.dfree = []
        self.dcnt = {}
        self.dres = []
        self.allres = []
        self.nwaits = 0
        self.nops = 0
        self.mute = False

    def stage(self, n):
        self.mute = n > KSTOP

    def res(self, name=""):
        r = Res(name)
        self.allres.append(r)
        return r

    def _need(self, eng, reads, writes):
        evs = []
        for r in reads:
            if r.w is not None:
                evs.append(r.w)
        for w in writes:
            if w.w is not None:
                evs.append(w.w)
            evs.extend(w.rs.values())
        need = {}
        own = self.sem[eng].num
        wd = self.waited[eng]
        for (s, v) in evs:
            if eng == "tensor" and s.num == own:
                continue
            if wd.get(s.num, 0) >= v:
                continue
            if s.num not in need or need[s.num][1] < v:
                need[s.num] = (s, v)
        for k, (s, v) in need.items():
            wd[k] = v
        self.nwaits += len(need)
        return list(need.values())

    def _mark(self, ev, reads, writes):
        for r in reads:
            if r in writes:
                continue
            k = ev[0].num
            if k not in r.rs or r.rs[k][1] < ev[1]:
                r.rs[k] = ev
        for w in writes:
            w.w = ev
            w.rs = {}

    def op(self, eng, fn, reads=(), writes=()):
        if self.mute:
            return
        need = self._need(eng, reads, writes)
        self.cnt[eng] += 1
        sem = self.sem[eng]
        ev = (sem, self.cnt[eng])
        self._mark(ev, reads, writes)
        self.nops += 1

        def emit(e, need=need, fn=fn, sem=sem):
            for (s, v) in need:
                e.wait_ge(s, v)
            fn(e).then_inc(sem, 1)

        self.ops[eng].append(emit)

    def dma(self, eng, fn, sres, reads=(), writes=()):
        if self.mute:
            return
        need = self._need(eng, reads, writes)
        if sres.dsem is None:
            if self.dfree:
                sres.dsem = self.dfree.pop()
            else:
                sres.dsem = self.nc.alloc_semaphore("dsem%d" % len(self.dcnt))
                self.dcnt[sres.dsem.num] = 0
            self.dres.append(sres)
        ds = sres.dsem
        self.dcnt[ds.num] += 16
        ev = (ds, self.dcnt[ds.num])
        self._mark(ev, reads, writes)
        self.pending[eng][ds.num] = ev
        self.nops += 1

        def emit(e, need=need, fn=fn, ds=ds):
            for (s, v) in need:
                e.wait_ge(s, v)
            fn(e).then_inc(ds, 16)

        self.ops[eng].append(emit)

    def flush(self):
        nc = self.nc
        self.mute = False
        for n in self.ENG:
            pend = list(self.pending[n].values())
            if pend:
                def emit(e, pend=pend):
                    for (s, v) in pend:
                        e.wait_ge(s, v)
                self.ops[n].append(emit)
            self.pending[n] = {}
        with nc.Block() as block:
            for n in self.ENG:
                ops = self.ops[n]
                if ops:
                    def body(e, ops=ops):
                        for o in ops:
                            o(e)
                    getattr(block, n)(body)
        self.ops = {n: [] for n in self.ENG}
        for r in self.dres:
            self.dfree.append(r.dsem)
            r.dsem = None
        self.dres = []
        for r in self.allres:
            r.w = None
            r.rs = {}
        self.allres = [r for r in self.allres]


class Pool:
    def __init__(self, S, stack, name, shape, dtype, n, psum=False):
        self.tiles = []
        for i in range(n):
            if psum:
                t = stack.enter_context(S.nc.psum_tensor(U("%s%d") % (name, i), shape, dtype))
            else:
                t = stack.enter_context(S.nc.sbuf_tensor(U("%s%d") % (name, i), shape, dtype))
            self.tiles.append((t, S.res("%s%d" % (name, i))))
        self.i = 0

    def next(self):
        t = self.tiles[self.i % len(self.tiles)]
        self.i += 1
        return t


def mk_helpers(S):
    def mm(out, lhsT, rhs, start, stop, reads, writes):
        S.op("tensor", lambda e: e.matmul(out, lhsT=lhsT, rhs=rhs, start=start, stop=stop), reads=reads, writes=writes)

    def tr(out, in_, ident, reads, writes):
        S.op("tensor", lambda e: e.transpose(out=out, in_=in_, identity=ident), reads=reads, writes=writes)

    def act(out, in_, func, reads, writes, **kw):
        S.op("scalar", lambda e: e.activation(out=out, in_=in_, func=func, **kw), reads=reads, writes=writes)

    def tt(out, in0, in1, op, reads, writes, eng="vector"):
        S.op(eng, lambda e: e.tensor_tensor(out=out, in0=in0, in1=in1, op=op), reads=reads, writes=writes)

    def ts(out, in0, s1, s2, op0, op1, reads, writes, eng="vector"):
        S.op(eng, lambda e: e.tensor_scalar(out=out, in0=in0, scalar1=s1, scalar2=s2, op0=op0, op1=op1),
             reads=reads, writes=writes)

    def stt(out, in0, scalar, in1, op0, op1, reads, writes):
        S.op("vector", lambda e: e.scalar_tensor_tensor(out=out, in0=in0, scalar=scalar, in1=in1, op0=op0, op1=op1),
             reads=reads, writes=writes)

    def cp(out, in_, reads, writes, eng="vector"):
        S.op(eng, lambda e: e.tensor_copy(out=out, in_=in_), reads=reads, writes=writes)

    def red(out, in_, reads, writes, op=ALU.add):
        S.op("vector", lambda e: e.tensor_reduce(out=out, in_=in_, axis=AX.X, op=op), reads=reads, writes=writes)

    def rcp(out, in_, reads, writes):
        S.op("vector", lambda e: e.reciprocal(out=out, in_=in_), reads=reads, writes=writes)

    def dma(eng, out, in_, sres, reads=(), writes=()):
        S.dma(eng, lambda e: e.dma_start(out=out, in_=in_), sres, reads=reads, writes=writes)

    return mm, tr, act, tt, ts, stt, cp, red, rcp, dma


def bcast_rows(dram_ap_1d, n, parts=128):
    return bass.AP(dram_ap_1d.tensor, dram_ap_1d.offset, [[0, parts], [1, n]])


def build(debug=()):
    nc = bass.Bass("TRN2", target_bir_lowering=False)
    S = Sched(nc)
    dbg = {}

    def din(name, shape, dt=F32):
        return nc.dram_tensor(name, list(shape), dt, kind="ExternalInput").ap()

    x = din("x", [SEQ, D])
    ctx = din("ctx", [CTX, D])
    ccol = din("ccol", [128, 16])
    w_mod = din("w_mod", [D, 6 * D])
    b_mod = din("b_mod", [6 * D])
    gains = din("gains", [4, D])
    ident_d = din("ident", [128, 128])
    out = nc.dram_tensor("out", [SEQ, D], F32, kind="ExternalOutput").ap()

    def dbg_out(name, shape, dt=F32):
        t = nc.dram_tensor("dbg_" + name, list(shape), dt, kind="ExternalOutput").ap()
        dbg[name] = t
        return t

    glob = ExitStack()
    ident = glob.enter_context(nc.sbuf_tensor(U("ident_sb"), [128, 128], F32))
    identb = glob.enter_context(nc.sbuf_tensor(U("identb_sb"), [128, 128], BF16))
    ident_r = S.res("ident")
    rowmod_d = nc.dram_tensor("rowmod_d", [4 * D], F32, kind="Internal").ap()
    colmod = glob.enter_context(nc.sbuf_tensor(U("colmod"), [128, 4, 8], F32))
    colmod_r = S.res("colmod")
    epsc = glob.enter_context(nc.sbuf_tensor(U("epsc"), [128, 1], F32))
    epsc_r = S.res("epsc")
    aff_all = glob.enter_context(nc.sbuf_tensor(U("aff_all"), [128, 32, 16], F32))
    aff_r = S.res("aff_all")
    if "hT" in debug:
        hT_d = dbg_out("hT", [128, 8, NT], BF16)
    else:
        hT_d = nc.dram_tensor("hT_scr", [128, 8, NT], BF16, kind="Internal").ap()

    def run_interleaved(gens, skew=0):
        gens = list(gens)
        next(gens[0])
        for g in gens[1:]:
            next(g)
        for _ in range(skew):
            try:
                next(gens[-1])
            except StopIteration:
                break
        while gens:
            alive = []
            for g in gens:
                try:
                    next(g)
                    alive.append(g)
                except StopIteration:
                    pass
            gens = alive

    with ExitStack() as st:
        rowmod = st.enter_context(nc.sbuf_tensor(U("rowmod"), [128, 4, D], F32))
        rowmod_r = S.res("rowmod")
        csb = st.enter_context(nc.sbuf_tensor(U("csb"), [128, 16], F32))
        csil = st.enter_context(nc.sbuf_tensor(U("csil"), [128, 16], F32))
        cbc = st.enter_context(nc.sbuf_tensor(U("cbc"), [128, 16, 128], F32))
        c_r = S.res("c")
        gb = st.enter_context(nc.sbuf_tensor(U("gb"), [128, 4, D], F32))
        gb_r = S.res("gb")
        bmb = st.enter_context(nc.sbuf_tensor(U("bmb"), [128, 6 * D], F32))
        bmb_r = S.res("bmb")
        rowc = st.enter_context(nc.sbuf_tensor(U("rowc"), [128, 6 * D], F32))
        rowx = st.enter_context(nc.sbuf_tensor(U("rowx"), [128, 2 * D], F32))
        row_r = S.res("row")
        gcol = st.enter_context(nc.sbuf_tensor(U("gcol"), [128, 8], F32))
        junk = st.enter_context(nc.sbuf_tensor(U("junk0"), [128, 128], F32))
        junk_r = S.res("junk0")
        wpool = Pool(S, st, "wmod", [128, 8, 512], F32, 2)
        pp = Pool(S, st, "ps0", [128, 512], F32, 2, psum=True)

        S.dma("sync", lambda e: e.dma_start(out=csb[:], in_=ccol), c_r, writes=[c_r])
        S.dma("sync", lambda e: e.dma_start(out=ident[:], in_=ident_d), ident_r, writes=[ident_r])
        S.op("vector", lambda e: e.tensor_copy(out=identb[:], in_=ident[:]), reads=[ident_r], writes=[ident_r])
        S.op("vector", lambda e: e.memset(epsc[:], EPS), writes=[epsc_r])
        S.dma("gpsimd", lambda e: e.dma_start(out=gb[:].rearrange("p a d -> p (a d)"),
                                               in_=bcast_rows(gains.rearrange("a d -> (a d)"), 4 * D)),
              gb_r, writes=[gb_r])
        S.dma("gpsimd", lambda e: e.dma_start(out=bmb[:], in_=bcast_rows(b_mod, 6 * D)), bmb_r, writes=[bmb_r])
        S.op("scalar", lambda e: e.activation(out=csil[:], in_=csb[:], func=AF.Silu), reads=[c_r], writes=[c_r])
        S.op("vector", lambda e: e.tensor_copy(out=cbc[:], in_=csil[:].unsqueeze(2).to_broadcast([128, 16, 128])),
             reads=[c_r], writes=[c_r])
        wv = w_mod.rearrange("(k p) n -> p k n", p=128)
        for blk in range(12):
            wt, wr = wpool.next()
            S.dma("sync", lambda e, wt=wt, blk=blk: e.dma_start(out=wt[:], in_=wv[:, :, blk * 512:(blk + 1) * 512]),
                  wr, writes=[wr])
            for which in range(2 if blk < 4 else 1):
                pt, pr = pp.next()
                for k in range(8):
                    S.op("tensor", lambda e, pt=pt, wt=wt, k=k, which=which: e.matmul(
                        pt[:], lhsT=cbc[:, which * 8 + k, :], rhs=wt[:, k, :], start=(k == 0), stop=(k == 7)),
                        reads=[c_r, wr], writes=[pr])
                dst = rowc if which == 0 else rowx
                S.op("vector", lambda e, pt=pt, dst=dst, blk=blk: e.tensor_tensor(
                    out=dst[:, blk * 512:(blk + 1) * 512], in0=pt[:], in1=bmb[:, blk * 512:(blk + 1) * 512],
                    op=ALU.add), reads=[pr, bmb_r], writes=[row_r])
        S.op("vector", lambda e: e.tensor_tensor(out=rowmod[:, 0, :], in0=rowc[:, 2 * D:3 * D], in1=gb[:, 1, :],
                                                 op=ALU.mult), reads=[row_r, gb_r], writes=[rowmod_r])
        S.op("vector", lambda e: e.scalar_tensor_tensor(out=rowmod[:, 1, :], in0=rowc[:, 4 * D:5 * D], scalar=1.0,
                                                        in1=gb[:, 2, :], op0=ALU.add, op1=ALU.mult),
             reads=[row_r, gb_r], writes=[rowmod_r])
        S.op("vector", lambda e: e.tensor_copy(out=rowmod[:, 2, :], in_=rowc[:, 3 * D:4 * D]),
             reads=[row_r], writes=[rowmod_r])
        S.op("vector", lambda e: e.tensor_tensor(out=rowmod[:, 3, :], in0=rowc[:, 5 * D:6 * D], in1=gb[:, 3, :],
                                                 op=ALU.mult), reads=[row_r, gb_r], writes=[rowmod_r])
        S.dma("sync", lambda e: e.dma_start(out=bass.AP(rowmod_d.tensor, 0, [[0, 1], [1, 4 * D]]),
                                            in_=rowmod[0:1].rearrange("p a d -> p (a d)")), rowmod_r, reads=[rowmod_r])
        for src in (rowc, rowx):
            S.op("vector", lambda e, src=src: e.scalar_tensor_tensor(
                out=src[:, D:2 * D], in0=src[:, D:2 * D], scalar=1.0, in1=gb[:, 0, :], op0=ALU.add, op1=ALU.mult),
                reads=[gb_r], writes=[row_r])
        for ci, (src, off) in enumerate([(rowc, D), (rowc, 0), (rowx, D), (rowx, 0)]):
            for j in range(8):
                S.op("vector", lambda e, src=src, off=off, j=j: e.tensor_tensor(
                    out=junk[:], in0=src[:, off + j * 128: off + (j + 1) * 128], in1=ident[:], op=ALU.mult),
                    reads=[row_r, ident_r], writes=[junk_r])
                S.op("vector", lambda e, j=j, ci=ci: e.tensor_reduce(
                    out=colmod[:, ci, j:j + 1], in_=junk[:], axis=AX.X, op=ALU.add),
                    reads=[junk_r], writes=[colmod_r])
        S.flush()

    mm, tr, act, tt, ts, stt, cp, red, rcp, dma = mk_helpers(S)
    w_in = din("w_in", [D, 4544])
    prot_d = din("prot", [128, 128])
    cosT_d = din("cosT", [128, NT])
    sinT_d = din("sinT", [128, NT])
    qdec_d = din("qdec", [2, 128, 512])
    kdec_d = din("kdec", [2, 128, 512])
    dmask_d = din("dmask", [2, 128, 1024])
    gC_d = din("gC", [128, 512])
    if "lat_f" in debug:
        lat_d = [dbg_out("lat_f", [SEQ, D], BF16), dbg_out("lat_b", [SEQ, D], BF16)]
    else:
        lat_d = [nc.dram_tensor("lat_f", [SEQ, D], BF16, kind="Internal").ap(),
                 nc.dram_tensor("lat_b", [SEQ, D], BF16, kind="Internal").ap()]

    def load_w_bf16(st_pool, dst, dst_r, src_ap, ncols):
        v = src_ap.rearrange("(k p) n -> p k n", p=128)
        for c0 in range(0, ncols, 256):
            cw = min(256, ncols - c0)
            stg, sr = st_pool.next()
            dma("sync", stg[:, :, :cw], v[:, :, c0:c0 + cw], sr, writes=[sr])
            act(dst[:, :, c0:c0 + cw], stg[:, :, :cw], AF.Copy, [sr], [dst_r])


    RC = 2560
    rw_wstack = ExitStack()
    Wrw = rw_wstack.enter_context(nc.sbuf_tensor(U("Wrw"), [128, 8, 1536 + 512], BF16))
    Wrw_r = S.res("Wrw")
    ret_wstack = ExitStack()
    wsb = ret_wstack.enter_context(nc.sbuf_tensor(U("ret_w"), [128, 5, 8, 512], BF16))
    wsb_r = S.res("ret_w")

    with ExitStack() as st:
        stg_pool1 = Pool(S, st, "wstg", [128, 8, 256], F32, 2)
        pf1 = []
        for wi, c0w in enumerate([0, 512, 1024, 1536, 2048]):
            for hh_ in range(2):
                pf1.append((wi, c0w, hh_))

        def prefetch1():
            if not pf1:
                return
            wi, c0w, hh_ = pf1.pop(0)
            stg, sr = stg_pool1.next()
            vv = w_in[:, c0w + hh_ * 256:c0w + hh_ * 256 + 256].rearrange("(k p) n -> p k n", p=128)
            dma("gpsimd", stg[:], vv, sr, writes=[sr])
            cp(wsb[:, wi, :, hh_ * 256:(hh_ + 1) * 256], stg[:], [sr], [wsb_r])
        zp = st.enter_context(nc.sbuf_tensor(U("zpad"), [128, 8, 2], BF16))
        zp_r = S.res("zpad")
        S.op("gpsimd", lambda e: e.memset(zp[:], 0.0), writes=[zp_r])
        with nc.allow_non_contiguous_dma(reason="tiny zero pad columns"):
            pass
        for col in (0, CTX0 + CTX, CTX0 + CTX + 1, NT - 1):
            S.dma("gpsimd", lambda e, col=col: e.dma_start(out=hT_d[:, :, col:col + 1], in_=zp[:, :, 0:1],
                                                           allow_slow_non_contiguous=True), zp_r, reads=[zp_r])
        tiles = [(ctx, i, CTX0 + i * 128, 2) for i in range(CTX // 128)] + \
                [(x, i, LAT0 + i * 128, 0) for i in range(SEQ // 128)]
        groups = [tiles[0:2]] + [tiles[2 + 4 * g_:2 + 4 * g_ + 4] for g_ in range(SEQ // 512)]

        def p1_gen(par):
            hTp = Pool(S, st, "hTt", [128, 8, 512], BF16, 2)
            xp = Pool(S, st, "xin", [128, D], F32, 2)
            xnp = Pool(S, st, "xn", [128, D], BF16, 1)
            sqp = Pool(S, st, "sqj", [128, D], BF16, 1)
            stp = Pool(S, st, "stat", [128, 4], F32, 2)
            tp = Pool(S, st, "pst", [128, 8, 128], BF16, 1, psum=True)
            yield
            for grp in groups[par::2]:
                for (src, i, c0, ci) in grp:
                    if ci == 0 and i >= 1:
                        prefetch1()
                    xt, xr = xp.next()
                    S.dma("sync", lambda e, xt=xt, src=src, i=i: e.dma_start(out=xt[:], in_=src[i * 128:(i + 1) * 128, :]),
                          xr, writes=[xr])
                    sq, sqr = sqp.next()
                    stq, sr = stp.next()
                    S.op("scalar", lambda e, sq=sq, xt=xt, stq=stq: e.activation(
                        out=sq[:], in_=xt[:], func=AF.Square, accum_out=stq[:, 0:1]), reads=[xr], writes=[sqr, sr])
                    S.op("scalar", lambda e, stq=stq: e.activation(
                        out=stq[:, 1:2], in_=stq[:, 0:1], func=AF.Sqrt, bias=epsc[:], scale=1.0 / D),
                        reads=[sr, epsc_r], writes=[sr])
                    S.op("vector", lambda e, stq=stq: e.reciprocal(out=stq[:, 2:3], in_=stq[:, 1:2]), reads=[sr], writes=[sr])
                    xn, xnr = xnp.next()
                    S.op("scalar", lambda e, xn=xn, xt=xt, stq=stq: e.activation(
                        out=xn[:], in_=xt[:], func=AF.Copy, scale=stq[:, 2:3]), reads=[xr, sr], writes=[xnr])
                    yield
                    pt, pr = tp.next()
                    for j in range(8):
                        S.op("tensor", lambda e, pt=pt, xn=xn, j=j: e.transpose(
                            out=pt[:, j, :], in_=xn[:, j * 128:(j + 1) * 128], identity=identb[:]),
                            reads=[xnr, ident_r], writes=[pr])
                    gsz = 2 if ci == 2 else 4
                    gi = i % gsz
                    if gi == 0:
                        ht, htr = hTp.next()
                    for j in range(8):
                        S.op("vector", lambda e, pt=pt, j=j, ht=ht, ci=ci, gi=gi: e.tensor_scalar(
                            out=ht[:, j, gi * 128:(gi + 1) * 128], in0=pt[:, j, :], scalar1=colmod[:, ci, j:j + 1],
                            scalar2=colmod[:, ci + 1, j:j + 1], op0=ALU.mult, op1=ALU.add),
                            reads=[pr, colmod_r], writes=[htr])
                    if gi == gsz - 1:
                        cb = c0 - gi * 128
                        S.dma("sync", lambda e, ht=ht, cb=cb, gsz=gsz: e.dma_start(
                            out=hT_d[:, :, cb:cb + gsz * 128], in_=ht[:, :, 0:gsz * 128]), htr, reads=[htr])
                    yield

        run_interleaved([p1_gen(0), p1_gen(1)])
        while pf1:
            prefetch1()
        S.flush()

    def dscr(name, shape, dt):
        if name in debug:
            return dbg_out(name, shape, dt)
        return nc.dram_tensor(name + "_scr", list(shape), dt, kind="Internal").ap()

    x1_d = dscr("x1", [SEQ, D], F32)
    h2_d = dscr("h2", [SEQ, D], BF16)
    ymoe_d = dscr("moe", [SEQ, D], F32)
    NTILE = SEQ // 128

    def ret_all():
        with ExitStack() as st:
            stg_pool = Pool(S, st, "wstg2", [128, 8, 256], F32, 2)
            pieces = [(0, RC, 1536)]
            for dr in range(2):
                b0 = 1536 + 256 * dr
                pieces += [(b0, RC + 1536 + 64 * dr, 64), (b0 + 64, RC + 1664, 64), (b0 + 128, RC + 1728 + 128 * dr, 128)]
            vfull = w_in.rearrange("(k p) n -> p k n", p=128)
            pf2 = []
            for (d0, s0, n) in pieces:
                for o in range(0, n, 256):
                    pf2.append((d0 + o, s0 + o, min(256, n - o)))

            def prefetch2():
                if not pf2:
                    return
                dd, ss, cwd = pf2.pop(0)
                stg, sr = stg_pool.next()
                dma("gpsimd", stg[:, :, :cwd], vfull[:, :, ss:ss + cwd], sr, writes=[sr])
                cp(Wrw[:, :, dd:dd + cwd], stg[:, :, :cwd], [sr], [Wrw_r])

            cst_r = S.res("ret_consts")
            protf = st.enter_context(nc.sbuf_tensor(U("protf"), [128, 128], F32))
            prot = st.enter_context(nc.sbuf_tensor(U("prot"), [128, 128], BF16))
            gC = st.enter_context(nc.sbuf_tensor(U("gC"), [128, 512], F32))
            for (dst, srcap) in [(protf[:], prot_d), (gC[:], gC_d)]:
                r_ = S.res("cst")
                dma("gpsimd", dst, srcap, r_, writes=[r_, cst_r])
            cp(prot[:], protf[:], [cst_r], [cst_r])
            zt, zt_r = st.enter_context(nc.sbuf_tensor(U("zt"), [128, D], F32)), S.res("zt")
            S.op("vector", lambda e: e.memset(zt[:], 0.0), writes=[zt_r])
            zf = list(range(NTILE))

            def zerofill():
                if zf:
                    i = zf.pop(0)
                    dma("gpsimd", ymoe_d[i * 128:(i + 1) * 128, :], zt[:], zt_r, reads=[zt_r])

            def ret_gen(dirn):
                csp = Pool(S, st, "cs", [128, 2, 128], F32, 2)
                hsp = Pool(S, st, "hsr", [128, 8, 128], BF16, 2)
                qdec = st.enter_context(nc.sbuf_tensor(U("qdec"), [128, 4, 128], F32))
                kdec = st.enter_context(nc.sbuf_tensor(U("kdec"), [128, 512], F32))
                dmask = st.enter_context(nc.sbuf_tensor(U("dmask"), [128, 8, 128], F32))
                dc_r = S.res("ret_dconsts")
                for (dst, srcap) in [(qdec[:].rearrange("p a t -> p (a t)"), qdec_d[dirn]), (kdec[:], kdec_d[dirn]),
                                     (dmask[:].rearrange("p a t -> p (a t)"), dmask_d[dirn])]:
                    r_ = S.res("cst")
                    dma("gpsimd", dst, srcap, r_, writes=[r_, dc_r])
                S32 = st.enter_context(nc.sbuf_tensor(U("S32"), [128, 512], F32))
                S16 = st.enter_context(nc.sbuf_tensor(U("S16"), [128, 512], BF16))
                S_r = S.res("S")
                S16_r = S.res("S16")
                S.op("vector", lambda e: e.memset(S32[:], 0.0), writes=[S_r])
                S.op("vector", lambda e: e.memset(S16[:], 0.0), writes=[S16_r])
                PA = Pool(S, st, "retPA", [128, 512], F32, 2, psum=True)
                PYp = Pool(S, st, "retPY", [128, 512], F32, 1, psum=True)
                qk_sb = Pool(S, st, "qk_sb", [128, 4, 128], BF16, 2)
                t1p = Pool(S, st, "t1", [128, 4, 128], F32, 1)
                t2p = Pool(S, st, "t2", [128, 4, 128], F32, 1)
                krp = Pool(S, st, "kr", [128, 3, 4, 128], BF16, 2)
                for (t_, r_) in krp.tiles:
                    S.op("gpsimd", lambda e, t_=t_: e.memset(t_[:], 0.0), writes=[r_])
                qrp = Pool(S, st, "qr", [128, 4, 128], BF16, 2)
                qpp = Pool(S, st, "qp", [128, 4, 128], BF16, 2)
                kptp = Pool(S, st, "kpt", [128, 512], BF16, 2)
                vsp = Pool(S, st, "vsb", [128, 512], BF16, 2)
                sTp = Pool(S, st, "sT", [128, 8, 128], BF16, 2)
                sqp2 = Pool(S, st, "sq2", [128, 8, 64], F32, 1)
                ynp = Pool(S, st, "yn", [128, 8, 64], F32, 1)
                sgp = Pool(S, st, "sg", [128, 512], F32, 2)
                latp = Pool(S, st, "lat", [128, 512], BF16, 2)
                st8 = Pool(S, st, "st8", [128, 3, 8], F32, 2)
                yield

                def proj_feat(wi, hs, hsr):
                    pt, pr = PA.next()
                    for p in range(4):
                        for kc in range(8):
                            mm(pt[:, p * 128:(p + 1) * 128], wsb[:, wi, kc, p * 128:(p + 1) * 128], hs[:, kc, :],
                               kc == 0, kc == 7, [wsb_r, hsr], [pr])
                    return pt, pr

                def proj_tok(wi, hs, hsr):
                    pt, pr = PA.next()
                    for kc in range(8):
                        mm(pt[:], hs[:, kc, :], wsb[:, wi, kc, :], kc == 0, kc == 7, [wsb_r, hsr], [pr])
                    return pt, pr

                def rope(wi, hs, hsr, outp, cs, csr):
                    pt, pr = proj_feat(wi, hs, hsr)
                    sb, sbr = qk_sb.next()
                    act(sb[:].rearrange("p a t -> p (a t)"), pt[:], AF.Copy, [pr], [sbr])
                    yield
                    rt_, rr = PA.next()
                    mm(rt_[:], prot[:], sb[:].rearrange("p a t -> p (a t)"), True, True, [cst_r, sbr], [rr])
                    t1, t1r = t1p.next()
                    t2, t2r = t2p.next()
                    cosb = cs[:, 0:1, :].to_broadcast([128, 4, 128])
                    sinb = cs[:, 1:2, :].to_broadcast([128, 4, 128])
                    tt(t1[:], sb[:], cosb, ALU.mult, [sbr, csr], [t1r])
                    yield
                    tt(t2[:], rt_[:].rearrange("p (a t) -> p a t", a=4), sinb, ALU.mult, [rr, csr], [t2r])
                    o, orr = outp.next()
                    if wi == 1:
                        tt(o[:, 0], t1[:], t2[:], ALU.add, [t1r, t2r], [orr])
                        act(o[0:64, 1], o[0:64, 0], AF.Copy, [], [orr])
                        act(o[64:128, 2], o[64:128, 0], AF.Copy, [], [orr])
                    else:
                        tt(o[:], t1[:], t2[:], ALU.add, [t1r, t2r], [orr])
                    yield
                    return o, orr

                PKV = Pool(S, st, "retPKV", [128, 512], F32, 1, psum=True)

                def stage_a(is_ctx, ci):
                    c0 = (CTX0 if is_ctx else LAT0) + ci * 128
                    hs, hsr = hsp.next()
                    dma("sync", hs[:], hT_d[:, :, c0:c0 + 128], hsr, writes=[hsr])
                    if dirn == 0:
                        zerofill()
                    else:
                        prefetch2()
                    cs, csr = csp.next()
                    dma("gpsimd", cs[:, 0, :], cosT_d[:, c0:c0 + 128], csr, writes=[csr])
                    dma("gpsimd", cs[:, 1, :], sinT_d[:, c0:c0 + 128], csr, writes=[csr])
                    kr, krr = yield from rope(1, hs, hsr, krp, cs, csr)
                    pk, pkr = PA.next()
                    pkb = pk.bitcast(BF16)
                    for p in range(4):
                        tr(pkb[:, p * 128:(p + 1) * 128], kr[:, 0, p, :], identb[:], [krr, ident_r], [pkr])
                    kpt, kptr = kptp.next()
                    tt(kpt[:], pkb[:, 0:512], kdec[:], ALU.mult, [pkr, dc_r], [kptr])
                    yield
                    pv, pvr = proj_tok(2, hs, hsr)
                    vs, vsr = vsp.next()
                    act(vs[:], pv[:], AF.Copy, [pvr], [vsr])
                    yield
                    ctx_ = dict(is_ctx=is_ctx, ci=ci, kpt=kpt, kptr=kptr, vs=vs, vsr=vsr)
                    if not is_ctx:
                        qr, qrr = yield from rope(0, hs, hsr, qrp, cs, csr)
                        qp, qpr = qpp.next()
                        tt(qp[:], qr[:], qdec[:], ALU.mult, [qrr, dc_r], [qpr])
                        sT, sTr = sTp.next()
                        for b_ in range(2):
                            ps, psr = PA.next()
                            for hh in range(4):
                                h = 4 * b_ + hh
                                p, hf = h // 2, h % 2
                                mm(ps[:, hh * 128:(hh + 1) * 128], kr[:, 1 + hf, p, :], qr[:, p, :], True, True,
                                   [krr, qrr], [psr])
                            tt(sT[:, 4 * b_:4 * b_ + 4, :], ps[:].rearrange("p (a t) -> p a t", a=4),
                               dmask[:, 4 * b_:4 * b_ + 4, :], ALU.mult, [psr, dc_r], [sTr])
                            yield
                        pg, pgr = proj_tok(3 + dirn, hs, hsr)
                        sg, sgr = sgp.next()
                        act(sg[:], pg[:], AF.Silu, [pgr], [sgr])
                        yield
                        ctx_.update(qp=qp, qpr=qpr, sT=sT, sTr=sTr, sg=sg, sgr=sgr)
                    return ctx_

                def stage_b(cx):
                    kpt, kptr, vs, vsr = cx["kpt"], cx["kptr"], cx["vs"], cx["vsr"]
                    if not cx["is_ctx"]:
                        qp, qpr, sT, sTr, sg, sgr, ci = cx["qp"], cx["qpr"], cx["sT"], cx["sTr"], cx["sg"], cx["sgr"], cx["ci"]
                        py, pyr = PYp.next()
                        for p in range(4):
                            mm(py[:, p * 128:(p + 1) * 128], qp[:, p, :], S16[:, p * 128:(p + 1) * 128], True, False,
                               [qpr, S16_r], [pyr])
                            for hf in range(2):
                                h = 2 * p + hf
                                mm(py[:, h * 64:(h + 1) * 64], sT[:, h, :], vs[:, h * 64:(h + 1) * 64], False, hf == 1,
                                   [sTr, vsr], [pyr])
                        yield
                    kv, kvr = PKV.next()
                    for p in range(4):
                        mm(kv[:, p * 128:(p + 1) * 128], kpt[:, p * 128:(p + 1) * 128], vs[:, p * 128:(p + 1) * 128],
                           True, True, [kptr, vsr], [kvr])
                    tt(S32[:], kv[:], S32[:], ALU.add, [kvr], [S_r])
                    tt(S32[:], S32[:], gC[:], ALU.mult, [cst_r], [S_r])
                    act(S16[:], S32[:], AF.Copy, [S_r], [S16_r])
                    yield
                    if not cx["is_ctx"]:
                        sq, sqr = sqp2.next()
                        py3 = py[:].rearrange("p (h e) -> p h e", h=8)
                        act(sq[:], py3, AF.Square, [pyr], [sqr])
                        yield
                        s8, s8r = st8.next()
                        red(s8[:, 0, :], sq[:], [sqr], [s8r])
                        act(s8[:, 1, :], s8[:, 0, :], AF.Sqrt, [s8r, epsc_r], [s8r], bias=epsc[:], scale=1.0 / 64)
                        rcp(s8[:, 2, :], s8[:, 1, :], [s8r], [s8r])
                        yield
                        yn, ynr = ynp.next()
                        tt(yn[:], py3, s8[:, 2, :].unsqueeze(2).to_broadcast([128, 8, 64]), ALU.mult, [pyr, s8r], [ynr])
                        lt, ltr = latp.next()
                        tt(lt[:], yn[:].rearrange("p h e -> p (h e)"), sg[:], ALU.mult, [ynr, sgr], [ltr])
                        dma("sync", lat_d[dirn][ci * 128:(ci + 1) * 128, 0:512], lt[:], ltr, reads=[ltr])
                        yield

                if dirn == 0:
                    order = [(True, 0), (True, 1)] + [(False, i) for i in range(SEQ // 128)]
                else:
                    order = [(True, 1), (True, 0)] + [(False, i) for i in reversed(range(SEQ // 128))]
                order = order[:KLIM]
                cx = yield from stage_a(*order[0])
                for k in range(len(order)):
                    gA = stage_a(*order[k + 1]) if k + 1 < len(order) else None
                    gB = stage_b(cx)
                    nxt = None
                    while gA is not None or gB is not None:
                        if gB is not None:
                            try:
                                next(gB)
                            except StopIteration:
                                gB = None
                        if gA is not None:
                            try:
                                next(gA)
                            except StopIteration as e_:
                                nxt = e_.value
                                gA = None
                        yield
                    cx = nxt

            run_interleaved([ret_gen(0), ret_gen(1)], skew=KSKEW_RET)
            while pf2:
                prefetch2()
            while zf:
                zerofill()
            S.flush()

    if KLIM >= 0:
        ret_all()
    ret_wstack.close()


    cw_d = din("cwT", [2, 128, 14 * 3])
    w2pad_d = din("w2pad", [2, 128, 512])
    a2pad_d = din("a2pad", [128, 512])
    g2_d = din("g2", [2, 128, 512])
    rwcols_d = din("rwcols", [128, 4 * 8])
    lnx_d = din("lnx", [2, 512])
    tri_d = din("tri", [4, 128, 128])
    bdm_d = din("bdmask", [128, 512])
    rmask_d = din("rmask", [128, 512])
    ind2_d = din("ind2", [128, 2])
    bones_d = din("bones", [128, 128])
    lvm_d = din("lvlmask", [14, 128, 128], U32)
    CDEC = float(np.exp(-0.5))
    GN_EPS = 64e-5

    def rwkv_all():
        with ExitStack() as st:
            cst_r = S.res("rw_consts")

            def cload(name, shape, dt, src_ap, cast_from=None):
                t = st.enter_context(nc.sbuf_tensor(U(name), shape, dt))
                r_ = S.res(name)
                if cast_from is None:
                    dma("gpsimd", t[:], src_ap, r_, writes=[r_, cst_r])
                    return t
                with ExitStack() as stc:
                    tf = st.enter_context(nc.sbuf_tensor(U(name + "f"), shape, cast_from))
                    dma("gpsimd", tf[:], src_ap, r_, writes=[r_])
                    cp(t[:], tf[:], [r_], [cst_r])
                return t

            cwl = [cload("cw%d" % d_, [128, 14 * 3], F32, cw_d[d_]) for d_ in range(2)]
            a2p = cload("a2p", [128, 512], BF16, a2pad_d, F32)
            w2pl = [cload("w2p%d" % d_, [128, 512], BF16, w2pad_d[d_], F32) for d_ in range(2)]
            g2sl = [cload("g2s%d" % d_, [128, 512], BF16, g2_d[d_], F32) for d_ in range(2)]
            rwc = cload("rwc", [128, 4, 8], F32, rwcols_d.rearrange("p (a b) -> p a b", a=4))
            lnxw = cload("lnxw", [128, 512], F32, bcast_rows(lnx_d[0], 512))
            lnxb = cload("lnxb", [128, 512], F32, bcast_rows(lnx_d[1], 512))
            tril = [cload("tri%d" % i_, [128, 128], BF16, tri_d[i_], F32) for i_ in range(4)]
            bdm = cload("bdm", [128, 4, 128], F32, bdm_d.rearrange("p (a b) -> p a b", a=4))
            rmask = cload("rmask", [128, 512], F32, rmask_d)
            ind2 = cload("ind2", [128, 2], BF16, ind2_d, F32)
            bones = cload("bones", [128, 128], BF16, bones_d, F32)
            lvm = cload("lvm", [128, 14, 128], U32, lvm_d.rearrange("a p t -> p a t"))
            tinyc = st.enter_context(nc.sbuf_tensor(U("tinyc"), [128, 2], F32))
            S.op("vector", lambda e: e.memset(tinyc[:, 0:1], 1e-24), writes=[cst_r])
            S.op("vector", lambda e: e.memset(tinyc[:, 1:2], GN_EPS), writes=[cst_r])
            omka = st.enter_context(nc.sbuf_tensor(U("omka"), [128, 4, 1], F32))
            ts(omka[:], rwc[:, :, 4:5], -1.0, 1.0, ALU.mult, ALU.add, [cst_r], [cst_r])
            S.flush()
            print("RWKV sbuf remaining after shared", nc.sbuf_bytes_remaining)

            def rwkv_gen(dirn):
                cw, w2p, g2s = cwl[dirn], w2pl[dirn], g2sl[dirn]
                sk = 0 if dirn == 0 else 2
                m_strict, m_incl, m_strictT = tril[sk], tril[sk + 1], tril[2 - sk]
                w0col = lambda blk: rwc[:, blk, dirn:dirn + 1]
                a0col = lambda blk: rwc[:, blk, 2:3]

                def wcols(blk):
                    if blk < 12:
                        return slice(blk * 128, (blk + 1) * 128)
                    o_ = 1536 + 256 * dirn + (blk - 12) * 128
                    return slice(o_, o_ + 128)

                def T(name, shape, dt):
                    return st.enter_context(nc.sbuf_tensor(U(name), shape, dt)), S.res(name)

                hsp = Pool(S, st, "hsw", [128, 8, 130], BF16, 2)
                H32, H32_r = T("H32", [128, 4, 128], F32)
                H16, H16_r = T("H16", [128, 4, 128], BF16)
                S.op("vector", lambda e: e.memset(H32[:], 0.0), writes=[H32_r])
                rw, rw_r = T("rw", [128, 14, 128], F32)
                lr, lr_r = T("lr", [128, 128], BF16)
                sgl, sgl_r = T("sgl", [128, 128], BF16)
                sig, sig_r = T("sig", [128, 4, 128], F32)
                icl, icl_r = T("icl", [128, 4, 128], F32)
                cum, cum_r = T("cum", [128, 4, 128], F32)
                pex, pex_r = T("pex", [128, 4, 128], F32)
                Er, Er_r = T("Er", [128, 512], F32)
                Ea, Ea_r = T("Ea", [128, 512], F32)
                Ek, Ek_r = T("Ek", [128, 512], F32)
                v3 = lambda t_: t_[:].rearrange("p (a t) -> p a t", a=4)
                v8 = lambda t_: t_[:].rearrange("p (h e) -> p h e", h=8)
                WC, WC_r = T("WC", [128, 4, 1], F32)
                kk, kk_r = sig, sig_r
                rn, rn_r = cum, cum_r
                t1, t1_r = pex, pex_r
                htmp, htmp_r = sig, sig_r
                kk2, kk2_r = T("kk2", [128, 4, 128], BF16)
                rkr, rkr_r = T("rkr", [128, 4, 128], BF16)
                rt, rt_r = T("rt", [128, 3, 4, 128], BF16)
                at, at_r = T("at", [128, 3, 4, 128], BF16)
                S.op("gpsimd", lambda e: e.memset(rt[:], 0.0), writes=[rt_r])
                S.op("gpsimd", lambda e: e.memset(at[:], 0.0), writes=[at_r])
                kt, kt_r = T("kt", [128, 4, 128], BF16)
                bt, bt_r = T("bt", [128, 4, 128], BF16)
                vbf, vbf_r = T("vbf", [128, 4, 128], BF16)
                vtok, vtok_r = T("vtok", [128, 512], BF16)
                ktok, ktok_r = T("ktok", [128, 512], BF16)
                btok, btok_r = T("btok", [128, 512], BF16)

                def T2g(name):
                    t_ = st.enter_context(nc.sbuf_tensor(U(name), [128, 8, 128], BF16))
                    return t_, [S.res(name + "0"), S.res(name + "1")]

                N0, N0g = T2g("N0")
                M0, M0g = T2g("M0")
                AakT, AakTg = T2g("AakT")
                ArbT, ArbTg = T2g("ArbT")
                ArkT, ArkTg = T2g("ArkT")
                Pd, Pdg = T2g("Pd")
                PdT, PdTg = T2g("PdT")
                T1s, T1sg = T2g("T1s")
                T2s, T2sg = T2g("T2s")
                rhs_sb, rhs_r = T("rhs_sb", [128, 512], BF16)
                u_sb, u_r = T("u_sb", [128, 512], BF16)
                s8, s8_r = T("s8", [128, 6, 8], F32)
                latp = Pool(S, st, "latr", [128, 512], BF16, 2)
                P1 = Pool(S, st, "P1", [128, 512], F32, 2, psum=True)
                P2 = Pool(S, st, "P2", [128, 4, 128], F32, 2, psum=True)
                print("RWKV sbuf remaining after gen alloc", dirn, nc.sbuf_bytes_remaining)
                yield

                if dirn == 0:
                    order = [(True, 0), (True, 1)] + [(False, i) for i in range(SEQ // 128)]
                else:
                    order = [(True, 1), (True, 0)] + [(False, i) for i in reversed(range(SEQ // 128))]
                ident_b4 = identb[:].unsqueeze(1).to_broadcast([128, 4, 128])
                fM, fN = (0, 1) if dirn == 0 else (1, 0)
                G = lambda t_, g: t_[:, 4 * g:4 * g + 4, :]
                def stage1(is_ctx, ci):
                    c0 = (CTX0 if is_ctx else LAT0) + ci * 128
                    hs, hsr = hsp.next()
                    dma("sync", hs[:], hT_d[:, :, c0 - 1:c0 + 129], hsr, writes=[hsr])
                    for blk in range(14):
                        if is_ctx and blk == 13:
                            continue
                        pt, pr = P1.next()
                        for kc in range(8):
                            mm(pt[:, 0:130], Wrw[:, kc, wcols(blk)], hs[:, kc, :], kc == 0, kc == 7, [Wrw_r, hsr], [pr])
                        act(rw[:, blk, :], pt[:, 1:129], AF.Copy, [pr, cst_r], [rw_r],
                            scale=cw[:, 3 * blk + 1:3 * blk + 2])
                        stt(rw[:, blk, :], pt[:, 0:128], cw[:, 3 * blk:3 * blk + 1], rw[:, blk, :], ALU.mult, ALU.add,
                            [pr, cst_r], [rw_r])
                        stt(rw[:, blk, :], pt[:, 2:130], cw[:, 3 * blk + 2:3 * blk + 3], rw[:, blk, :], ALU.mult,
                            ALU.add, [pr, cst_r], [rw_r])
                        if blk % 2 == 1:
                            yield

                order = order[:KLIM]
                yield from stage1(*order[0])
                for k_, (is_ctx, ci) in enumerate(order):
                    gS1 = [None]

                    def step_s1():
                        if gS1[0] is not None:
                            try:
                                next(gS1[0])
                            except StopIteration:
                                gS1[0] = None
                    rr_, kk_, vv_ = rw[:, 0:4, :], rw[:, 4:8, :], rw[:, 8:12, :]
                    act(lr[0:64, :], rw[0:64, 12, :], AF.Tanh, [rw_r], [lr_r])
                    act(lr[64:128, :], rw[64:128, 12, :], AF.Copy, [rw_r], [lr_r])
                    if not is_ctx:
                        act(sgl[:], rw[:, 13, :], AF.Sigmoid, [rw_r], [sgl_r])
                    act(vbf[:], vv_, AF.Copy, [rw_r], [vbf_r])
                    pz, pzr = P1.next()
                    for blk in range(4):
                        mm(pz[:, blk * 128:(blk + 1) * 128], w2p[:, blk * 128:(blk + 1) * 128], lr[:], True, True,
                           [cst_r, lr_r], [pzr])
                    for blk in range(4):
                        act(sig[:, blk, :], pz[:, blk * 128:(blk + 1) * 128], AF.Sigmoid, [pzr, cst_r], [sig_r],
                            bias=w0col(blk))
                    yield
                    pi_, pir = P1.next()
                    for blk in range(4):
                        mm(pi_[:, blk * 128:(blk + 1) * 128], a2p[:, blk * 128:(blk + 1) * 128], lr[:], True, True,
                           [cst_r, lr_r], [pir])
                    for blk in range(4):
                        act(icl[:, blk, :], pi_[:, blk * 128:(blk + 1) * 128], AF.Sigmoid, [pir, cst_r], [icl_r],
                            bias=a0col(blk))
                    flat = lambda t_: t_[:].rearrange("p a t -> p (a t)")
                    S.op("vector", lambda e: e.tensor_tensor_scan(out=flat(cum), data0=rmask[:], data1=flat(sig),
                                                                  initial=0.0, op0=ALU.mult, op1=ALU.add),
                         reads=[cst_r, sig_r], writes=[cum_r])
                    yield
                    tt(pex[:], cum[:], sig[:], ALU.subtract, [cum_r, sig_r], [pex_r])
                    if dirn == 0:
                        act(v3(Er), cum[:], AF.Exp, [cum_r], [Er_r], scale=-CDEC)
                        act(v3(Ea), pex[:], AF.Exp, [pex_r], [Ea_r], scale=-CDEC)
                        act(v3(Ek), cum[:], AF.Exp, [cum_r], [Ek_r], scale=CDEC)
                    else:
                        act(v3(Er), pex[:], AF.Exp, [pex_r], [Er_r], scale=CDEC)
                        act(v3(Ea), cum[:], AF.Exp, [cum_r], [Ea_r], scale=CDEC)
                        act(v3(Ek), pex[:], AF.Exp, [pex_r], [Ek_r], scale=-CDEC)
                    act(WC[:], cum[:, :, 127:128], AF.Exp, [cum_r], [WC_r], scale=-CDEC)
                    yield
                    tt(kk[:], kk_, rwc[:, :, 3:4].to_broadcast([128, 4, 128]), ALU.mult, [rw_r, cst_r], [kk_r])
                    act(kk2[:], kk[:], AF.Square, [kk_r], [kk2_r])
                    pss, pssr = P1.next()
                    for blk in range(4):
                        mm(pss[:, blk * 128:(blk + 1) * 128], bones[:], kk2[:, blk, :], True, True, [cst_r, kk2_r], [pssr])
                    act(rn[:].rearrange("p a t -> p (a t)"), pss[:], AF.Ln, [pssr, cst_r], [rn_r], bias=tinyc[:, 0:1])
                    act(rn[:], rn[:], AF.Exp, [], [rn_r], scale=-0.5)
                    yield
                    tt(kk[:], kk[:], rn[:], ALU.mult, [rn_r], [kk_r])
                    tt(t1[:], icl[:], rwc[:, :, 4:5].to_broadcast([128, 4, 128]), ALU.mult, [icl_r, cst_r], [t1_r])
                    tt(t1[:], t1[:], omka[:].to_broadcast([128, 4, 128]), ALU.add, [cst_r], [t1_r])
                    tt(t1[:], kk_, t1[:], ALU.mult, [rw_r], [t1_r])
                    tt(icl[:], kk[:], icl[:], ALU.mult, [kk_r], [icl_r])
                    yield
                    if not is_ctx:
                        tt(rn[:], rr_, rwc[:, :, 5:6].to_broadcast([128, 4, 128]), ALU.mult, [rw_r, cst_r], [rn_r])
                        tt(rkr[:], rn[:], t1[:], ALU.mult, [rn_r, t1_r], [rkr_r])
                        tt(rt[:, 0], rr_, v3(Er), ALU.mult, [rw_r, Er_r], [rt_r])
                        act(rt[0:64, 1], rt[0:64, 0], AF.Copy, [], [rt_r])
                        act(rt[64:128, 2], rt[64:128, 0], AF.Copy, [], [rt_r])
                    stt(at[:, 0], kk[:], -1.0, v3(Ea), ALU.mult, ALU.mult, [kk_r, Ea_r], [at_r])
                    act(at[0:64, 1], at[0:64, 0], AF.Copy, [], [at_r])
                    act(at[64:128, 2], at[64:128, 0], AF.Copy, [], [at_r])
                    tt(kt[:], t1[:], v3(Ek), ALU.mult, [t1_r, Ek_r], [kt_r])
                    tt(bt[:], icl[:], v3(Ek), ALU.mult, [icl_r, Ek_r], [bt_r])
                    yield
                    if k_ + 1 < len(order):
                        gS1[0] = stage1(*order[k_ + 1])
                    for (srcT, srcr, dst, dstr) in ((vbf, vbf_r, vtok, vtok_r), (kt, kt_r, ktok, ktok_r),
                                                    (bt, bt_r, btok, btok_r)):
                        pt, pr = P1.next()
                        ptb = pt.bitcast(BF16)
                        for blk in range(4):
                            tr(ptb[:, blk * 128:(blk + 1) * 128], srcT[:, blk, :], identb[:], [srcr, ident_r], [pr])
                        act(dst[:], ptb[:, 0:512], AF.Copy, [pr], [dstr])
                    step_s1()
                    yield

                    def scores(g, lhs, lhs_r, rhsm, rhsm_r, mask, dst, dst_rg, lhs_masked=False):
                        p2, p2r = P2.next()
                        for hh in range(4):
                            h = 4 * g + hh
                            p, hf = h // 2, h % 2
                            if lhs_masked:
                                mm(p2[:, hh, :], lhs[:, 1 + hf, p, :], rhsm[:, p, :], True, True, [lhs_r, rhsm_r], [p2r])
                            else:
                                mm(p2[:, hh, :], lhs[:, p, :], rhsm[:, 1 + hf, p, :], True, True, [lhs_r, rhsm_r], [p2r])
                        tt(G(dst, g), p2[:], mask[:].unsqueeze(1).to_broadcast([128, 4, 128]), ALU.mult,
                           [p2r, cst_r], [dst_rg[g]])

                    for g in range(2):
                        scores(g, bt, bt_r, at, at_r, m_strict, N0, N0g)
                        scores(g, at, at_r, bt, bt_r, m_strictT, M0, M0g, lhs_masked=True)
                        step_s1()
                        yield
                    for g in range(2):
                        scores(g, kt, kt_r, at, at_r, m_strict, AakT, AakTg)
                        if not is_ctx:
                            scores(g, bt, bt_r, rt, rt_r, m_incl, ArbT, ArbTg)
                            scores(g, kt, kt_r, rt, rt_r, m_incl, ArkT, ArkTg)
                        step_s1()
                        yield

                    def pred(dst, dst_rg, g, lvl, form, data_ap, data_r):
                        mk = lvm[:, form * 7 + lvl - 1, :].unsqueeze(1).to_broadcast([128, 4, 128])
                        S.op("vector", lambda e: e.copy_predicated(out=G(dst, g), mask=mk, data=data_ap),
                             reads=[cst_r, data_r], writes=[dst_rg[g]])

                    for g in range(2):
                        cp(G(Pd, g), ident_b4, [ident_r], [Pdg[g]])
                        cp(G(PdT, g), ident_b4, [ident_r], [PdTg[g]])
                        pred(Pd, Pdg, g, 1, fM, G(M0, g), M0g[g])
                        pred(PdT, PdTg, g, 1, fN, G(N0, g), N0g[g])
                    step_s1()
                    yield
                    for lvl in range(2, 8):
                        last = lvl == 7
                        for g in range(2):
                            if not last:
                                pT1, pT1r = P2.next()
                                for hh in range(4):
                                    h = 4 * g + hh
                                    mm(pT1[:, hh, :], N0[:, h, :], Pd[:, h, :], True, True, [N0g[g], Pdg[g]], [pT1r])
                                act(G(T1s, g), pT1[:], AF.Copy, [pT1r], [T1sg[g]])
                            pT2, pT2r = P2.next()
                            for hh in range(4):
                                h = 4 * g + hh
                                mm(pT2[:, hh, :], M0[:, h, :], PdT[:, h, :], True, True, [M0g[g], PdTg[g]], [pT2r])
                            act(G(T2s, g), pT2[:], AF.Copy, [pT2r], [T2sg[g]])
                            step_s1()
                            yield
                            if not last:
                                pX, pXr = P2.next()
                                for hh in range(4):
                                    h = 4 * g + hh
                                    mm(pX[:, hh, :], PdT[:, h, :], T1s[:, h, :], True, True, [PdTg[g], T1sg[g]], [pXr])
                            pXT, pXTr = P2.next()
                            for hh in range(4):
                                h = 4 * g + hh
                                mm(pXT[:, hh, :], Pd[:, h, :], T2s[:, h, :], True, True, [Pdg[g], T2sg[g]], [pXTr])
                            if not last:
                                pred(Pd, Pdg, g, lvl, fM, pX[:], pXr)
                            pred(PdT, PdTg, g, lvl, fN, pXT[:], pXTr)
                            step_s1()
                            yield
                    Q16, Q16g = PdT, PdTg
                    wcb = WC[:].to_broadcast([128, 4, 128])
                    if dirn == 1:
                        tt(H32[:], H32[:], wcb, ALU.mult, [WC_r], [H32_r])
                    act(H16[:], H32[:], AF.Copy, [H32_r], [H16_r])
                    pr_, prr = P1.next()
                    for p in range(4):
                        mm(pr_[:, p * 128:(p + 1) * 128], at[:, 0, p, :], H16[:, p, :], True, False, [at_r, H16_r], [prr])
                        for hf in range(2):
                            h = 2 * p + hf
                            mm(pr_[:, h * 64:(h + 1) * 64], AakT[:, h, :], vtok[:, h * 64:(h + 1) * 64], False, hf == 1,
                               [AakTg[h // 4], vtok_r], [prr])
                    act(rhs_sb[:], pr_[:], AF.Copy, [prr], [rhs_r])
                    step_s1()
                    yield
                    pu, pur = P1.next()
                    for h in range(8):
                        mm(pu[:, h * 64:(h + 1) * 64], Q16[:, h, :], rhs_sb[:, h * 64:(h + 1) * 64], True, True,
                           [Q16g[h // 4], rhs_r], [pur])
                    act(u_sb[:], pu[:], AF.Copy, [pur], [u_r])
                    step_s1()
                    yield
                    if not is_ctx:
                        py_, pyr = P2.next()
                        py = py_[:].rearrange("p a t -> p (a t)")
                        for p in range(4):
                            mm(py[:, p * 128:(p + 1) * 128], rt[:, 0, p, :], H16[:, p, :], True, False, [rt_r, H16_r],
                               [pyr])
                            for hf in range(2):
                                h = 2 * p + hf
                                mm(py[:, h * 64:(h + 1) * 64], ArbT[:, h, :], u_sb[:, h * 64:(h + 1) * 64], False, False,
                                   [ArbTg[h // 4], u_r], [pyr])
                                mm(py[:, h * 64:(h + 1) * 64], ArkT[:, h, :], vtok[:, h * 64:(h + 1) * 64], False,
                                   hf == 1, [ArkTg[h // 4], vtok_r], [pyr])
                    p2h, p2hr = P2.next()
                    for p in range(4):
                        mm(p2h[:, p, :], btok[:, p * 128:(p + 1) * 128], u_sb[:, p * 128:(p + 1) * 128], True, False,
                           [btok_r, u_r], [p2hr])
                        mm(p2h[:, p, :], ktok[:, p * 128:(p + 1) * 128], vtok[:, p * 128:(p + 1) * 128], False, True,
                           [ktok_r, vtok_r], [p2hr])
                    tt(htmp[:], p2h[:], bdm[:], ALU.mult, [p2hr, cst_r], [htmp_r])
                    tt(H32[:], htmp[:], H32[:], ALU.add, [htmp_r], [H32_r])
                    if dirn == 0:
                        tt(H32[:], H32[:], wcb, ALU.mult, [WC_r], [H32_r])
                    step_s1()
                    yield
                    if is_ctx:
                        while gS1[0] is not None:
                            step_s1()
                            yield
                        continue
                    py3 = py.rearrange("p (h e) -> p h e", h=8)
                    red(s8[:, 0, :], py3, [pyr], [s8_r])
                    act(v8(Er), py3, AF.Square, [pyr], [Er_r])
                    red(s8[:, 1, :], v8(Er), [Er_r], [s8_r])
                    ts(s8[:, 2, :], s8[:, 0, :], 1.0 / 64, None, ALU.mult, ALU.bypass, [], [s8_r])
                    tt(s8[:, 3, :], s8[:, 2, :], s8[:, 2, :], ALU.mult, [], [s8_r])
                    stt(s8[:, 4, :], s8[:, 1, :], 1.0 / 64, s8[:, 3, :], ALU.mult, ALU.subtract, [], [s8_r])
                    act(s8[:, 5, :], s8[:, 4, :], AF.Sqrt, [cst_r], [s8_r], bias=tinyc[:, 1:2])
                    rcp(s8[:, 5, :], s8[:, 5, :], [], [s8_r])
                    tt(v8(Ea), py3, s8[:, 2, :].unsqueeze(2).to_broadcast([128, 8, 64]), ALU.subtract, [pyr, s8_r],
                       [Ea_r])
                    step_s1()
                    yield
                    prk, prkr = P1.next()
                    for blk in range(4):
                        mm(prk[:, 2 * blk:2 * blk + 2], rkr[:, blk, :], ind2[:], True, True, [rkr_r, cst_r], [prkr])
                    pg, pgr = P1.next()
                    mm(pg[:], sgl[:], g2s[:], True, True, [sgl_r, cst_r], [pgr])
                    tt(v8(Ea), v8(Ea), s8[:, 5, :].unsqueeze(2).to_broadcast([128, 8, 64]), ALU.mult, [s8_r], [Ea_r])
                    tt(Ea[:], Ea[:], lnxw[:], ALU.mult, [cst_r], [Ea_r])
                    tt(Ea[:], Ea[:], lnxb[:], ALU.add, [cst_r], [Ea_r])
                    tt(v8(Ek), vtok[:].rearrange("p (h e) -> p h e", h=8),
                       prk[:, 0:8].unsqueeze(2).to_broadcast([128, 8, 64]), ALU.mult, [vtok_r, prkr], [Ek_r])
                    tt(Ea[:], Ea[:], Ek[:], ALU.add, [Ek_r], [Ea_r])
                    lt, ltr = latp.next()
                    tt(lt[:], Ea[:], pg[:], ALU.mult, [Ea_r, pgr], [ltr])
                    dma("sync", lat_d[dirn][ci * 128:(ci + 1) * 128, 512:1024], lt[:], ltr, reads=[ltr])
                    step_s1()
                    yield
                    while gS1[0] is not None:
                        step_s1()
                        yield

            run_interleaved([rwkv_gen(0), rwkv_gen(1)], skew=KSKEW_RW)
            S.flush()

    if KLIM >= 0 and KRW:
        rwkv_all()
    rw_wstack.close()


    w_out = din("w_out", [D, D])
    w_router = din("w_router", [D, 16])
    w_gate = din("w_gate", [16, D, D])
    w_up = din("w_up", [16, D, D])
    w_down = din("w_down", [16, D, D])
    tid_d = din("tidc", [128, 32 * 2])
    iota_d = din("iota512", [128, 512])
    rm16_d = din("rmask16", [128, 512])
    ones_d = din("onesc", [128, 128])


    with ExitStack() as st:
        wout = st.enter_context(nc.sbuf_tensor(U("wout"), [128, 8, D], BF16))
        wout_r = S.res("wout")
        with ExitStack() as st0:
            stg_pool = Pool(S, st0, "wstg4", [128, 8, 256], F32, 2)
            load_w_bf16(stg_pool, wout, wout_r, w_out, D)
            S.flush()
        c4 = S.res("c4")
        wr = st.enter_context(nc.sbuf_tensor(U("wr"), [128, 8, 16], F32))
        rows = st.enter_context(nc.sbuf_tensor(U("rows4"), [128, 3, D], F32))
        r_ = S.res("c4a")
        dma("gpsimd", wr[:], w_router.rearrange("(k p) e -> p k e", p=128), r_, writes=[r_, c4])
        r_ = S.res("c4b")
        dma("gpsimd", rows[:].rearrange("p a d -> p (a d)"), bcast_rows(rowmod_d, 3 * D), r_, writes=[r_, c4])
        def p4_gen(parity):
            lfp = Pool(S, st, "lf", [128, D], BF16, 2)
            lbp = Pool(S, st, "lb", [128, D], BF16, 2)
            ltp = Pool(S, st, "l4", [128, D], BF16, 1)
            lTp = Pool(S, st, "lT", [128, 8, 128], BF16, 1)
            xip = Pool(S, st, "xi4", [128, D], F32, 2)
            tmp_ = Pool(S, st, "tm4", [128, D], F32, 1)
            x1p = Pool(S, st, "x14", [128, D], F32, 1)
            h2p = Pool(S, st, "h24", [128, D], F32, 1)
            h2bp = Pool(S, st, "h2b4", [128, D], BF16, 1)
            h2Tp = Pool(S, st, "h2T4", [128, 8, 128], F32, 1)
            jkp = Pool(S, st, "jk4", [128, D], BF16, 1)
            stp4 = Pool(S, st, "st4", [128, 12], F32, 2)
            exp_ = Pool(S, st, "ex4", [128, 16], F32, 2)
            pbig = Pool(S, st, "pbig4", [128, D], F32, 1, psum=True)
            psml = Pool(S, st, "psml4", [128, 512], F32, 1, psum=True)
            yield
            for i in range(parity, NTILE if KLIM > 100 else min(NTILE, max(KLIM, 0)), 2):
                r0 = i * 128
                lf, lfr = lfp.next()
                lb, lbr = lbp.next()
                dma("sync", lf[:], lat_d[0][r0:r0 + 128, :], lfr, writes=[lfr])
                dma("sync", lb[:], lat_d[1][r0:r0 + 128, :], lbr, writes=[lbr])
                lt, ltr = ltp.next()
                tt(lt[:], lf[:], lb[:], ALU.add, [lfr, lbr], [ltr])
                pt_, ptr_ = psml.next()
                pt = pt_.bitcast(BF16)[:, 0:1024].rearrange("p (j t) -> p j t", j=8)
                for j in range(8):
                    tr(pt[:, j, :], lt[:, j * 128:(j + 1) * 128], identb[:], [ltr, ident_r], [ptr_])
                lT, lTr = lTp.next()
                act(lT[:], pt, AF.Copy, [ptr_], [lTr])
                yield
                pm, pmr = pbig.next()
                for dh in range(2):
                    for fc in range(8):
                        mm(pm[:, dh * 512:(dh + 1) * 512], lT[:, fc, :], wout[:, fc, dh * 512:(dh + 1) * 512],
                           fc == 0, fc == 7, [lTr, wout_r], [pmr])
                s4, s4r = stp4.next()
                jk, jkr = jkp.next()
                act(jk[:], pm[:], AF.Square, [pmr], [jkr, s4r], accum_out=s4[:, 0:1])
                act(s4[:, 1:2], s4[:, 0:1], AF.Sqrt, [s4r, epsc_r], [s4r], bias=epsc[:], scale=1.0 / D)
                rcp(s4[:, 2:3], s4[:, 1:2], [s4r], [s4r])
                yield
                tm, tmr = tmp_.next()
                tt(tm[:], pm[:], rows[:, 0, :], ALU.mult, [pmr, c4], [tmr])
                xi, xir = xip.next()
                dma("sync", xi[:], x[r0:r0 + 128, :], xir, writes=[xir])
                x1, x1r = x1p.next()
                stt(x1[:], tm[:], s4[:, 2:3], xi[:], ALU.mult, ALU.add, [tmr, s4r, xir], [x1r])
                dma("sync", x1_d[r0:r0 + 128, :], x1[:], x1r, reads=[x1r])
                yield
                act(jk[:], x1[:], AF.Square, [x1r], [jkr, s4r], accum_out=s4[:, 3:4])
                act(s4[:, 4:5], s4[:, 3:4], AF.Sqrt, [s4r, epsc_r], [s4r], bias=epsc[:], scale=1.0 / D)
                rcp(s4[:, 5:6], s4[:, 4:5], [s4r], [s4r])
                h2, h2r = h2p.next()
                stt(h2[:], x1[:], s4[:, 5:6], rows[:, 1, :], ALU.mult, ALU.mult, [x1r, s4r, c4], [h2r])
                tt(h2[:], h2[:], rows[:, 2, :], ALU.add, [c4], [h2r])
                h2b, h2br = h2bp.next()
                act(h2b[:], h2[:], AF.Copy, [h2r], [h2br])
                dma("sync", h2_d[r0:r0 + 128, :], h2b[:], h2br, reads=[h2br])
                yield
                p2t_, p2tr = pbig.next()
                p2t = p2t_[:].rearrange("p (j t) -> p j t", j=8)
                for j in range(8):
                    tr(p2t[:, j, :], h2[:, j * 128:(j + 1) * 128], ident[:], [h2r, ident_r], [p2tr])
                h2T, h2Tr = h2Tp.next()
                cp(h2T[:], p2t, [p2tr], [h2Tr])
                yield
                pl_, plr = psml.next()
                pl = pl_[:, 0:16]
                for kc in range(8):
                    mm(pl, h2T[:, kc, :], wr[:, kc, :], kc == 0, kc == 7, [h2Tr, c4], [plr])
                red(s4[:, 6:7], pl, [plr], [s4r], op=ALU.max)
                ts(s4[:, 7:8], s4[:, 6:7], -1.0, None, ALU.mult, ALU.bypass, [], [s4r])
                ex, exr = exp_.next()
                act(ex[:], pl, AF.Exp, [plr, s4r], [exr, s4r], bias=s4[:, 7:8], accum_out=s4[:, 8:9])
                rcp(s4[:, 9:10], s4[:, 8:9], [s4r], [s4r])
                ts(aff_all[:, i, :], ex[:], s4[:, 9:10], None, ALU.mult, ALU.bypass, [exr, s4r], [aff_r])
        run_interleaved([p4_gen(0), p4_gen(1)])
        if "aff" in debug:
            d_ = dbg_out("aff", [SEQ, 16], F32)
            dma("sync", d_.rearrange("(a p) e -> p a e", p=128), aff_all[:], aff_r, reads=[aff_r])
        S.flush()

    idxu = glob.enter_context(nc.sbuf_tensor(U("idxu"), [128, 16, 4], U32))
    gate = glob.enter_context(nc.sbuf_tensor(U("gate"), [128, 16, 4], F32))
    route_r = S.res("route")
    wstack = ExitStack()
    NEXP = 16 if KLIM > 100 else 0
    wbuf = [[(wstack.enter_context(nc.sbuf_tensor(U("wexp"), [128, 8, D], BF16)), S.res("wexp")) for _ in range(3)]
            for _ in range(2)]
    nld = [0]
    stg6 = Pool(S, wstack, "stg6", [128, 4, D], F32, 2)

    def load_expert(e_):
        for wi, wsrc in enumerate((w_gate, w_up, w_down)):
            wt, wtr = wbuf[e_ % 2][wi]
            v = wsrc[e_].rearrange("(k p) n -> p k n", p=128)
            for kh in range(2):
                nld[0] += 1
                if nld[0] % 2 == 0:
                    dma("gpsimd", wt[:, kh * 4:(kh + 1) * 4, :], v[:, kh * 4:(kh + 1) * 4, :], wtr, writes=[wtr])
                else:
                    stg, sr = stg6.next()
                    dma("sync", stg[:], v[:, kh * 4:(kh + 1) * 4, :], sr, writes=[sr])
                    if nld[0] % 4 == 1:
                        act(wt[:, kh * 4:(kh + 1) * 4, :], stg[:], AF.Copy, [sr], [wtr])
                    else:
                        cp(wt[:, kh * 4:(kh + 1) * 4, :], stg[:], [sr], [wtr])


    with ExitStack() as st:
        c5 = S.res("c5")

        def cl5(name, shape, dt, src_ap, cast_from=None):
            t = st.enter_context(nc.sbuf_tensor(U(name), shape, dt))
            r_ = S.res(name)
            if cast_from is None:
                dma("gpsimd", t[:], src_ap, r_, writes=[r_, c5])
                return t
            tf = st.enter_context(nc.sbuf_tensor(U(name + "f"), shape, cast_from))
            dma("gpsimd", tf[:], src_ap, r_, writes=[r_])
            cp(t[:], tf[:], [r_], [c5])
            return t

        onesb = cl5("onesb", [128, 128], BF16, ones_d, F32)
        sutb = cl5("sutb", [128, 128], BF16, tri_d[0], F32)
        iota = cl5("iota", [128, 512], F32, iota_d)
        rm16 = cl5("rm16", [128, 512], F32, rm16_d)
        tidc = cl5("tidc", [128, 32, 2], F32, tid_d.rearrange("p (a c) -> p a c", c=2))

        def T5(name, shape, dt):
            return st.enter_context(nc.sbuf_tensor(U(name), shape, dt)), S.res(name)

        lo, lo_r = T5("lo", [128, 16], F32)
        cand, cand_r = T5("cand", [128, 16], F32)
        cmpb, cmp_r = T5("cmpb", [128, 32, 16], BF16)
        cnt, cnt_r = T5("cnt", [128, 16], F32)
        incr, incr_r = T5("incr", [128, 16], F32)
        maskf, maskf_r = T5("maskf", [128, 32, 16], F32)
        cs5, cs5_r = T5("cs5", [128, 16, 32], F32)
        inc5, inc5_r = T5("inc5", [128, 16, 32], F32)
        pos, pos_r = T5("pos", [128, 32, 16], F32)
        pos2 = st.enter_context(nc.sbuf_tensor(U("pos2"), [128, 32, 16], F32))
        iotab = st.enter_context(nc.sbuf_tensor(U("iotab"), [128, 256], BF16))
        tv, tv_r = T5("tv", [128, 32, 16, 5], BF16)
        tmp5, tmp5_r = T5("tmp5", [128, 32, 16], F32)
        a1, a1_r = T5("a1", [128, 32, 16], F32)
        racc, racc_r = T5("racc", [128, 16, 4, 5], F32)
        idxf, idxf_r = T5("idxf", [128, 16, 4], F32)
        selp = Pool(S, st, "sel", [128, 512], BF16, 4)
        pcp = Pool(S, st, "pc5", [128, 512], F32, 2, psum=True)
        pap = Pool(S, st, "pa5", [128, 512], F32, 4, psum=True)
        S.op("vector", lambda e: e.memset(lo[:], 0.0), writes=[lo_r])
        for e0_ in range(min(2, NEXP)):
            load_expert(e0_)
        flat5 = lambda t_: t_[:].rearrange("p a e -> p (a e)")
        for it in range(27):
            cst = float(2.0 ** -(it + 1))
            ts(cand[:], lo[:], cst, None, ALU.add, ALU.bypass, [lo_r], [cand_r])
            tt(cmpb[:], aff_all[:], cand[:].unsqueeze(1).to_broadcast([128, 32, 16]), ALU.is_ge, [aff_r, cand_r],
               [cmp_r])
            pc, pcr = pcp.next()
            mm(pc[:], onesb[:], flat5(cmpb), True, True, [c5, cmp_r], [pcr])
            red(cnt[:], pc[:].rearrange("p (a e) -> p e a", e=16), [pcr], [cnt_r])
            ts(incr[:], cnt[:], 512.0, cst, ALU.is_ge, ALU.mult, [cnt_r], [incr_r])
            tt(lo[:], lo[:], incr[:], ALU.add, [incr_r], [lo_r])
        lob = lo[:].unsqueeze(1).to_broadcast([128, 32, 16])
        tt(cmpb[:], aff_all[:], lob, ALU.is_ge, [aff_r, lo_r], [cmp_r])
        tt(maskf[:], aff_all[:], lob, ALU.is_ge, [aff_r, lo_r], [maskf_r])
        pw, pwr = pcp.next()
        mm(pw[:], sutb[:], flat5(cmpb), True, True, [c5, cmp_r], [pwr])
        pcs, pcsr = pcp.next()
        mm(pcs[:], onesb[:], flat5(cmpb), True, True, [c5, cmp_r], [pcsr])
        cp(cs5[:], pcs[:].rearrange("p (a e) -> p e a", e=16), [pcsr], [cs5_r])
        S.op("vector", lambda e: e.tensor_tensor_scan(out=inc5[:].rearrange("p e a -> p (e a)"), data0=rm16[:],
                                                      data1=cs5[:].rearrange("p e a -> p (e a)"), initial=0.0,
                                                      op0=ALU.mult, op1=ALU.add),
             reads=[c5, cs5_r], writes=[inc5_r])
        tt(inc5[:], inc5[:], cs5[:], ALU.subtract, [cs5_r], [inc5_r])
        tt(pos[:], pw[:].rearrange("p (a e) -> p a e", e=16), inc5[:].rearrange("p e a -> p a e"), ALU.add,
           [pwr, inc5_r], [pos_r])
        ts(tmp5[:], maskf[:], -1.0e4, 1.0e4, ALU.mult, ALU.add, [maskf_r], [tmp5_r])
        tt(pos[:], pos[:], tmp5[:], ALU.add, [tmp5_r], [pos_r])
        ts(pos2[:], pos[:], -256.0, None, ALU.add, ALU.bypass, [pos_r], [pos_r])
        cp(iotab[:], iota[:, 0:256], [c5], [c5])
        for c_ in range(2):
            cp(tv[:, :, :, c_], tidc[:, :, c_:c_ + 1].to_broadcast([128, 32, 16]), [c5], [tv_r])
        cp(tv[:, :, :, 2], aff_all[:], [aff_r], [tv_r])
        tt(a1[:], aff_all[:], tv[:, :, :, 2], ALU.subtract, [aff_r, tv_r], [a1_r])
        cp(tv[:, :, :, 3], a1[:], [a1_r], [tv_r])
        tt(a1[:], a1[:], tv[:, :, :, 3], ALU.subtract, [tv_r], [a1_r])
        cp(tv[:, :, :, 4], a1[:], [a1_r], [tv_r])
        posA = st.enter_context(nc.sbuf_tensor(U("posA"), [128, 32, 16], BF16))
        posB = st.enter_context(nc.sbuf_tensor(U("posB"), [128, 32, 16], BF16))
        pk_r = S.res("poskeys")
        ts(tmp5[:], pos[:], 256.0, None, ALU.is_lt, ALU.bypass, [pos_r], [tmp5_r])
        stt(a1[:], pos[:], 1.0, tmp5[:], ALU.add, ALU.mult, [pos_r, tmp5_r], [a1_r])
        ts(posA[:], a1[:], -1.0, None, ALU.add, ALU.bypass, [a1_r], [pk_r])
        ts(tmp5[:], pos[:], 256.0, None, ALU.is_ge, ALU.bypass, [pos_r], [tmp5_r])
        ts(a1[:], pos[:], 512.0, None, ALU.is_lt, ALU.bypass, [pos_r], [a1_r])
        tt(tmp5[:], tmp5[:], a1[:], ALU.mult, [a1_r], [tmp5_r])
        stt(a1[:], pos[:], -255.0, tmp5[:], ALU.add, ALU.mult, [pos_r, tmp5_r], [a1_r])
        ts(posB[:], a1[:], -1.0, None, ALU.add, ALU.bypass, [a1_r], [pk_r])
        selAp = Pool(S, st, "selA", [128, 32, 256], BF16, 1)
        selBp = Pool(S, st, "selB", [128, 32, 256], BF16, 1)
        iob = iotab[:].unsqueeze(1).to_broadcast([128, 32, 256])
        for e_ in range(16 if KLIM > 100 else 0):
            pa0, par0 = pap.next()
            pa1, par1 = pap.next()
            pav = [pa0[:, 0:320].rearrange("p (j a c) -> p j a c", j=2, a=32),
                   pa1[:, 0:320].rearrange("p (j a c) -> p j a c", j=2, a=32)]
            parr = [par0, par1]
            sA, sAr = selAp.next()
            sB, sBr = selBp.next()
            tt(sA[:], iob, posA[:, :, e_:e_ + 1].to_broadcast([128, 32, 256]), ALU.is_equal, [c5, pk_r], [sAr])
            tt(sB[:], iob, posB[:, :, e_:e_ + 1].to_broadcast([128, 32, 256]), ALU.is_equal, [c5, pk_r], [sBr])
            for a_ in range(32):
                for j in range(4):
                    sel_, selr_ = (sA, sAr) if j < 2 else (sB, sBr)
                    mm(pav[j // 2][:, j % 2, a_, :], sel_[:, a_, (j % 2) * 128:(j % 2 + 1) * 128], tv[:, a_, e_, :],
                       True, True, [selr_, tv_r], [parr[j // 2]])
            for jj in range(2):
                red(racc[:, e_, 2 * jj:2 * jj + 2], pav[jj].rearrange("p j a c -> p j c a"), [parr[jj]], [racc_r])
        stt(idxf[:], racc[:, :, :, 0], 64.0, racc[:, :, :, 1], ALU.mult, ALU.add, [racc_r], [idxf_r])
        cp(idxu[:], idxf[:], [idxf_r], [route_r])
        tt(gate[:], racc[:, :, :, 2], racc[:, :, :, 3], ALU.add, [racc_r], [route_r])
        tt(gate[:], gate[:], racc[:, :, :, 4], ALU.add, [racc_r], [route_r])
        if "route" in debug:
            d_ = dbg_out("idx", [128, 64], U32)
            dma("sync", d_, idxu[:].rearrange("p e j -> p (e j)"), route_r, reads=[route_r])
            d2_ = dbg_out("gate", [128, 64], F32)
            r2_ = S.res("gdbg")
            dma("sync", d2_, gate[:].rearrange("p e j -> p (e j)"), r2_, reads=[route_r])
        S.flush()

    with ExitStack() as st:
        ym_r = S.res("ymoe")
        xsTp = Pool(S, st, "xsT", [128, 8, 512], BF16, 2)
        hidp = Pool(S, st, "hidT", [128, 8, 512], BF16, 1)
        silp = Pool(S, st, "sil", [128, 512], F32, 2)
        yep = Pool(S, st, "ye", [128, D], F32, 2)
        ptx = Pool(S, st, "ptx", [128, 8, 128], BF16, 2, psum=True)
        pgu = Pool(S, st, "pgu", [128, 512], F32, 4, psum=True)
        pdn = Pool(S, st, "pdn", [128, 512], F32, 2, psum=True)
        xs_slots = [(st.enter_context(nc.sbuf_tensor(U("xsg"), [128, 4, D], BF16)), [S.res("xsg") for _ in range(4)])
                    for _ in range(2)]

        def gather(e_):
            xs, xsrs = xs_slots[e_ % 2]
            for j in range(4):
                S.dma("gpsimd", lambda e, xs=xs, j=j, e_=e_: e.indirect_dma_start(
                    out=xs[:, j, :], out_offset=None, in_=h2_d,
                    in_offset=bass.IndirectOffsetOnAxis(ap=idxu[:, e_, j:j + 1], axis=0)),
                    xsrs[j], reads=[route_r], writes=[xsrs[j]])

        if NEXP:
            gather(0)
        for e_ in range(NEXP):
            if e_ + 1 < NEXP:
                if e_ + 1 >= 2:
                    load_expert(e_ + 1)
                gather(e_ + 1)
            (wg, wgr), (wu, wur), (wd, wdr) = wbuf[e_ % 2]
            xs, xsrs = xs_slots[e_ % 2]
            xsT, xsTr = xsTp.next()
            for j in range(4):
                pt, ptr_ = ptx.next()
                for kc in range(8):
                    tr(pt[:, kc, :], xs[:, j, kc * 128:(kc + 1) * 128], identb[:], [xsrs[j], ident_r], [ptr_])
                act(xsT[:, :, j * 128:(j + 1) * 128], pt[:], AF.Copy, [ptr_], [xsTr])
            hid, hidr = hidp.next()
            for fc in range(8):
                pg_, pgr_ = pgu.next()
                pu_, pur_ = pgu.next()
                for kc in range(8):
                    mm(pg_[:], wg[:, kc, fc * 128:(fc + 1) * 128], xsT[:, kc, :], kc == 0, kc == 7, [wgr, xsTr], [pgr_])
                for kc in range(8):
                    mm(pu_[:], wu[:, kc, fc * 128:(fc + 1) * 128], xsT[:, kc, :], kc == 0, kc == 7, [wur, xsTr], [pur_])
                sl, slr = silp.next()
                act(sl[:], pg_[:], AF.Silu, [pgr_], [slr])
                tt(hid[:, fc, :], pu_[:], sl[:], ALU.mult, [pur_, slr], [hidr])
            for j in range(4):
                ye, yer = yep.next()
                for dh in range(2):
                    pd_, pdr_ = pdn.next()
                    for fc in range(8):
                        mm(pd_[:], hid[:, fc, j * 128:(j + 1) * 128], wd[:, fc, dh * 512:(dh + 1) * 512], fc == 0, fc == 7,
                           [hidr, wdr], [pdr_])
                    if dh == 0:
                        act(ye[:, 0:512], pd_[:], AF.Copy, [pdr_, route_r], [yer], scale=gate[:, e_, j:j + 1])
                    else:
                        ts(ye[:, 512:1024], pd_[:], gate[:, e_, j:j + 1], None, ALU.mult, ALU.bypass, [pdr_, route_r],
                           [yer])
                S.dma("gpsimd", lambda e, ye=ye, j=j, e_=e_: e.indirect_dma_start(
                    out=ymoe_d, out_offset=bass.IndirectOffsetOnAxis(ap=idxu[:, e_, j:j + 1], axis=0),
                    in_=ye[:], in_offset=None, compute_op=ALU.add),
                    yer, reads=[yer, route_r], writes=[ym_r])
        S.flush()

    wstack.close()
    with ExitStack() as st:
        g2row = st.enter_context(nc.sbuf_tensor(U("g2row"), [128, D], F32))
        g2r = S.res("g2row")
        dma("gpsimd", g2row[:], bcast_rows(rowmod_d[3 * D:4 * D], D), g2r, writes=[g2r])
        x1p = Pool(S, st, "x17", [128, D], F32, 3)
        ymp = Pool(S, st, "ym7", [128, D], F32, 3)
        jk7 = Pool(S, st, "jk7", [128, D], BF16, 1)
        t7p = Pool(S, st, "t7", [128, D], F32, 2)
        o7p = Pool(S, st, "o7", [128, D], F32, 3)
        s7p = Pool(S, st, "s7", [128, 4], F32, 3)
        for i in range(NTILE):
            r0 = i * 128
            x1, x1r = x1p.next()
            ym, ymr = ymp.next()
            dma("sync", x1[:], x1_d[r0:r0 + 128, :], x1r, writes=[x1r])
            dma("gpsimd", ym[:], ymoe_d[r0:r0 + 128, :], ymr, writes=[ymr])
            s7, s7r = s7p.next()
            jk, jkr = jk7.next()
            act(jk[:], ym[:], AF.Square, [ymr], [jkr, s7r], accum_out=s7[:, 0:1])
            act(s7[:, 1:2], s7[:, 0:1], AF.Sqrt, [s7r, epsc_r], [s7r], bias=epsc[:], scale=1.0 / D)
            rcp(s7[:, 2:3], s7[:, 1:2], [s7r], [s7r])
            t7, t7r = t7p.next()
            tt(t7[:], ym[:], g2row[:], ALU.mult, [ymr, g2r], [t7r])
            o7, o7r = o7p.next()
            stt(o7[:], t7[:], s7[:, 2:3], x1[:], ALU.mult, ALU.add, [t7r, s7r, x1r], [o7r])
            dma("scalar" if i % 2 == 0 else "sync", out[r0:r0 + 128, :], o7[:], o7r, reads=[o7r])
        S.flush()

    glob.close()
    return nc, dbg


def make_consts():
    c = {}
    c["ident"] = np.eye(128, dtype=np.float32)
    prot = np.zeros((128, 128), np.float32)
    for o in (0, 64):
        for i in range(32):
            prot[o + i + 32, o + i] = -1.0
            prot[o + i, o + 32 + i] = 1.0
    c["prot"] = prot
    t = np.arange(SEQ)
    row = (t // 64).astype(np.float64)
    col = (t % 64).astype(np.float64)
    freq = 10000.0 ** (-np.arange(16, dtype=np.float64) / 16)
    ang = np.concatenate([row[:, None] * freq, col[:, None] * freq], axis=-1)
    cosT = np.ones((128, NT), np.float64)
    sinT = np.zeros((128, NT), np.float64)
    for p in range(128):
        f = p % 32
        cosT[p, LAT0:LAT0 + SEQ] = np.cos(ang[:, f].astype(np.float32).astype(np.float64))
        sinT[p, LAT0:LAT0 + SEQ] = np.sin(ang[:, f].astype(np.float32).astype(np.float64))
    c["cosT"] = cosT.astype(np.float32)
    c["sinT"] = sinT.astype(np.float32)
    lg = np.log1p(-np.exp2(-5.0 - np.arange(8, dtype=np.float64)))
    i = np.arange(128, dtype=np.float64)
    qdec = np.zeros((2, 128, 4, 128))
    kdec = np.zeros((2, 128, 8, 64))
    dmask = np.zeros((2, 128, 8, 128))
    gC = np.zeros((128, 4, 128))
    for h in range(8):
        p, hf = h // 2, h % 2
        qdec[0, hf * 64:(hf + 1) * 64, p, :] = np.exp(lg[h] * (i + 1))[None, :]
        qdec[1, hf * 64:(hf + 1) * 64, p, :] = np.exp(lg[h] * (128 - i))[None, :]
        kdec[0, :, h, :] = 0.125 * np.exp(-lg[h] * (i + 1))[:, None]
        kdec[1, :, h, :] = 0.125 * np.exp(-lg[h] * (128 - i))[:, None]
        jj, ii = np.meshgrid(i, i, indexing="ij")
        dmask[0, :, h, :] = 0.125 * np.where(ii >= jj, np.exp(lg[h] * np.maximum(ii - jj, 0)), 0.0)
        dmask[1, :, h, :] = 0.125 * np.where(jj >= ii, np.exp(lg[h] * np.maximum(jj - ii, 0)), 0.0)
        gC[hf * 64:(hf + 1) * 64, p, hf * 64:(hf + 1) * 64] = np.exp(lg[h] * 128)
    c["qdec"] = qdec.reshape(2, 128, 512).astype(np.float32)
    c["kdec"] = kdec.reshape(2, 128, 512).astype(np.float32)
    c["dmask"] = dmask.reshape(2, 128, 1024).astype(np.float32)
    c["gC"] = gC.reshape(128, 512).astype(np.float32)

    tri = np.zeros((4, 128, 128), np.float32)
    s_, t_ = np.meshgrid(np.arange(128), np.arange(128), indexing="ij")
    tri[0] = (s_ < t_); tri[1] = (s_ <= t_); tri[2] = (s_ > t_); tri[3] = (s_ >= t_)
    c["tri"] = tri
    bd = np.zeros((128, 4, 128), np.float32)
    bd[0:64, :, 0:64] = 1.0
    bd[64:128, :, 64:128] = 1.0
    c["bdmask"] = bd.reshape(128, 512)
    rm = np.ones((128, 4, 128), np.float32)
    rm[:, :, 0] = 0.0
    c["rmask"] = rm.reshape(128, 512)
    ind2 = np.zeros((128, 2), np.float32)
    ind2[0:64, 0] = 1.0
    ind2[64:128, 1] = 1.0
    c["ind2"] = ind2
    bo = np.zeros((128, 128), np.float32)
    bo[0:64, 0:64] = 1.0
    bo[64:128, 64:128] = 1.0
    c["bones"] = bo
    lv = np.zeros((14, 128, 128), np.uint32)
    for L in range(1, 8):
        B = 2 ** L
        hb = B // 2
        mk = (s_ // B == t_ // B) & (s_ % B >= hb) & (t_ % B < hb)
        lv[L - 1] = mk
        lv[7 + L - 1] = mk.T
    c["lvlmask"] = lv
    tid = np.zeros((128, 32, 2), np.float32)
    tok = np.arange(32)[None, :] * 128 + np.arange(128)[:, None]
    tid[:, :, 0] = tok // 64
    tid[:, :, 1] = tok % 64
    c["tidc"] = tid.reshape(128, 64)
    c["iota512"] = np.tile(np.arange(512, dtype=np.float32)[None, :], (128, 1))
    rm = np.ones((128, 16, 32), np.float32)
    rm[:, :, 0] = 0.0
    c["rmask16"] = rm.reshape(128, 512)
    c["onesc"] = np.ones((128, 128), np.float32)
    return c


def make_in_maps(inputs):
    consts = make_consts()
    maps = []
    for b in range(NCORES):
        m = {}
        m["x"] = np.ascontiguousarray(inputs["x"][b])
        m["ctx"] = np.ascontiguousarray(inputs["ctx"][b])
        cc = np.concatenate([inputs["c"][b].reshape(8, 128).T, inputs["c_ctx"].reshape(8, 128).T], axis=1)
        m["ccol"] = np.ascontiguousarray(cc.astype(np.float32))
        m["w_mod"] = np.ascontiguousarray(inputs["w_mod"][0])
        m["b_mod"] = np.ascontiguousarray(inputs["b_mod"][0])
        m["gains"] = np.ascontiguousarray(inputs["norm_gains"][0])
        m["w_in"] = np.ascontiguousarray(inputs["w_in"][0])
        RC_ = 2560
        conv = inputs["rwkv_conv"][0]
        cwT = np.zeros((2, 128, 14, 3), np.float32)
        for dr in range(2):
            cols = list(range(0, 1536)) + list(range(1536 + 64 * dr, 1600 + 64 * dr)) + list(range(1664, 1728)) + \
                list(range(1728 + 128 * dr, 1856 + 128 * dr))
            cwT[dr] = conv[:, cols].T.reshape(14, 128, 3).transpose(1, 0, 2)
        m["cwT"] = np.ascontiguousarray(cwT.reshape(2, 128, 42))
        w2pad = np.zeros((2, 128, 512), np.float32)
        w2pad[:, 0:64, :] = inputs["rwkv_w2"][0]
        m["w2pad"] = w2pad
        a2pad = np.zeros((128, 512), np.float32)
        a2pad[64:128, :] = inputs["rwkv_a2"][0]
        m["a2pad"] = a2pad
        m["g2"] = np.ascontiguousarray(inputs["rwkv_g2"][0])
        colv = lambda v: v.reshape(4, 128).T
        rwcols = np.zeros((128, 4, 8), np.float32)
        rwcols[:, :, 0] = colv(inputs["rwkv_w0"][0, 0])
        rwcols[:, :, 1] = colv(inputs["rwkv_w0"][0, 1])
        rwcols[:, :, 2] = colv(inputs["rwkv_a0"][0])
        rwcols[:, :, 3] = colv(inputs["rwkv_k_k"][0])
        rwcols[:, :, 4] = colv(inputs["rwkv_k_a"][0])
        rwcols[:, :, 5] = colv(inputs["rwkv_r_k"][0])
        m["rwcols"] = np.ascontiguousarray(rwcols.reshape(128, 32))
        m["w_out"] = np.ascontiguousarray(inputs["w_out"][0])
        m["w_router"] = np.ascontiguousarray(inputs["w_router"][0])
        m["w_gate"] = np.ascontiguousarray(inputs["w_gate"][0])
        m["w_up"] = np.ascontiguousarray(inputs["w_up"][0])
        m["w_down"] = np.ascontiguousarray(inputs["w_down"][0])
        m["lnx"] = np.ascontiguousarray(np.stack([inputs["rwkv_lnx_w"][0], inputs["rwkv_lnx_b"][0]], axis=0))
        m.update(consts)
        maps.append(m)
    return maps


def kernel(**inputs):
    inputs = {k: np.asarray(v) for k, v in inputs.items()}
    nc, _ = build()
    maps = make_in_maps(inputs)
    res = run_bass_kernel_spmd(nc, maps, core_ids=list(range(NCORES)))
    return np.stack([r["out"] for r in res.results], axis=0).astype(np.float32)
```
